# Optimizing a Trainium2 kernel written in Bass

```python
import math
import jax
import jax.numpy as jnp
from jax import lax
import numpy as np

D_MODEL = 2048
BATCH = 4
SEQ = 4096
DEPTH = 4

HEAD_DIM = 64
MOBA_HEADS = 12
MOBA_BLOCK = 256
MOBA_TOPK = 3
MOBA_Q_CHUNK = 32
MOBA_WIDTH = MOBA_HEADS * HEAD_DIM
RWKV_HEADS = 12
RWKV_WIDTH = RWKV_HEADS * HEAD_DIM
RWKV_DECAY_LORA = 64
RWKV_A_LORA = 64
RWKV_MV_LORA = 32
RWKV_GATE_LORA = 128
RWKV_COLS = 3 * RWKV_WIDTH + RWKV_DECAY_LORA + RWKV_A_LORA + RWKV_GATE_LORA
RWKV_GN_EPS = 64e-5
DIL_GROUPS = ((128, 1), (512, 4), (2048, 16))
DIL_HEADS_PER_GROUP = 4
DIL_HEADS = DIL_HEADS_PER_GROUP * len(DIL_GROUPS)
DIL_WIDTH = DIL_HEADS * HEAD_DIM
DIL_OUT_WIDTH = DIL_HEADS_PER_GROUP * HEAD_DIM
REL_BUCKETS = 32
REL_MAX_DISTANCE = 2048
ATTN_HEADS = MOBA_HEADS + DIL_HEADS
N_BRANCH = 3
OFF_A = 0
OFF_C = OFF_A + 3 * MOBA_WIDTH
OFF_B = OFF_C + 3 * DIL_WIDTH
OFF_G = OFF_B + RWKV_COLS
IN_COLS = OFF_G + N_BRANCH * D_MODEL
MOE_GROUPS = 8
MOE_EXPERTS_PER_GROUP = 8
MOE_EXPERTS = MOE_GROUPS * MOE_EXPERTS_PER_GROUP
MOE_TOPK = 2
EXPERT_FF = D_MODEL // 8
MOE_BLOCK = 256
LN_EPS = 1e-5
DEEPNORM_ALPHA = (2 * DEPTH) ** 0.25
DEEPNORM_BETA = (8 * DEPTH) ** -0.25

kernel_name = 'hybrid_moba_rwkv7_dilated_hmoe'


def layer_norm(x, g, b):
    xf = x.astype(jnp.float32)
    mu = jnp.mean(xf, axis=-1, keepdims=True)
    var = jnp.mean(jnp.square(xf - mu), axis=-1, keepdims=True)
    return ((xf - mu) * lax.rsqrt(var + LN_EPS) * g + b).astype(x.dtype)


def t5_bucket(dist):
    n = jnp.maximum(dist, 0)
    max_exact = REL_BUCKETS // 2
    nf = jnp.maximum(n, 1).astype(jnp.float32)
    large = max_exact + (jnp.log(nf / max_exact) / math.log(REL_MAX_DISTANCE / max_exact)
                         * (REL_BUCKETS - max_exact)).astype(jnp.int32)
    large = jnp.minimum(large, REL_BUCKETS - 1)
    return jnp.where(n < max_exact, n, large)


def moba_attention(q, k, v, bias_h):
    B, H, L, hd = q.shape
    nb = L // MOBA_BLOCK
    n_sel = min(MOBA_TOPK, nb)
    scale = hd ** -0.5
    kb = k.reshape(B, H, nb, MOBA_BLOCK, hd)
    vb = v.reshape(B, H, nb, MOBA_BLOCK, hd)
    k_mean = jnp.mean(kb.astype(jnp.float32), axis=3)
    blk_ids = jnp.arange(nb)
    in_blk = jnp.arange(MOBA_BLOCK)
    head_ix = jnp.arange(H)[None, :, None, None, None]
    gather = jax.vmap(jax.vmap(lambda xb, idx: xb[idx]))

    def one_chunk(ci):
        start = ci * MOBA_Q_CHUNK
        qc = lax.dynamic_slice_in_dim(q, start, MOBA_Q_CHUNK, axis=2)
        qpos = start + jnp.arange(MOBA_Q_CHUNK)
        qblk = start // MOBA_BLOCK
        gate = jnp.einsum('bhqd,bhnd->bhqn', qc.astype(jnp.float32), k_mean)
        gate = jnp.where(blk_ids < qblk, gate, -jnp.inf)
        _, sel = lax.top_k(gate, n_sel)
        k_sel = gather(kb, sel)
        v_sel = gather(vb, sel)
        s_sel = jnp.einsum('bhqd,bhqnjd->bhqnj', qc, k_sel).astype(jnp.float32) * scale
        kpos = sel[..., None] * MOBA_BLOCK + in_blk
        s_sel = s_sel + bias_h[head_ix, t5_bucket(qpos[:, None, None] - kpos)]
        s_sel = jnp.where((sel < qblk)[..., None], s_sel, -jnp.inf)
        k_own = lax.dynamic_index_in_dim(kb, qblk, axis=2, keepdims=False)
        v_own = lax.dynamic_index_in_dim(vb, qblk, axis=2, keepdims=False)
        s_own = jnp.einsum('bhqd,bhjd->bhqj', qc, k_own).astype(jnp.float32) * scale
        d_own = qpos[:, None] - (qblk * MOBA_BLOCK + in_blk)[None, :]
        s_own = s_own + bias_h[:, t5_bucket(d_own)]
        s_own = jnp.where(d_own >= 0, s_own, -jnp.inf)
        logits = jnp.concatenate([s_sel.reshape(B, H, MOBA_Q_CHUNK, n_sel * MOBA_BLOCK), s_own], axis=-1)
        p = jax.nn.softmax(logits, axis=-1)
        p_sel = p[..., :n_sel * MOBA_BLOCK].reshape(B, H, MOBA_Q_CHUNK, n_sel, MOBA_BLOCK).astype(v.dtype)
        p_own = p[..., n_sel * MOBA_BLOCK:].astype(v.dtype)
        return (jnp.einsum('bhqnj,bhqnjd->bhqd', p_sel, v_sel)
                + jnp.einsum('bhqj,bhjd->bhqd', p_own, v_own))

    out = lax.map(one_chunk, jnp.arange(L // MOBA_Q_CHUNK))
    return out.transpose(1, 2, 0, 3, 4).reshape(B, H, L, hd)


def dilated_group(q, k, v, bias_h, window, dilation):
    B, G, L, hd = q.shape
    span = window // dilation
    M = L // dilation
    Mp = -(-M // span) * span
    nb = Mp // span

    def to_res(t):
        t = t.reshape(B, G, M, dilation, hd).transpose(0, 1, 3, 2, 4)
        t = jnp.pad(t, ((0, 0), (0, 0), (0, 0), (0, Mp - M), (0, 0)))
        return t.reshape(B, G, dilation, nb, span, hd)

    def with_prev(t):
        prev = jnp.pad(t, ((0, 0), (0, 0), (0, 0), (1, 0), (0, 0), (0, 0)))[:, :, :, :nb]
        return jnp.concatenate([prev, t], axis=4)

    qb, kb, vb = to_res(q), to_res(k), to_res(v)
    kc, vc = with_prev(kb), with_prev(vb)
    s = jnp.einsum('bgrnid,bgrnjd->bgrnij', qb, kc).astype(jnp.float32) * hd ** -0.5
    rel = span + jnp.arange(span)[:, None] - jnp.arange(2 * span)[None, :]
    n_k = jnp.arange(nb)[:, None, None] * span - span + jnp.arange(2 * span)[None, None, :]
    valid = (rel >= 0) & (rel <= span) & (n_k >= 0)
    bias = bias_h[:, t5_bucket(rel * dilation)]
    s = jnp.where(valid, s + bias[None, :, None, None], -jnp.inf)
    m = jnp.max(s, axis=-1)
    e = jnp.exp(s - m[..., None])
    l = jnp.sum(e, axis=-1)
    o = jnp.einsum('bgrnij,bgrnjd->bgrnid', e, vc.astype(jnp.float32)) / l[..., None]

    def from_res(t):
        tail = t.shape[5:]
        t = t.reshape((B, G, dilation, Mp) + tail)[:, :, :, :M]
        return jnp.moveaxis(t, 2, 3).reshape((B, G, L) + tail)

    return from_res(o), from_res(m), from_res(l)


def dilated_attention(q, k, v, bias_h):
    outs, maxs, dens = [], [], []
    for g, (window, dilation) in enumerate(DIL_GROUPS):
        sl = slice(g * DIL_HEADS_PER_GROUP, (g + 1) * DIL_HEADS_PER_GROUP)
        o, m, l = dilated_group(q[:, sl], k[:, sl], v[:, sl], bias_h[sl], window, dilation)
        outs.append(o)
        maxs.append(m)
        dens.append(l)
    o_all, m_all, l_all = jnp.stack(outs), jnp.stack(maxs), jnp.stack(dens)
    wgt = l_all * jnp.exp(m_all - jnp.max(m_all, axis=0))
    return jnp.sum(wgt[..., None] * o_all, axis=0) / jnp.sum(wgt, axis=0)[..., None]


def rwkv7_time_mix(z, v_first, v_res, mu, w0, w_up, a0, a_up, g_up, k_k, k_a, r_k, ln_g, ln_b):
    B, S, _ = z.shape
    H, N, W = RWKV_HEADS, HEAD_DIM, RWKV_WIDTH
    zf = z.astype(jnp.float32)
    z_prev = jnp.pad(zf, ((0, 0), (1, 0), (0, 0)))[:, :S]
    zf = zf + mu * (z_prev - zf)
    r, k, v = zf[..., :W], zf[..., W:2 * W], zf[..., 2 * W:3 * W]
    o = 3 * W
    w_lo = zf[..., o:o + RWKV_DECAY_LORA]
    a_lo = zf[..., o + RWKV_DECAY_LORA:o + RWKV_DECAY_LORA + RWKV_A_LORA]
    g_lo = zf[..., o + RWKV_DECAY_LORA + RWKV_A_LORA:]
    log_w = -jax.nn.softplus(-(w0 + jnp.tanh(w_lo) @ w_up)) - 0.5
    decay = jnp.exp(-jnp.exp(log_w))
    a = jax.nn.sigmoid(a0 + a_lo @ a_up)
    g = jax.nn.sigmoid(g_lo) @ g_up
    if v_res is None:
        v_first = v
    else:
        v0, mv_down, mv_up = v_res
        v = v + (v_first - v) * jax.nn.sigmoid(v0 + (v @ mv_down) @ mv_up)
    to_h = lambda t: t.reshape(B, S, H, N)
    kk = to_h(k * k_k)
    kk = kk / jnp.maximum(jnp.sqrt(jnp.sum(kk * kk, axis=-1, keepdims=True)), 1e-12)
    k = k * (1.0 + (a - 1.0) * k_a)
    rh, wh, kh, vh, ah = to_h(r), to_h(decay), to_h(k), to_h(v), to_h(a)
    xs = tuple(jnp.moveaxis(t, 1, 0) for t in (rh, wh, kh, vh, kk, ah))

    def step(state, inp):
        r_t, w_t, k_t, v_t, kk_t, a_t = inp
        s_kk = jnp.einsum('bhij,bhj->bhi', state, kk_t)
        state = (state * w_t[:, :, None, :]
                 - s_kk[..., None] * (kk_t * a_t)[:, :, None, :]
                 + v_t[..., None] * k_t[:, :, None, :])
        return state, jnp.einsum('bhij,bhj->bhi', state, r_t)

    state0 = jnp.zeros((B, H, N, N), jnp.float32)
    _, y = lax.scan(step, state0, xs)
    y = jnp.moveaxis(y, 0, 1)
    mean = jnp.mean(y, axis=-1, keepdims=True)
    var = jnp.mean(jnp.square(y - mean), axis=-1, keepdims=True)
    y = ((y - mean) * lax.rsqrt(var + RWKV_GN_EPS)).reshape(B, S, W) * ln_g + ln_b
    bonus = jnp.sum(rh * kh * r_k.reshape(H, N), axis=-1, keepdims=True) * vh
    y = (y + bonus.reshape(B, S, W)) * g
    return y, v_first


def token_mixer(h, bias_h, v_first, v_res, w_in, p_a, p_b, p_c, w_o,
                mu, w0, w_up, a0, a_up, g_up, k_k, k_a, r_k, ln_g, ln_b):
    B, S, D = h.shape
    L = -(-S // MOBA_BLOCK) * MOBA_BLOCK
    proj = h @ w_in

    def cols(off, width):
        return proj[..., off:off + width]

    def heads(t, n):
        t = jnp.pad(t, ((0, 0), (0, L - S), (0, 0)))
        return t.reshape(B, L, n, HEAD_DIM).transpose(0, 2, 1, 3)

    qa = heads(cols(OFF_A, MOBA_WIDTH), MOBA_HEADS)
    ka = heads(cols(OFF_A + MOBA_WIDTH, MOBA_WIDTH), MOBA_HEADS)
    va = heads(cols(OFF_A + 2 * MOBA_WIDTH, MOBA_WIDTH), MOBA_HEADS)
    o_a = moba_attention(qa, ka, va, bias_h[:MOBA_HEADS])
    o_a = o_a[:, :, :S].transpose(0, 2, 1, 3).reshape(B, S, MOBA_WIDTH).astype(h.dtype)
    qc = heads(cols(OFF_C, DIL_WIDTH), DIL_HEADS)
    kc = heads(cols(OFF_C + DIL_WIDTH, DIL_WIDTH), DIL_HEADS)
    vc = heads(cols(OFF_C + 2 * DIL_WIDTH, DIL_WIDTH), DIL_HEADS)
    o_c = dilated_attention(qc, kc, vc, bias_h[MOBA_HEADS:])
    o_c = o_c[:, :, :S].transpose(0, 2, 1, 3).reshape(B, S, DIL_OUT_WIDTH).astype(h.dtype)
    o_b, v_first = rwkv7_time_mix(cols(OFF_B, RWKV_COLS), v_first, v_res, mu, w0, w_up,
                                  a0, a_up, g_up, k_k, k_a, r_k, ln_g, ln_b)
    o_b = o_b.astype(h.dtype)
    gates = jax.nn.sigmoid(cols(OFF_G, N_BRANCH * D).astype(jnp.float32)).astype(h.dtype)
    g_a, g_b, g_c = gates[..., :D], gates[..., D:2 * D], gates[..., 2 * D:]
    merged = g_a * (o_a @ p_a) + g_b * (o_b @ p_b) + g_c * (o_c @ p_c)
    return merged @ w_o, v_first


def routed_experts(t, expert_id, gate_w, w_gate, w_up, w_down):
    T, D = t.shape
    E = w_gate.shape[0]
    A = T * MOE_TOPK
    n_blocks = (A + E * (MOE_BLOCK - 1) + MOE_BLOCK - 1) // MOE_BLOCK
    flat_e = expert_id.reshape(A)
    flat_tok = jnp.repeat(jnp.arange(T, dtype=jnp.int32), MOE_TOPK)
    flat_w = gate_w.reshape(A)
    order = jnp.argsort(flat_e)
    e_s, tok_s, w_s = flat_e[order], flat_tok[order], flat_w[order]
    counts = jnp.bincount(flat_e, length=E)
    padded = (counts + MOE_BLOCK - 1) // MOE_BLOCK * MOE_BLOCK
    pad_end = jnp.cumsum(padded)
    pad_start = pad_end - padded
    start = jnp.cumsum(counts) - counts
    dest = pad_start[e_s] + jnp.arange(A) - start[e_s]
    slot_tok = jnp.full((n_blocks * MOE_BLOCK,), T, jnp.int32).at[dest].set(tok_s)
    slot_w = jnp.zeros((n_blocks * MOE_BLOCK,), jnp.float32).at[dest].set(w_s)
    block_e = jnp.minimum(jnp.searchsorted(pad_end, jnp.arange(n_blocks) * MOE_BLOCK, side='right'), E - 1)
    t_pad = jnp.concatenate([t, jnp.zeros((1, D), t.dtype)], axis=0)

    def run_block(args):
        toks, e = args
        xb = t_pad[toks]
        hid = jax.nn.silu(xb @ w_gate[e]) * (xb @ w_up[e])
        return hid @ w_down[e]

    out = lax.map(run_block, (slot_tok.reshape(n_blocks, MOE_BLOCK), block_e))
    out = out.reshape(n_blocks * MOE_BLOCK, D) * slot_w[:, None].astype(t.dtype)
    return jnp.zeros((T + 1, D), t.dtype).at[slot_tok].add(out)[:T]


def hier_moe(h, w_grp, b_grp, w_exp, b_exp, w_gate, w_up, w_down):
    B, S, D = h.shape
    T = B * S
    t = h.reshape(T, D)
    tf = t.astype(jnp.float32)
    grp_prob = jax.nn.softmax(tf @ w_grp.astype(jnp.float32) + b_grp.astype(jnp.float32), axis=-1)
    grp_p, grp_i = lax.top_k(grp_prob, 1)
    exp_logits = (tf @ w_exp.astype(jnp.float32) + b_exp.astype(jnp.float32)).reshape(T, MOE_GROUPS, MOE_EXPERTS_PER_GROUP)
    in_grp = exp_logits[jnp.arange(T), grp_i[:, 0]]
    top_l, top_i = lax.top_k(in_grp, MOE_TOPK)
    gate_w = grp_p * jax.nn.softmax(top_l, axis=-1)
    expert_id = grp_i * MOE_EXPERTS_PER_GROUP + top_i
    return routed_experts(t, expert_id, gate_w, w_gate, w_up, w_down).reshape(B, S, D)


def setup_inputs(seed: int = 0) -> dict:
    key = jax.random.key(seed)
    keys = iter(jax.random.split(key, 48))

    def nrm(shape, scale):
        return jax.random.normal(next(keys), shape, jnp.float32) * scale

    def unif(shape):
        return jax.random.uniform(next(keys), shape, jnp.float32)

    D, L, W = D_MODEL, DEPTH, RWKV_WIDTH
    E, F, G = MOE_EXPERTS, EXPERT_FF, MOE_GROUPS
    n_res = max(L - 1, 0)
    col_scale = np.ones((IN_COLS,), np.float32)
    col_scale[OFF_A + 2 * MOBA_WIDTH:OFF_A + 3 * MOBA_WIDTH] = DEEPNORM_BETA
    col_scale[OFF_C + 2 * DIL_WIDTH:OFF_C + 3 * DIL_WIDTH] = DEEPNORM_BETA
    col_scale[OFF_B + 2 * W:OFF_B + 3 * W] = DEEPNORM_BETA
    return {
        'x': nrm((BATCH, SEQ, D), 1.0),
        'c': nrm((BATCH, D), 1.0),
        'rel_bias': nrm((REL_BUCKETS, ATTN_HEADS), 0.5),
        'w_in': nrm((L, D, IN_COLS), D ** -0.5) * jnp.asarray(col_scale),
        'p_a': nrm((L, MOBA_WIDTH, D), MOBA_WIDTH ** -0.5),
        'p_b': nrm((L, W, D), W ** -0.5),
        'p_c': nrm((L, DIL_OUT_WIDTH, D), DIL_OUT_WIDTH ** -0.5),
        'w_o': nrm((L, D, D), DEEPNORM_BETA * D ** -0.5),
        'rwkv_mu': unif((L, RWKV_COLS)),
        'rwkv_w0': -6.5 + 5.0 * unif((L, W)) ** 0.85,
        'rwkv_w_up': nrm((L, RWKV_DECAY_LORA, W), 0.5 * RWKV_DECAY_LORA ** -0.5),
        'rwkv_a0': nrm((L, W), 0.1),
        'rwkv_a_up': nrm((L, RWKV_A_LORA, W), 0.5 * RWKV_A_LORA ** -0.5),
        'rwkv_g_up': nrm((L, RWKV_GATE_LORA, W), RWKV_GATE_LORA ** -0.5),
        'rwkv_k_k': 0.85 + nrm((L, W), 0.02),
        'rwkv_k_a': 1.0 + nrm((L, W), 0.02),
        'rwkv_r_k': nrm((L, W), 0.1),
        'rwkv_ln_g': 1.0 + nrm((L, W), 0.02),
        'rwkv_ln_b': nrm((L, W), 0.02),
        'rwkv_v0': 1.0 + nrm((n_res, W), 0.1),
        'rwkv_mv_down': nrm((n_res, W, RWKV_MV_LORA), W ** -0.5),
        'rwkv_mv_up': nrm((n_res, RWKV_MV_LORA, W), 0.5 * RWKV_MV_LORA ** -0.5),
        'w_ada': nrm((L, D, 6 * D), 0.2 * D ** -0.5),
        'b_ada': nrm((L, 6 * D), 0.02),
        'ln1_g': 1.0 + nrm((L, D), 0.02),
        'ln1_b': nrm((L, D), 0.02),
        'ln2_g': 1.0 + nrm((L, D), 0.02),
        'ln2_b': nrm((L, D), 0.02),
        'router_grp_w': nrm((L, D, G), D ** -0.5),
        'router_grp_b': nrm((L, G), 0.01),
        'router_exp_w': nrm((L, D, E), D ** -0.5),
        'router_exp_b': nrm((L, E), 0.01),
        'exp_w_gate': nrm((L, E, D, F), D ** -0.5),
        'exp_w_up': nrm((L, E, D, F), D ** -0.5),
        'exp_w_down': nrm((L, E, F, D), DEEPNORM_BETA * F ** -0.5),
    }


def reference(x, c, rel_bias, w_in, p_a, p_b, p_c, w_o, rwkv_mu, rwkv_w0, rwkv_w_up, rwkv_a0,
              rwkv_a_up, rwkv_g_up, rwkv_k_k, rwkv_k_a, rwkv_r_k, rwkv_ln_g, rwkv_ln_b, rwkv_v0,
              rwkv_mv_down, rwkv_mv_up, w_ada, b_ada, ln1_g, ln1_b, ln2_g, ln2_b, router_grp_w,
              router_grp_b, router_exp_w, router_exp_b, exp_w_gate, exp_w_up, exp_w_down):
    bias_h = rel_bias.T.astype(jnp.float32)
    cond = jax.nn.silu(c)
    v_first = None
    for l in range(DEPTH):
        mod = cond @ w_ada[l] + b_ada[l]
        sh1, sc1, g1, sh2, sc2, g2 = [m[:, None, :] for m in jnp.split(mod, 6, axis=-1)]
        h = x * (1.0 + sc1) + sh1
        v_res = None if l == 0 else (rwkv_v0[l - 1], rwkv_mv_down[l - 1], rwkv_mv_up[l - 1])
        mix, v_first = token_mixer(h, bias_h, v_first, v_res, w_in[l], p_a[l], p_b[l], p_c[l], w_o[l],
                                   rwkv_mu[l], rwkv_w0[l], rwkv_w_up[l], rwkv_a0[l], rwkv_a_up[l],
                                   rwkv_g_up[l], rwkv_k_k[l], rwkv_k_a[l], rwkv_r_k[l],
                                   rwkv_ln_g[l], rwkv_ln_b[l])
        x = layer_norm(DEEPNORM_ALPHA * x + (1.0 + g1) * mix, ln1_g[l], ln1_b[l])
        h = x * (1.0 + sc2) + sh2
        ffn = hier_moe(h, router_grp_w[l], router_grp_b[l], router_exp_w[l], router_exp_b[l],
                       exp_w_gate[l], exp_w_up[l], exp_w_down[l])
        x = layer_norm(DEEPNORM_ALPHA * x + (1.0 + g2) * ffn, ln2_g[l], ln2_b[l])
    return x
```

```python
import numpy as np
from contextlib import ExitStack
import concourse.bass as bass
import concourse.mybir as mybir
from concourse.bass_utils import run_bass_kernel_spmd

F32 = mybir.dt.float32
BF16 = mybir.dt.bfloat16
AF = mybir.ActivationFunctionType
ALU = mybir.AluOpType
AX = mybir.AxisListType

ENGS = ('pe', 'act', 'dve', 'pool', 'sp')
NDMASEM = 24


class Res:
    __slots__ = ('w', 'r', 'name', 'excl')

    def __init__(self, name='', excl=False):
        self.w = None
        self.r = {}
        self.name = name
        self.excl = excl


class Sched:
    def __init__(self, nc):
        self.nc = nc
        self.ops = {e: [] for e in ENGS}
        self.cnt = {e: 0 for e in ENGS}
        self.known = {e: {} for e in ENGS}
        self.dcnt = [0] * NDMASEM
        self.dnext = 0
        self.dlast = [None] * NDMASEM
        self.epoch = 0

    def _need(self, eng, tok, waits, isdma=False):
        if tok is None:
            return
        k, v = tok
        if k[0] != 'd' and k[1] < self.epoch:
            return
        if k[0] != 'd' and k[0] == eng and not isdma:
            return
        if self.known[eng].get(k, 0) >= v:
            return
        self.known[eng][k] = v
        waits.append((k, v))

    def _deps(self, eng, reads, writes, isdma=False):
        waits = []
        for r in reads:
            self._need(eng, r.w, waits, isdma or eng != 'pe')
        for w in writes:
            self._need(eng, w.w, waits, isdma)
            for k, v in w.r.items():
                self._need(eng, (k, v), waits, isdma)
        return waits

    def _commit(self, tok, reads, writes):
        k, v = tok
        for r in reads:
            if r.r.get(k, 0) < v:
                r.r[k] = v
        for w in writes:
            w.w = tok
            w.r = {}

    def op(self, eng, fn, reads=(), writes=()):
        ex = [r for r in reads if r.excl]
        if ex:
            reads = [r for r in reads if not r.excl]
            writes = list(writes) + [r for r in ex if r not in writes]
        waits = self._deps(eng, reads, writes)
        self.cnt[eng] += 1
        tok = ((eng, self.epoch), self.cnt[eng])
        self.ops[eng].append((waits, fn, (eng, self.epoch), 1))
        self._commit(tok, reads, writes)

    def dma(self, q, fn, reads=(), writes=()):
        waits = self._deps(q, reads, writes, True)
        i = self.dnext
        self.dnext = (self.dnext + 1) % NDMASEM
        self._need(q, self.dlast[i], waits)
        self.dcnt[i] += 16
        tok = (('d', i), self.dcnt[i])
        self.dlast[i] = tok
        self.ops[q].append((waits, fn, ('d', i), 16))
        self._commit(tok, reads, writes)

    def barrier(self):
        toks = [((e, self.epoch), self.cnt[e]) for e in ENGS if self.cnt[e] > 0]
        toks += [t for t in self.dlast if t is not None]
        for e in ENGS:
            waits = []
            for t in toks:
                self._need(e, t, waits)
            if waits:
                self.ops[e].append((waits, None, None, 0))

    def new_epoch(self):
        self.barrier()
        self.epoch += 1
        self.cnt = {e: 0 for e in ENGS}

    def emit(self):
        nc = self.nc
        with ExitStack() as st:
            sems = {}
            for ep in range(self.epoch + 1):
                for e in ENGS:
                    sems[(e, ep)] = st.enter_context(nc.semaphore('s_%s_%d' % (e, ep)))
            for i in range(NDMASEM):
                sems[('d', i)] = st.enter_context(nc.semaphore('s_d%d' % i))
            block = st.enter_context(nc.Block())
            self.barrier()

            def runner(e):
                def f(eng):
                    for waits, fn, inc, amt in self.ops[e]:
                        for k, v in waits:
                            eng.wait_ge(sems[k], v)
                        if fn is not None:
                            ins = fn(eng)
                            ins.then_inc(sems[inc], amt)
                return f
            block.tensor(runner('pe'))
            block.scalar(runner('act'))
            block.vector(runner('dve'))
            block.gpsimd(runner('pool'))
            block.sync(runner('sp'))


D = 2048; S = 4096; KC = 16; NTG = 8; TG = 512
IN_COLS = 13312
OFF_A = 0; OFF_C = 2304; OFF_B = 4608; OFF_G = 7168
TVLEN = 3072; HKW = 2944

def t5b(n):
    n = np.maximum(n, 0)
    nf = np.maximum(n, 1).astype(np.float32)
    large = 16 + (np.log(nf / np.float32(16)) / np.float32(np.log(128.0)) * np.float32(16)).astype(np.int32)
    large = np.minimum(large, 31)
    return np.where(n < 16, n, large)

NCONST = int(np.max(np.nonzero(t5b(np.arange(5000)) < 31)[0])) + 1

def host_consts():
    oh = np.zeros((4, 33, TVLEN), np.float32)
    m = np.arange(TVLEN); dist = m - 511
    for tab, (span, dil) in enumerate([(None, 1), (128, 1), (128, 4), (128, 16)]):
        valid = dist >= 0 if span is None else ((dist >= 0) & (dist <= span))
        bk = t5b(np.maximum(dist, 0) * dil)
        oh[tab, bk[valid], m[valid]] = 1.0
        oh[tab, 32, m[~valid]] = 1.0
    mc = np.zeros((3, 16, 16), np.float32)
    for qb in range(16):
        for j in range(16):
            mc[0, qb, j] = 0.0 if j < qb else -1e30
            mc[1, qb, j] = 1.0 if j < qb else 0.0
            mc[2, qb, j] = 1.0 if j == qb else 0.0
    sel = np.zeros((16, 16, 128), np.float32)
    for j in range(16):
        sel[j, j, :] = 1.0
    C = 64
    mus = np.triu(np.ones((C, C), np.float32), 1); mls = np.tril(np.ones((C, C), np.float32), -1); mui = np.triu(np.ones((C, C), np.float32), 0)
    rm = np.stack([np.stack([m] * 4, 0) for m in (-mus, -mls, mui, np.eye(C, dtype=np.float32))], 0)
    rm = rm.transpose(2, 0, 1, 3).reshape(C, 4 * 4 * C).copy()
    return {"rwmask": rm, "ohtab": oh, "negrow": np.full((1, 24), -30000.0, np.float32), "mconst": mc,
            "selc": sel.reshape(16, 2048), "jflip": np.eye(128, dtype=np.float32)[::-1].copy(), "ident": np.eye(128, dtype=np.float32)}


def build(n_layers=1, debug=True, NH_A=12, NH_C=4, DO_F=True, DO_E=True, RW_BLOCKS=None, RW_STAGE=3, DO_X0=True, DO_CD=True, DO_Z=True, DO_G=True, NEXP=64, MOE_GROUPS=None):
    nc = bass.Bass("TRN2", target_bir_lowering=False)
    sc = Sched(nc)
    def din(name, shape, dt=F32):
        return nc.dram_tensor(name, list(shape), dt, kind="ExternalInput").ap()
    def dscr(name, shape, dt):
        return nc.dram_tensor(name, list(shape), dt, kind="ExternalOutput" if debug else "Internal").ap()
    if DO_X0:
        x = din("x", [S, D]); c = din("c", [D])
        w_in = din("w_in", [4, D, IN_COLS]); w_ada = din("w_ada", [4, D, 6 * D]); b_ada = din("b_ada", [4, 6 * D])
    ident_d = din("ident", [128, 128])
    XT = dscr("XT", [D, S], F32); R_XT = [Res() for _ in range(NTG)]
    PROJ = dscr("PROJ", [IN_COLS, S], BF16) if DO_X0 else din("PROJ", [IN_COLS, S], BF16); R_PROJ = Res()
    p_a = din("p_a", [4, 768, D]); p_b = din("p_b", [4, 768, D]); p_c = din("p_c", [4, 256, D]);
    rwkv_v0 = din("rwkv_v0", [3, 768]); rwkv_mv_down = din("rwkv_mv_down", [3, 768, 32]); rwkv_mv_up = din("rwkv_mv_up", [3, 32, 768])
    ln2_g = din("ln2_g", [4, D]); ln2_b = din("ln2_b", [4, D])
    router_grp_w = din("router_grp_w", [4, D, 8]); router_grp_b = din("router_grp_b", [4, 8]); router_exp_w = din("router_exp_w", [4, D, 64]); router_exp_b = din("router_exp_b", [4, 64])
    exp_w_gate = din("exp_w_gate", [4, 64, D, 256]); exp_w_up = din("exp_w_up", [4, 64, D, 256]); exp_w_down = din("exp_w_down", [4, 64, 256, D])
    GW = dscr("GW", [64, S], F32); R_GW = Res(); w_o = din("w_o", [4, D, D])
    ln1_g = din("ln1_g", [4, D]); ln1_b = din("ln1_b", [4, D])
    rwkv_mu = din("rwkv_mu", [4, 2560]); rwkv_w0 = din("rwkv_w0", [4, 768]); rwkv_w_up = din("rwkv_w_up", [4, 64, 768])
    rwkv_a0 = din("rwkv_a0", [4, 768]); rwkv_a_up = din("rwkv_a_up", [4, 64, 768]); rwkv_g_up = din("rwkv_g_up", [4, 128, 768])
    rwkv_k_k = din("rwkv_k_k", [4, 768]); rwkv_k_a = din("rwkv_k_a", [4, 768]); rwkv_r_k = din("rwkv_r_k", [4, 768])
    rwkv_ln_g = din("rwkv_ln_g", [4, 768]); rwkv_ln_b = din("rwkv_ln_b", [4, 768])
    rwmask = din("rwmask", [64, 4 * 4 * 64])
    OB = dscr("OB", [768, S], BF16); R_OB = Res()
    VF = dscr("VF", [768, S], F32); R_VF = Res()
    rel_bias = din("rel_bias", [32, 24]); ohtab = din("ohtab", [4, 33, TVLEN]); negrow = din("negrow", [1, 24])
    mconst = din("mconst", [3, 16, 16]); selc = din("selc", [16, 16 * 128]); jflip = din("jflip", [128, 128])
    TV = dscr("TV", [4 * 24 * TVLEN], BF16); R_TV = Res()
    OA = dscr("OA", [768, S], BF16); R_OA = Res()
    OC = dscr("OC", [256, S], BF16); R_OC = Res()
    Y = nc.dram_tensor("y", [S, D], F32, kind="ExternalOutput").ap(); R_Y = Res()
    uid = [0]
    with ExitStack() as glob:
        def sb(name, shape, dt, st=glob):
            uid[0] += 1
            return st.enter_context(nc.sbuf_tensor("sb%d_%s" % (uid[0], name), list(shape), dt))
        psum = [glob.enter_context(nc.psum_tensor("ps%d" % i, [128, 512], F32)) for i in range(7)]
        R_ps = [Res(excl=True) for _ in range(8)]
        ident = sb("ident", [128, 128], F32); R_ident = Res()
        identb = sb("identb", [128, 128], BF16)
        sc.dma('sp', lambda e: e.dma_start(out=ident[:], in_=ident_d[:, :]), [], [R_ident])
        sc.op('dve', lambda e: e.tensor_copy(out=identb[:], in_=ident[:]), [R_ident], [R_ident])
        condT = sb("condT", [128, KC], F32); R_cond = Res()
        if DO_X0:
            sc.dma('sp', lambda e: e.dma_start(out=condT[:], in_=c.rearrange("(k p) -> p k", p=128), allow_slow_non_contiguous=True), [], [R_cond])
            sc.op('act', lambda e: e.activation(out=condT[:], in_=condT[:], func=AF.Silu), [R_cond], [R_cond])
        modT = sb("modT", [128, 96], F32); R_mod = Res()
        pscnt = [0]
        def nextps():
            i = pscnt[0] % 7; pscnt[0] += 1
            return i
        psT_b = glob.enter_context(nc.psum_tensor("psTb", [128, 1024], BF16));
        MC = sb("MC", [128, 3, 16, 16], F32); SEL = sb("SEL", [16, 16, 128], BF16); JF = sb("JF", [128, 128], BF16)
        ones_b = sb("ones_b", [128, 128], BF16); ones_f = sb("ones_f", [128, 128], F32); CB = sb("CB", [128, 24], F32)
        rb33 = sb("rb33", [33, 24], F32); R_MC = Res()
        sc.dma('sp', lambda e: e.dma_start(out=MC[:].rearrange("p a b c -> p (a b c)"), in_=bass.AP(mconst.tensor, 0, [[0, 128], [1, 768]])), [], [R_MC])
        sc.dma('pool', lambda e: e.dma_start(out=SEL[:].rearrange("p a b -> p (a b)"), in_=selc[:, :]), [], [R_MC])
        sc.dma('pool', lambda e: e.dma_start(out=JF[:], in_=jflip[:, :]), [], [R_MC])
        sc.dma('sp', lambda e: e.dma_start(out=CB[:], in_=bass.AP(rel_bias.tensor, 31 * 24, [[0, 128], [1, 24]])), [], [R_MC])
        sc.dma('sp', lambda e: e.dma_start(out=rb33[0:32, :], in_=rel_bias[:, :]), [], [R_MC])
        sc.dma('sp', lambda e: e.dma_start(out=rb33[32:33, :], in_=negrow[:, :]), [], [R_MC])
        sc.op('dve', lambda e: e.memset(ones_b[:], 1.0), [], [R_MC])
        sc.op('dve', lambda e: e.memset(ones_f[:], 1.0), [], [R_MC])
        with ExitStack() as ph:
            oh = sb("oh", [33, TVLEN], F32, ph); R_oh = Res()
            tvs = sb("tvs", [24, TVLEN], BF16, ph); R_tvs = Res()
            for tab in range(4):
                sc.dma('sp', lambda e, tab=tab: e.dma_start(out=oh[:], in_=ohtab[tab]), [], [R_oh])
                for ch in range(TVLEN // 512):
                    pi = nextps()
                    sc.op('pe', lambda e, pi=pi, ch=ch: e.matmul(psum[pi][0:24, :], lhsT=rb33[:], rhs=oh[:, ch * 512:(ch + 1) * 512], start=True, stop=True), [R_oh, R_MC], [R_ps[pi]])
                    sc.op('act', lambda e, pi=pi, ch=ch: e.mul(out=tvs[:, ch * 512:(ch + 1) * 512], in_=psum[pi][0:24, :], mul=8.0), [R_ps[pi]], [R_tvs])
                sc.dma('sp', lambda e, tab=tab: e.dma_start(out=bass.AP(TV.tensor, tab * 24 * TVLEN, [[TVLEN, 24], [1, TVLEN]]), in_=tvs[:]), [R_tvs], [R_TV])
        sc.barrier()

        with ExitStack() as ph:
          if DO_X0:
            xin = [sb("xin%d" % i, [128, D], F32, ph) for i in range(2)]; R_xin = [Res(), Res()]
            xst = [sb("xst%d" % i, [128, KC, 128], F32, ph) for i in range(2)]; R_xst = [Res(), Res()]
            for t in range(S // 128):
                b = t % 2
                sc.dma('sp', lambda e, t=t, b=b: e.dma_start(out=xin[b][:], in_=x[t * 128:(t + 1) * 128, :]), [], [R_xin[b]])
                for g in range(4):
                    pi = nextps()
                    for j in range(4):
                        dc = g * 4 + j
                        sc.op('pe', lambda e, pi=pi, j=j, dc=dc, b=b: e.transpose(out=psum[pi][:, j * 128:(j + 1) * 128], in_=xin[b][:, dc * 128:(dc + 1) * 128], identity=ident[:]),
                              [R_xin[b], R_ident], [R_ps[pi]])
                    eng = 'act' if g % 2 == 0 else 'dve'
                    if eng == 'act':
                        sc.op('act', lambda e, pi=pi, g=g, b=b: e.copy(out=xst[b][:, g * 4:(g + 1) * 4, :], in_=psum[pi][:].rearrange("p (j t) -> p j t", j=4)), [R_ps[pi]], [R_xst[b]])
                    else:
                        sc.op('dve', lambda e, pi=pi, g=g, b=b: e.tensor_copy(out=xst[b][:, g * 4:(g + 1) * 4, :], in_=psum[pi][:].rearrange("p (j t) -> p j t", j=4)), [R_ps[pi]], [R_xst[b]])
                sc.dma('pool', lambda e, t=t, b=b: e.dma_start(out=XT.rearrange("(k p) s -> p k s", p=128)[:, :, t * 128:(t + 1) * 128], in_=xst[b][:]), [R_xst[b]], [R_XT[t // 4]])
        sc.barrier()

        def ln_feature_major(xy, R_xy, W, lng, lnb, R_ln, tA, R_tA, tB, R_tB, mean, rstd, R_st):
            ps_s = nextps(); ps_q = nextps()
            for dc in range(KC):
                def one(dc):
                    i = dc % 2
                    sc.op('act', lambda e: e.activation(out=tA[i][:, 0:W], in_=xy[:, dc, :], func=AF.Square), [R_xy], [R_tA[i]])
                    sc.op('pe', lambda e: e.matmul(psum[ps_s][:, 0:W], lhsT=ones_f[:], rhs=xy[:, dc, :], start=(dc == 0), stop=(dc == KC - 1)), [R_xy, R_MC], [R_ps[ps_s]])
                    sc.op('pe', lambda e: e.matmul(psum[ps_q][:, 0:W], lhsT=ones_f[:], rhs=tA[i][:, 0:W], start=(dc == 0), stop=(dc == KC - 1)), [R_tA[i], R_MC], [R_ps[ps_q]])
                one(dc)
            sc.op('act', lambda e: e.mul(out=mean[:, 0:W], in_=psum[ps_s][:, 0:W], mul=1.0 / D), [R_ps[ps_s]], [R_st])
            sc.op('act', lambda e: e.mul(out=rstd[:, 0:W], in_=psum[ps_q][:, 0:W], mul=1.0 / D), [R_ps[ps_q]], [R_st])
            sc.op('dve', lambda e: e.tensor_tensor(out=tB[0][:, 0:W], in0=mean[:, 0:W], in1=mean[:, 0:W], op=ALU.mult), [R_st], [R_tB[0]])
            sc.op('dve', lambda e: e.tensor_tensor(out=rstd[:, 0:W], in0=rstd[:, 0:W], in1=tB[0][:, 0:W], op=ALU.subtract), [R_st, R_tB[0]], [R_st])
            sc.op('dve', lambda e: e.tensor_scalar(out=rstd[:, 0:W], in0=rstd[:, 0:W], scalar1=1e-5, scalar2=None, op0=ALU.add), [R_st], [R_st])
            sc.op('act', lambda e: e.activation(out=rstd[:, 0:W], in_=rstd[:, 0:W], func=AF.Sqrt), [R_st], [R_st])
            sc.op('dve', lambda e: e.reciprocal(out=rstd[:, 0:W], in_=rstd[:, 0:W]), [R_st], [R_st])
            for dc in range(KC):
                def two(dc):
                    i = dc % 2
                    sc.op('dve', lambda e: e.tensor_tensor(out=tA[i][:, 0:W], in0=xy[:, dc, :], in1=mean[:, 0:W], op=ALU.subtract), [R_xy, R_st], [R_tA[i]])
                    sc.op('pool', lambda e: e.tensor_tensor(out=tA[i][:, 0:W], in0=tA[i][:, 0:W], in1=rstd[:, 0:W], op=ALU.mult), [R_tA[i], R_st], [R_tA[i]])
                    sc.op('act', lambda e: e.activation(out=xy[:, dc, :], in_=tA[i][:, 0:W], func=AF.Identity, bias=lnb[:, dc:dc + 1], scale=lng[:, dc:dc + 1]), [R_tA[i], R_ln, R_xy], [R_xy])
                two(dc)

        def layer(l):
            with ExitStack() as ph:
              if DO_X0:
                wa = [sb("wa%d" % i, [128, KC, 512], F32, ph) for i in range(2)]; R_wa = [Res(), Res()]
                bT = sb("bT", [128, 96], F32, ph); R_bT = Res()
                with nc.allow_non_contiguous_dma(reason="small"):
                    sc.dma('sp', lambda e: e.dma_start(out=bT[:], in_=b_ada[l].rearrange("(k p) -> p k", p=128), allow_slow_non_contiguous=True), [], [R_bT])
                pi = nextps()
                for cg in range(24):
                    b = cg % 2
                    sc.dma('sp' if cg % 2 == 0 else 'pool', lambda e, cg=cg, b=b: e.dma_start(out=wa[b][:], in_=w_ada[l].rearrange("(k p) n -> p k n", p=128)[:, :, cg * 512:(cg + 1) * 512]), [], [R_wa[b]])
                    for j in range(4):
                        cc = cg * 4 + j
                        for k in range(KC):
                            sc.op('pe', lambda e, cc=cc, k=k, b=b, j=j: e.matmul(psum[pi][:, cc:cc + 1], lhsT=wa[b][:, k, j * 128:(j + 1) * 128], rhs=condT[:, k:k + 1], start=(k == 0), stop=(k == KC - 1)),
                                  [R_wa[b], R_cond], [R_ps[pi]])
                sc.op('dve', lambda e: e.tensor_tensor(out=modT[:], in0=psum[pi][:, 0:96], in1=bT[:], op=ALU.add), [R_ps[pi], R_bT], [R_mod])
            sc.barrier()
            with ExitStack() as ph:
              if DO_X0:
                hT = sb("hT", [128, KC, S], BF16, ph); R_hT = [Res() for _ in range(NTG)]
                sc1p = sb("sc1p", [128, KC], F32, ph); R_sc1p = Res()
                sc.op('dve', lambda e: e.tensor_scalar(out=sc1p[:], in0=modT[:, 16:32], scalar1=1.0, scalar2=None, op0=ALU.add), [R_mod], [R_sc1p])
                xt = [sb("xt%d" % i, [128, 4, TG], F32, ph) for i in range(2)]; R_xt = [Res(), Res()]
                n = 0
                for tg in range(NTG):
                    for q in range(4):
                        b = n % 2; n += 1
                        sc.dma('sp', lambda e, tg=tg, q=q, b=b: e.dma_start(out=xt[b][:], in_=XT.rearrange("(k p) s -> p k s", p=128)[:, q * 4:(q + 1) * 4, tg * TG:(tg + 1) * TG]), [R_XT[tg]], [R_xt[b]])
                        for j in range(4):
                            dc = q * 4 + j
                            sc.op('dve', lambda e, tg=tg, dc=dc, j=j, b=b: e.tensor_scalar(out=hT[:, dc, tg * TG:(tg + 1) * TG], in0=xt[b][:, j, :], scalar1=sc1p[:, dc:dc + 1], scalar2=modT[:, dc:dc + 1], op0=ALU.mult, op1=ALU.add),
                                  [R_xt[b], R_sc1p, R_mod], [R_hT[tg]])
                wt = [sb("wt%d" % i, [128, KC, 256], BF16, ph) for i in range(2)]; R_wt = [Res(), Res()]
                ost = [sb("ost%d" % i, [128, TG], BF16, ph) for i in range(4)]; R_ost = [Res() for _ in range(4)]
                no = 0
                for cp in range(IN_COLS // 256):
                    b = cp % 2
                    sc.dma('pool', lambda e, cp=cp, b=b: e.dma_start(out=wt[b][:], in_=w_in[l].rearrange("(k p) n -> p k n", p=128)[:, :, cp * 256:(cp + 1) * 256]), [], [R_wt[b]])
                    for j in range(2):
                        col0 = cp * 256 + j * 128
                        isgate = col0 >= OFF_G
                        for tg in range(NTG):
                            pi = nextps()
                            for k in range(KC):
                                sc.op('pe', lambda e, pi=pi, k=k, b=b, j=j, tg=tg: e.matmul(psum[pi][:], lhsT=wt[b][:, k, j * 128:(j + 1) * 128], rhs=hT[:, k, tg * TG:(tg + 1) * TG], start=(k == 0), stop=(k == KC - 1)),
                                      [R_wt[b], R_hT[tg]], [R_ps[pi]])
                            ob = no % 4; no += 1
                            if isgate:
                                sc.op('act', lambda e, pi=pi, ob=ob: e.activation(out=ost[ob][:], in_=psum[pi][:], func=AF.Sigmoid), [R_ps[pi]], [R_ost[ob]])
                            elif no % 2 == 0:
                                sc.op('act', lambda e, pi=pi, ob=ob: e.copy(out=ost[ob][:], in_=psum[pi][:]), [R_ps[pi]], [R_ost[ob]])
                            else:
                                sc.op('dve', lambda e, pi=pi, ob=ob: e.tensor_copy(out=ost[ob][:], in_=psum[pi][:]), [R_ps[pi]], [R_ost[ob]])
                            sc.dma('sp', lambda e, col0=col0, tg=tg, ob=ob: e.dma_start(out=PROJ[col0:col0 + 128, tg * TG:(tg + 1) * TG], in_=ost[ob][:]), [R_ost[ob]], [R_PROJ])
            sc.barrier()
            with ExitStack() as ph:
              if DO_CD:
                QT = [sb("QT%d" % i, [64, S], BF16, ph) for i in range(2)]
                KT = [sb("KT%d" % i, [64, S], BF16, ph) for i in range(2)]
                VT = [sb("VT%d" % i, [64, S], BF16, ph) for i in range(2)]
                Vt = [sb("Vt%d" % i, [128, 32, 64], BF16, ph) for i in range(2)]
                HK = [sb("HK%d" % i, [128, HKW], BF16, ph) for i in range(2)]
                R_hd = [Res(), Res()]; R_Vt = [Res(), Res()]; R_HK = [Res(), Res()]
                kmf = sb("kmf", [64, 16], F32, ph); kmhi = sb("kmhi", [64, 16], BF16, ph); kmlo = sb("kmlo", [64, 16], BF16, ph); R_km = Res()
                maskT = [sb("maskT%d" % i, [16, 512], BF16, ph) for i in range(2)]; R_maskT = [Res(), Res()]
                gm = sb("gm", [128, 16], F32, ph); top8 = sb("top8", [128, 8], F32, ph); R_gm = Res()
                PT = [sb("PT%d" % i, [128, 512], BF16, ph) for i in range(3)]; R_PT = [Res() for _ in range(3)]
                rl = sb("rl", [64, 512], F32, ph); R_rl = Res()
                ostg = [sb("ostg%d" % i, [64, 512], BF16, ph) for i in range(2)]; R_ostg = [Res(), Res()]
                npt = [0]; nsb = [0]

                def load_head(b, qrow, krow, vrow, tab, hidx):
                    for (T_, row) in ((QT, qrow), (KT, krow), (VT, vrow)):
                        sc.dma('sp', lambda e, T_=T_, row=row: e.dma_start(out=T_[b][:], in_=PROJ[row:row + 64, :]), [R_PROJ], [R_hd[b]])
                    off = (tab * 24 + hidx) * TVLEN
                    sc.dma('sp', lambda e: e.dma_start(out=HK[b][:], in_=bass.AP(TV.tensor, off, [[1, 128], [1, HKW]])), [R_TV], [R_HK[b]])

                def make_V(b, src, R_src):
                    for g4 in range(8):
                        for j in range(4):
                            t = g4 * 4 + j
                            sc.op('pe', lambda e, t=t, j=j: e.transpose(out=psT_b[:, j * 64:(j + 1) * 64], in_=src[:, t * 128:(t + 1) * 128], identity=identb[0:64, 0:64]), [R_src, R_ident], [R_ps[7]])
                        sc.op('dve', lambda e, g4=g4: e.tensor_copy(out=Vt[b][:, g4 * 4:(g4 + 1) * 4, :], in_=psT_b[:, 0:256].rearrange("p (j d) -> p j d", j=4)), [R_ps[7]], [R_Vt[b]])

                def moba_gate(b, mb, qt, t):
                    qb = qt // 2
                    sc.op('pe', lambda e: e.matmul(psum[6][:, 0:16], lhsT=QT[b][:, qt * 128:(qt + 1) * 128], rhs=kmhi[:], start=True, stop=False), [R_hd[b], R_km], [R_ps[6]])
                    sc.op('pe', lambda e: e.matmul(psum[6][:, 0:16], lhsT=QT[b][:, qt * 128:(qt + 1) * 128], rhs=kmlo[:], start=False, stop=True), [R_hd[b], R_km], [R_ps[6]])
                    sc.op('dve', lambda e: e.tensor_tensor(out=gm[:], in0=psum[6][:, 0:16], in1=MC[:, 0, qb, :], op=ALU.add), [R_ps[6], R_MC], [R_gm])
                    sc.op('dve', lambda e: e.max(out=top8[:], in_=gm[:]), [R_gm], [R_gm])
                    sc.op('dve', lambda e: e.tensor_scalar(out=gm[:], in0=gm[:], scalar1=top8[:, 2:3], scalar2=None, op0=ALU.is_ge), [R_gm], [R_gm])
                    sc.op('dve', lambda e: e.tensor_tensor(out=gm[:], in0=gm[:], in1=MC[:, 1, qb, :], op=ALU.mult), [R_gm, R_MC], [R_gm])
                    sc.op('dve', lambda e: e.tensor_tensor(out=gm[:], in0=gm[:], in1=MC[:, 2, qb, :], op=ALU.add), [R_gm, R_MC], [R_gm])
                    sc.op('dve', lambda e: e.tensor_scalar(out=gm[:], in0=gm[:], scalar1=-1.0, scalar2=240000.0, op0=ALU.add, op1=ALU.mult), [R_gm], [R_gm])
                    sc.op('pe', lambda e: e.transpose(out=psum[6][0:16, 128:256], in_=gm[:], identity=ident[:]), [R_gm, R_ident], [R_ps[6]])
                    sc.op('act', lambda e: e.copy(out=maskT[mb][:, t * 128:(t + 1) * 128], in_=psum[6][0:16, 128:256]), [R_ps[6]], [R_maskT[mb]])

                def moba_tile(h, b, mb, QG, kt, nk, pO, pL):
                    D0 = QG * 512 - kt * 128
                    far = (D0 - 127) >= NCONST
                    pS = npt[0] % 3; pb = npt[0] % 3; npt[0] += 1
                    sc.op('pe', lambda e: e.matmul(psum[pS][:], lhsT=KT[b][:, kt * 128:(kt + 1) * 128], rhs=QT[b][:, QG * 512:(QG + 1) * 512], start=True, stop=False), [R_hd[b]], [R_ps[pS]])
                    sc.op('pe', lambda e: e.matmul(psum[pS][:], lhsT=SEL[:, kt // 2, :], rhs=maskT[mb][:], start=False, stop=far), [R_maskT[mb], R_MC], [R_ps[pS]])
                    if not far:
                        sc.op('pe', lambda e: e.matmul(psum[pS][:], lhsT=JF[:], rhs=HK[b][:, D0 + 384:D0 + 384 + 512], start=False, stop=True), [R_HK[b], R_MC], [R_ps[pS]])
                        sc.op('act', lambda e: e.activation(out=PT[pb][:], in_=psum[pS][:], func=AF.Exp, scale=0.125), [R_ps[pS]], [R_PT[pb]])
                    else:
                        sc.op('act', lambda e: e.activation(out=PT[pb][:], in_=psum[pS][:], func=AF.Exp, scale=0.125, bias=CB[:, h:h + 1]), [R_ps[pS], R_MC], [R_PT[pb]])
                    sc.op('pe', lambda e: e.matmul(psum[pO][0:64, :], lhsT=Vt[b][:, kt, :], rhs=PT[pb][:], start=(kt == 0), stop=(kt == nk - 1)), [R_Vt[b], R_PT[pb]], [R_ps[pO]])
                    sc.op('pe', lambda e: e.matmul(psum[pL][0:64, :], lhsT=ones_b[:, 0:64], rhs=PT[pb][:], start=(kt == 0), stop=(kt == nk - 1)), [R_PT[pb], R_MC], [R_ps[pL]])

                def moba_qg(h, b, QG):
                    mb = QG % 2
                    for t in range(4):
                        moba_gate(b, mb, QG * 4 + t, t)
                    pO = 3 + (QG % 2); pL = 5
                    nk = 4 * QG + 4
                    for kt in range(nk):
                        moba_tile(h, b, mb, QG, kt, nk, pO, pL)
                    ob = nsb[0] % 2; nsb[0] += 1
                    sc.op('dve', lambda e: e.reciprocal(out=rl[:], in_=psum[pL][0:64, :]), [R_ps[pL]], [R_rl])
                    sc.op('dve', lambda e: e.tensor_tensor(out=ostg[ob][:], in0=psum[pO][0:64, :], in1=rl[:], op=ALU.mult), [R_ps[pO], R_rl], [R_ostg[ob]])
                    sc.dma('sp', lambda e: e.dma_start(out=OA[h * 64:(h + 1) * 64, QG * 512:(QG + 1) * 512], in_=ostg[ob][:]), [R_ostg[ob]], [R_OA])

                def moba_head(h, b):
                    load_head(b, OFF_A + h * 64, OFF_A + 768 + h * 64, OFF_A + 1536 + h * 64, 0, h)
                    make_V(b, VT[b], R_hd[b])
                    sc.op('dve', lambda e: e.tensor_reduce(out=kmf[:], in_=KT[b][:].rearrange("p (j k) -> p j k", k=256), axis=AX.X, op=ALU.add), [R_hd[b]], [R_km])
                    sc.op('dve', lambda e: e.tensor_scalar(out=kmf[:], in0=kmf[:], scalar1=1.0 / 256, scalar2=None, op0=ALU.mult), [R_km], [R_km])
                    sc.op('dve', lambda e: e.tensor_copy(out=kmhi[:], in_=kmf[:]), [R_km], [R_km])
                    sc.op('dve', lambda e: e.tensor_tensor(out=kmlo[:], in0=kmf[:], in1=kmhi[:], op=ALU.subtract), [R_km], [R_km])
                    for QG in range(8):
                        moba_qg(h, b, QG)

                for h in range(NH_A):
                    moba_head(h, h % 2)

                QP = sb("QP", [64, S], BF16, ph); KP = sb("KP", [64, S], BF16, ph); VP = sb("VP", [64, S], BF16, ph); R_perm = Res()
                Og = sb("Og", [64, S], F32, ph); Lg = sb("Lg", [64, S], F32, ph); R_OL = Res()
                Os = sb("Os", [64, S], F32, ph); Ls = sb("Ls", [64, S], F32, ph); R_sum = Res()
                obig = sb("obig", [64, S], BF16, ph)

                def dil_qtile(b, qt, tps, g4, jj, srcQ, srcK, R_src, pO, pL):
                    kts = ([qt - 1] if qt % tps != 0 else []) + [qt]
                    for i, kt in enumerate(kts):
                        D0 = (qt - kt) * 128
                        pS = npt[0] % 3; pb = npt[0] % 3; npt[0] += 1
                        sc.op('pe', lambda e, kt=kt, pS=pS: e.matmul(psum[pS][:, 0:128], lhsT=srcK[:, kt * 128:(kt + 1) * 128], rhs=srcQ[:, qt * 128:(qt + 1) * 128], start=True, stop=False), [R_src], [R_ps[pS]])
                        sc.op('pe', lambda e, pS=pS, D0=D0: e.matmul(psum[pS][:, 0:128], lhsT=JF[:], rhs=HK[b][:, D0 + 384:D0 + 384 + 128], start=False, stop=True), [R_HK[b], R_MC], [R_ps[pS]])
                        sc.op('act', lambda e, pS=pS, pb=pb: e.activation(out=PT[pb][:, 0:128], in_=psum[pS][:, 0:128], func=AF.Exp, scale=0.125), [R_ps[pS]], [R_PT[pb]])
                        first = (i == 0) and (jj == 0)
                        sc.op('pe', lambda e, kt=kt, pb=pb, first=first: e.matmul(psum[pO][0:64, jj * 128:(jj + 1) * 128], lhsT=Vt[b][:, kt, :], rhs=PT[pb][:, 0:128], start=first, stop=(kt == qt)), [R_Vt[b], R_PT[pb]], [R_ps[pO]])
                        sc.op('pe', lambda e, kt=kt, pb=pb, first=first: e.matmul(psum[pL][0:64, jj * 128:(jj + 1) * 128], lhsT=ones_b[:, 0:64], rhs=PT[pb][:, 0:128], start=first, stop=(kt == qt)), [R_PT[pb], R_MC], [R_ps[pL]])

                def dil_head(g, j, b):
                    dil = (1, 4, 16)[g]
                    hc = g * 4 + j
                    load_head(b, OFF_C + hc * 64, OFF_C + 768 + hc * 64, OFF_C + 1536 + hc * 64, 1 + g, 12 + hc)
                    if dil > 1:
                        for (src, dst, eng) in ((QT[b], QP, 'pool'), (KT[b], KP, 'pool'), (VT[b], VP, 'act')):
                            if eng == 'pool':
                                sc.op('pool', lambda e, src=src, dst=dst: e.tensor_copy(out=dst[:].rearrange("p (r n) -> p r n", r=dil), in_=src[:].rearrange("p (n r) -> p r n", r=dil)), [R_hd[b]], [R_perm])
                            else:
                                sc.op('act', lambda e, src=src, dst=dst: e.copy(out=dst[:].rearrange("p (r n) -> p r n", r=dil), in_=src[:].rearrange("p (n r) -> p r n", r=dil)), [R_hd[b]], [R_perm])
                        sQ, sK, sV, R_src = QP, KP, VP, R_perm
                    else:
                        sQ, sK, sV, R_src = QT[b], KT[b], VT[b], R_hd[b]
                    make_V(b, sV, R_src)
                    tps = (S // dil) // 128
                    for g4 in range(8):
                        pO = 3 + (g4 % 2); pL = 5 + (g4 % 2)
                        for jj in range(4):
                            dil_qtile(b, g4 * 4 + jj, tps, g4, jj, sQ, sK, R_src, pO, pL)
                        sc.op('act', lambda e, g4=g4, pO=pO: e.copy(out=Og[:, g4 * 512:(g4 + 1) * 512], in_=psum[pO][0:64, :]), [R_ps[pO]], [R_OL])
                        sc.op('dve', lambda e, g4=g4, pL=pL: e.tensor_copy(out=Lg[:, g4 * 512:(g4 + 1) * 512], in_=psum[pL][0:64, :]), [R_ps[pL]], [R_OL])
                    if g == 0:
                        sc.op('dve', lambda e: e.tensor_copy(out=Os[:], in_=Og[:]), [R_OL], [R_sum])
                        sc.op('pool', lambda e: e.tensor_copy(out=Ls[:], in_=Lg[:]), [R_OL], [R_sum])
                    else:
                        sc.op('dve', lambda e: e.tensor_tensor(out=Os[:].rearrange("p (n r) -> p r n", r=dil), in0=Os[:].rearrange("p (n r) -> p r n", r=dil), in1=Og[:].rearrange("p (r n) -> p r n", r=dil), op=ALU.add), [R_OL], [R_sum])
                        sc.op('pool', lambda e: e.tensor_tensor(out=Ls[:].rearrange("p (n r) -> p r n", r=dil), in0=Ls[:].rearrange("p (n r) -> p r n", r=dil), in1=Lg[:].rearrange("p (r n) -> p r n", r=dil), op=ALU.add), [R_OL], [R_sum])

                def dil_slot(j):
                    for g in range(3):
                        dil_head(g, j, (j * 3 + g) % 2)
                    sc.op('dve', lambda e: e.reciprocal(out=Ls[:], in_=Ls[:]), [R_sum], [R_sum])
                    sc.op('dve', lambda e: e.tensor_tensor(out=obig[:], in0=Os[:], in1=Ls[:], op=ALU.mult), [R_sum], [R_sum])
                    sc.dma('sp', lambda e: e.dma_start(out=OC[j * 64:(j + 1) * 64, :], in_=obig[:]), [R_sum], [R_OC])

                for j in range(NH_C):
                    dil_slot(j)
            sc.barrier()
            if DO_E:
                RB = 256; NCH = 4; NBLK = S // RB
                with ExitStack() as ph:
                    def prm(name, src, n=12):
                        t = sb(name, [64, n], F32, ph)
                        sc.dma('sp', lambda e: e.dma_start(out=t[:], in_=src.rearrange("(h d) -> d h", d=64), allow_slow_non_contiguous=True), [], [R_prm])
                        return t
                    R_prm = Res()
                    mu_r = prm("mu_r", rwkv_mu[l, 0:768]); mu_k = prm("mu_k", rwkv_mu[l, 768:1536]); mu_v = prm("mu_v", rwkv_mu[l, 1536:2304])
                    mu_w = prm("mu_w", rwkv_mu[l, 2304:2368], 1); mu_a = prm("mu_a", rwkv_mu[l, 2368:2432], 1)
                    mu_g = sb("mu_g", [128, 1], F32, ph)
                    sc.dma('sp', lambda e: e.dma_start(out=mu_g[:], in_=rwkv_mu[l, 2432:2560].rearrange("(h d) -> d h", d=128), allow_slow_non_contiguous=True), [], [R_prm])
                    w0 = prm("w0", rwkv_w0[l]); a0 = prm("a0", rwkv_a0[l]); k_k = prm("k_k", rwkv_k_k[l]); k_a = prm("k_a", rwkv_k_a[l])
                    r_k = prm("r_k", rwkv_r_k[l]); ln_g = prm("ln_g", rwkv_ln_g[l]); ln_b = prm("ln_b", rwkv_ln_b[l])
                    omka = sb("omka", [64, 12], F32, ph)
                    sc.op('dve', lambda e: e.tensor_scalar(out=omka[:], in0=k_a[:], scalar1=-1.0, scalar2=1.0, op0=ALU.mult, op1=ALU.add), [R_prm], [R_prm])
                    wup = sb("wup", [64, 768], BF16, ph); aup = sb("aup", [64, 768], BF16, ph); gup = sb("gup", [128, 768], BF16, ph)
                    sc.dma('pool', lambda e: e.dma_start(out=wup[:], in_=rwkv_w_up[l]), [], [R_prm])
                    sc.dma('pool', lambda e: e.dma_start(out=aup[:], in_=rwkv_a_up[l]), [], [R_prm])
                    sc.dma('pool', lambda e: e.dma_start(out=gup[:], in_=rwkv_g_up[l]), [], [R_prm])
                    RM = sb("RM", [64, 4, 4, 64], F32, ph); I4 = sb("I4", [64, 4, 64], BF16, ph)
                    sc.dma('sp', lambda e: e.dma_start(out=RM[:].rearrange("p a b c -> p (a b c)"), in_=rwmask[:, :]), [], [R_prm])
                    sc.op('dve', lambda e: e.tensor_copy(out=I4[:], in_=RM[:, 3, :, :]), [R_prm], [R_prm])
                    QtT = sb("QtT", [64, 12, RB], BF16, ph); RtT = sb("RtT", [64, 12, RB], BF16, ph); KhT = sb("KhT", [64, 12, RB], BF16, ph)
                    BhT = sb("BhT", [64, 12, RB], BF16, ph); KbT = sb("KbT", [64, 12, RB], BF16, ph); BbT = sb("BbT", [64, 12, RB], BF16, ph)
                    VTb = sb("VTb", [64, 12, RB], BF16, ph); Gg = sb("Gg", [64, 12, RB], BF16, ph); BON = sb("BON", [64, 12, RB], BF16, ph)
                    GC = sb("GC", [64, 12, NCH], F32, ph); yT = sb("yT", [64, 12, RB], F32, ph)
                    R_opsH = [Res() for _ in range(12)]; R_yT = [Res() for _ in range(3)]
                    Pf = sb("Pf", [64, 12, 64], F32, ph); Pb = sb("Pb", [64, 12, 64], BF16, ph); R_P = [Res() for _ in range(3)]
                    sc.op('dve', lambda e: e.memset(Pf[:], 0.0), [], R_P)
                    sc.op('dve', lambda e: e.memset(Pb[:], 0.0), [], R_P)
                    zr = [sb("zr%d" % i, [64, 12, RB + 1], BF16, ph) for i in range(3)]; R_z = [Res() for _ in range(3)]
                    zlo = sb("zlo", [128, 2, RB + 1], BF16, ph); zgl = sb("zgl", [128, RB + 1], BF16, ph); R_zlo = Res()
                    wl = sb("wl", [64, RB], BF16, ph); al = sb("al", [64, RB], BF16, ph); gl = sb("gl", [128, RB], BF16, ph); R_lo = Res()
                    T = [sb("T%d" % i, [64, RB], F32, ph) for i in range(12)]; R_T = [Res() for _ in range(12)]
                    MZ = sb("MZ", [64, 4, 192], BF16, ph); R_MZ = Res()
                    SC = sb("SC", [64, 3, 4, 64], BF16, ph); R_SC = Res()
                    TM = sb("TM", [64, 4, 192], BF16, ph); R_TM = Res()
                    Xs = sb("Xs", [64, 4, 64], BF16, ph); Un = sb("Un", [64, 4, 64], BF16, ph); R_X = Res(); R_U = Res()
                    o1 = ones_f[0:64, 0:64]
                    pA, pB, pC, pD, pE, pF, pG = range(7)
                    ppre = [0]

                    def pre_ps():
                        i = ppre[0] % 2; ppre[0] += 1
                        return (pC, pE)[i]

                    def shift(eng, out, zt, hsl, mu_ap, tmp, R_src, R_tmp, R_out):
                        sc.op('dve', lambda e: e.tensor_tensor(out=tmp, in0=zt[hsl + (slice(0, RB),)], in1=zt[hsl + (slice(1, RB + 1),)], op=ALU.subtract), [R_src], [R_tmp])
                        sc.op(eng, lambda e: e.scalar_tensor_tensor(out=out, in0=tmp, scalar=mu_ap, in1=zt[hsl + (slice(1, RB + 1),)], op0=ALU.mult, op1=ALU.add), [R_src, R_tmp, R_prm], [R_out])

                    def load_block(blk):
                        t0 = blk * RB
                        for i, off in enumerate((OFF_B, OFF_B + 768, OFF_B + 1536)):
                            src = PROJ[off:off + 768, :].rearrange("(h d) s -> d h s", d=64)
                            if blk == 0:
                                sc.op('pool', lambda e, i=i: e.memset(zr[i][:, :, 0:1], 0.0), [], [R_z[i]])
                                sc.dma('sp', lambda e, i=i, src=src: e.dma_start(out=zr[i][:, :, 1:RB + 1], in_=src[:, :, 0:RB]), [R_PROJ], [R_z[i]])
                            else:
                                sc.dma('sp', lambda e, i=i, src=src: e.dma_start(out=zr[i][:], in_=src[:, :, t0 - 1:t0 + RB]), [R_PROJ], [R_z[i]])
                        lo0 = OFF_B + 2304
                        if blk == 0:
                            sc.op('pool', lambda e: e.memset(zlo[:, :, 0:1], 0.0), [], [R_zlo])
                            sc.op('pool', lambda e: e.memset(zgl[:, 0:1], 0.0), [], [R_zlo])
                            sc.dma('sp', lambda e: e.dma_start(out=zlo[0:64, 0, 1:RB + 1], in_=PROJ[lo0:lo0 + 64, 0:RB]), [R_PROJ], [R_zlo])
                            sc.dma('sp', lambda e: e.dma_start(out=zlo[0:64, 1, 1:RB + 1], in_=PROJ[lo0 + 64:lo0 + 128, 0:RB]), [R_PROJ], [R_zlo])
                            sc.dma('sp', lambda e: e.dma_start(out=zgl[:, 1:RB + 1], in_=PROJ[lo0 + 128:lo0 + 256, 0:RB]), [R_PROJ], [R_zlo])
                        else:
                            sc.dma('sp', lambda e: e.dma_start(out=zlo[0:64, 0, :], in_=PROJ[lo0:lo0 + 64, t0 - 1:t0 + RB]), [R_PROJ], [R_zlo])
                            sc.dma('sp', lambda e: e.dma_start(out=zlo[0:64, 1, :], in_=PROJ[lo0 + 64:lo0 + 128, t0 - 1:t0 + RB]), [R_PROJ], [R_zlo])
                            sc.dma('sp', lambda e: e.dma_start(out=zgl[:], in_=PROJ[lo0 + 128:lo0 + 256, t0 - 1:t0 + RB]), [R_PROJ], [R_zlo])
                        shift('dve', T[0][:], zlo, (slice(0, 64), 0), mu_w[:, 0:1], T[1][:], R_zlo, R_T[1], R_T[0])
                        sc.op('act', lambda e: e.activation(out=wl[:], in_=T[0][:], func=AF.Tanh), [R_T[0]], [R_lo])
                        shift('dve', al[:], zlo, (slice(0, 64), 1), mu_a[:, 0:1], T[1][:], R_zlo, R_T[1], R_lo)
                        sc.op('dve', lambda e: e.tensor_tensor(out=T128[:], in0=zgl[:, 0:RB], in1=zgl[:, 1:RB + 1], op=ALU.subtract), [R_zlo], [R_T128])
                        sc.op('dve', lambda e: e.scalar_tensor_tensor(out=T128[:], in0=T128[:], scalar=mu_g[:, 0:1], in1=zgl[:, 1:RB + 1], op0=ALU.mult, op1=ALU.add), [R_zlo, R_T128, R_prm], [R_T128])
                        sc.op('act', lambda e: e.activation(out=gl[:], in_=T128[:], func=AF.Sigmoid), [R_T128], [R_lo])

                    T128 = sb("T128", [128, RB], F32, ph); R_T128 = Res()

                    def pre_head(blk, h):
                        t0 = blk * RB
                        hs = slice(h * 64, (h + 1) * 64)
                        tr, tk, tv, ta, tld, tlg, tkap, tkt, tb, tx, ty, tz = T
                        Rr, Rk, Rv, Ra, Rld, Rlg, Rkap, Rkt, Rb, Rx, Ry, Rz = R_T
                        hsl = (slice(0, 64), h)
                        shift('dve', tr[:], zr[0], hsl, mu_r[:, h:h + 1], tx[:], R_z[0], Rx, Rr)
                        shift('dve', tk[:], zr[1], hsl, mu_k[:, h:h + 1], ty[:], R_z[1], Ry, Rk)
                        sc.op('pool', lambda e: e.tensor_copy(out=tv[:], in_=V32[:, h, :]), [R_V32], [Rv])
                        p1 = pre_ps()
                        sc.op('pe', lambda e: e.matmul(psum[p1][0:64, 256:512], lhsT=wup[:, hs], rhs=wl[:], start=True, stop=True), [R_lo, R_prm], [R_ps[p1]])
                        sc.op('act', lambda e: e.activation(out=tld[:], in_=psum[p1][0:64, 256:512], func=AF.Sigmoid, bias=w0[:, h:h + 1]), [R_ps[p1], R_prm], [Rld])
                        sc.op('dve', lambda e: e.tensor_scalar(out=tld[:], in0=tld[:], scalar1=-0.6065306597126334, scalar2=None, op0=ALU.mult), [Rld], [Rld])
                        p2 = pre_ps()
                        sc.op('pe', lambda e: e.matmul(psum[p2][0:64, 256:512], lhsT=aup[:, hs], rhs=al[:], start=True, stop=True), [R_lo, R_prm], [R_ps[p2]])
                        sc.op('act', lambda e: e.activation(out=ta[:], in_=psum[p2][0:64, 256:512], func=AF.Sigmoid, bias=a0[:, h:h + 1]), [R_ps[p2], R_prm], [Ra])
                        p3 = pre_ps()
                        sc.op('pe', lambda e: e.matmul(psum[p3][0:64, 256:512], lhsT=gup[:, hs], rhs=gl[:], start=True, stop=True), [R_lo, R_prm], [R_ps[p3]])
                        sc.op('act', lambda e: e.copy(out=Gg[:, h, :], in_=psum[p3][0:64, 256:512]), [R_ps[p3]], [R_opsH[h]])
                        sc.op('dve', lambda e: e.tensor_scalar(out=tkap[:], in0=tk[:], scalar1=k_k[:, h:h + 1], scalar2=None, op0=ALU.mult), [Rk, R_prm], [Rkap])
                        sc.op('act', lambda e: e.activation(out=tx[:], in_=tkap[:], func=AF.Square), [Rkap], [Rx])
                        p4 = pre_ps()
                        sc.op('pe', lambda e: e.matmul(psum[p4][0:64, 256:512], lhsT=o1, rhs=tx[:], start=True, stop=True), [Rx, R_MC], [R_ps[p4]])
                        sc.op('act', lambda e: e.activation(out=tx[:], in_=psum[p4][0:64, 256:512], func=AF.Sqrt), [R_ps[p4]], [Rx])
                        sc.op('dve', lambda e: e.tensor_scalar(out=tx[:], in0=tx[:], scalar1=1e-12, scalar2=None, op0=ALU.max), [Rx], [Rx])
                        sc.op('dve', lambda e: e.reciprocal(out=tx[:], in_=tx[:]), [Rx], [Rx])
                        sc.op('dve', lambda e: e.tensor_tensor(out=tkap[:], in0=tkap[:], in1=tx[:], op=ALU.mult), [Rkap, Rx], [Rkap])
                        sc.op('dve', lambda e: e.tensor_scalar(out=ty[:], in0=ta[:], scalar1=k_a[:, h:h + 1], scalar2=omka[:, h:h + 1], op0=ALU.mult, op1=ALU.add), [Ra, R_prm], [Ry])
                        sc.op('pool', lambda e: e.tensor_tensor(out=tkt[:], in0=tk[:], in1=ty[:], op=ALU.mult), [Rk, Ry], [Rkt])
                        sc.op('pool', lambda e: e.tensor_tensor(out=tb[:], in0=tkap[:], in1=ta[:], op=ALU.mult), [Rkap, Ra], [Rb])
                        sc.op('dve', lambda e: e.tensor_tensor(out=tz[:], in0=tr[:], in1=tkt[:], op=ALU.mult), [Rr, Rkt], [Rz])
                        sc.op('dve', lambda e: e.tensor_scalar(out=tz[:], in0=tz[:], scalar1=r_k[:, h:h + 1], scalar2=None, op0=ALU.mult), [Rz, R_prm], [Rz])
                        p5 = pre_ps()
                        sc.op('pe', lambda e: e.matmul(psum[p5][0:64, 256:512], lhsT=o1, rhs=tz[:], start=True, stop=True), [Rz, R_MC], [R_ps[p5]])
                        sc.op('dve', lambda e: e.tensor_tensor(out=BON[:, h, :], in0=psum[p5][0:64, 256:512], in1=tv[:], op=ALU.mult), [R_ps[p5], Rv], [R_opsH[h]])
                        for c in range(NCH):
                            cs = slice(c * 64, (c + 1) * 64)
                            sc.op('dve', lambda e, cs=cs: e.tensor_tensor_scan(out=tlg[:, cs], data0=ones_f[0:64, 0:64], data1=tld[:, cs], initial=0.0, op0=ALU.mult, op1=ALU.add), [Rld, R_MC], [Rlg])
                        sc.op('act', lambda e: e.activation(out=tx[:], in_=tlg[:], func=AF.Exp), [Rlg], [Rx])
                        sc.op('act', lambda e: e.activation(out=ty[:], in_=tlg[:], func=AF.Exp, scale=-1.0), [Rlg], [Ry])
                        sc.op('dve', lambda e: e.tensor_tensor(out=tz[:], in0=tlg[:], in1=tld[:], op=ALU.subtract), [Rlg, Rld], [Rz])
                        sc.op('act', lambda e: e.activation(out=tz[:], in_=tz[:], func=AF.Exp), [Rz], [Rz])
                        sc.op('dve', lambda e: e.tensor_copy(out=GC[:, h, :], in_=tx[:].rearrange("p (c t) -> p c t", t=64)[:, :, 63]), [Rx], [R_opsH[h]])
                        sc.op('dve', lambda e: e.tensor_tensor(out=QtT[:, h, :], in0=tkap[:], in1=tz[:], op=ALU.mult), [Rkap, Rz], [R_opsH[h]])
                        sc.op('pool', lambda e: e.tensor_tensor(out=RtT[:, h, :], in0=tr[:], in1=tx[:], op=ALU.mult), [Rr, Rx], [R_opsH[h]])
                        sc.op('dve', lambda e: e.tensor_tensor(out=KhT[:, h, :], in0=tkt[:], in1=ty[:], op=ALU.mult), [Rkt, Ry], [R_opsH[h]])
                        sc.op('pool', lambda e: e.tensor_tensor(out=BhT[:, h, :], in0=tb[:], in1=ty[:], op=ALU.mult), [Rb, Ry], [R_opsH[h]])
                        for c in range(NCH):
                            cs = slice(c * 64, (c + 1) * 64)
                            sc.op('dve', lambda e, cs=cs, c=c: e.tensor_scalar(out=ty[:, cs], in0=ty[:, cs], scalar1=tx[:, c * 64 + 63:c * 64 + 64], scalar2=None, op0=ALU.mult), [Ry, Rx], [Ry])
                        sc.op('dve', lambda e: e.tensor_tensor(out=KbT[:, h, :], in0=tkt[:], in1=ty[:], op=ALU.mult), [Rkt, Ry], [R_opsH[h]])
                        sc.op('pool', lambda e: e.tensor_tensor(out=BbT[:, h, :], in0=tb[:], in1=ty[:], op=ALU.mult), [Rb, Ry], [R_opsH[h]])
                        sc.op('act', lambda e: e.copy(out=VTb[:, h, :], in_=tv[:]), [Rv], [R_opsH[h]])

                    def chunk_group(blk, c, hg):
                        cs = slice(c * 64, (c + 1) * 64)
                        hh = [hg * 4 + q for q in range(4)]
                        Rh = [R_opsH[h] for h in hh]
                        for q, h in enumerate(hh):
                            sc.op('pe', lambda e, q=q, h=h: e.matmul(psum[pA][0:64, q * 128:q * 128 + 64], lhsT=BhT[:, h, cs], rhs=QtT[:, h, cs], start=True, stop=True), [Rh[q]], [R_ps[pA]])
                            sc.op('pe', lambda e, q=q, h=h: e.matmul(psum[pA][0:64, q * 128 + 64:q * 128 + 128], lhsT=QtT[:, h, cs], rhs=BhT[:, h, cs], start=True, stop=True), [Rh[q]], [R_ps[pA]])
                            sc.op('pe', lambda e, q=q, h=h: e.matmul(psum[pB][0:64, q * 128:q * 128 + 64], lhsT=KhT[:, h, cs], rhs=QtT[:, h, cs], start=True, stop=True), [Rh[q]], [R_ps[pB]])
                            sc.op('pe', lambda e, q=q, h=h: e.matmul(psum[pB][0:64, q * 128 + 64:q * 128 + 128], lhsT=KhT[:, h, cs], rhs=RtT[:, h, cs], start=True, stop=True), [Rh[q]], [R_ps[pB]])
                            sc.op('pe', lambda e, q=q, h=h: e.matmul(psum[pC][0:64, q * 64:q * 64 + 64], lhsT=BhT[:, h, cs], rhs=RtT[:, h, cs], start=True, stop=True), [Rh[q]], [R_ps[pC]])
                        vA = psum[pA][0:64, :].rearrange("p (q x) -> p q x", x=128)
                        vB = psum[pB][0:64, :].rearrange("p (q x) -> p q x", x=128)
                        vC = psum[pC][0:64, 0:256].rearrange("p (q x) -> p q x", x=64)
                        sc.op('dve', lambda e: e.tensor_tensor(out=MZ[:, :, 0:64], in0=vA[:, :, 0:64], in1=RM[:, 0, :, :], op=ALU.mult), [R_ps[pA], R_prm], [R_MZ])
                        sc.op('dve', lambda e: e.tensor_tensor(out=MZ[:, :, 128:192], in0=vA[:, :, 64:128], in1=RM[:, 1, :, :], op=ALU.mult), [R_ps[pA], R_prm], [R_MZ])
                        sc.op('dve', lambda e: e.tensor_tensor(out=MZ[:, :, 64:128], in0=MZ[:, :, 0:64], in1=I4[:], op=ALU.add), [R_MZ, R_prm], [R_MZ])
                        sc.op('dve', lambda e: e.tensor_tensor(out=SC[:, 0, :, :], in0=vB[:, :, 0:64], in1=RM[:, 0, :, :], op=ALU.mult), [R_ps[pB], R_prm], [R_SC])
                        sc.op('dve', lambda e: e.tensor_scalar(out=SC[:, 0, :, :], in0=SC[:, 0, :, :], scalar1=-1.0, scalar2=None, op0=ALU.mult), [R_SC], [R_SC])
                        sc.op('dve', lambda e: e.tensor_tensor(out=SC[:, 1, :, :], in0=vB[:, :, 64:128], in1=RM[:, 2, :, :], op=ALU.mult), [R_ps[pB], R_prm], [R_SC])
                        sc.op('dve', lambda e: e.tensor_tensor(out=SC[:, 2, :, :], in0=vC, in1=RM[:, 2, :, :], op=ALU.mult), [R_ps[pC], R_prm], [R_SC])
                        for q, h in enumerate(hh):
                            for i, src in enumerate((KbT, BbT, VTb)):
                                sc.op('pe', lambda e, q=q, h=h, i=i, src=src: e.transpose(out=psT_b[0:64, q * 192 + i * 64:q * 192 + i * 64 + 64], in_=src[:, h, cs], identity=identb[0:64, 0:64]), [Rh[q], R_ident], [R_ps[7]])
                        sc.op('act', lambda e: e.copy(out=TM[:].rearrange("p q x -> p (q x)"), in_=psT_b[0:64, 0:768]), [R_ps[7]], [R_TM])
                        vD = psum[pD][0:64, :].rearrange("p (q x) -> p q x", x=128)
                        vE = psum[pE][0:64, 0:256].rearrange("p (q x) -> p q x", x=64)
                        for lev in range(6):
                            for q in range(4):
                                if lev == 0:
                                    sc.op('pe', lambda e, q=q: e.matmul(psum[pD][0:64, q * 128:q * 128 + 64], lhsT=MZ[:, q, 128:192], rhs=MZ[:, q, 0:64], start=True, stop=True), [R_MZ], [R_ps[pD]])
                                elif lev < 5:
                                    sc.op('pe', lambda e, q=q: e.matmul(psum[pD][0:64, q * 128:q * 128 + 128], lhsT=MZ[:, q, 128:192], rhs=MZ[:, q, 0:128], start=True, stop=True), [R_MZ], [R_ps[pD]])
                                else:
                                    sc.op('pe', lambda e, q=q: e.matmul(psum[pD][0:64, q * 128 + 64:q * 128 + 128], lhsT=MZ[:, q, 128:192], rhs=MZ[:, q, 64:128], start=True, stop=True), [R_MZ], [R_ps[pD]])
                                if lev < 5:
                                    sc.op('pe', lambda e, q=q: e.matmul(psum[pE][0:64, q * 64:q * 64 + 64], lhsT=MZ[:, q, 0:64], rhs=MZ[:, q, 128:192], start=True, stop=True), [R_MZ], [R_ps[pE]])
                            if lev >= 1:
                                sc.op('dve', lambda e: e.tensor_tensor(out=MZ[:, :, 64:128], in0=vD[:, :, 64:128], in1=MZ[:, :, 64:128], op=ALU.add), [R_ps[pD], R_MZ], [R_MZ])
                            if lev < 5:
                                sc.op('act', lambda e: e.copy(out=MZ[:, :, 0:64], in_=vD[:, :, 0:64]), [R_ps[pD]], [R_MZ])
                                sc.op('act', lambda e: e.copy(out=MZ[:, :, 128:192], in_=vE), [R_ps[pE]], [R_MZ])
                        RP = R_P[hg]
                        for q, h in enumerate(hh):
                            sc.op('pe', lambda e, q=q, h=h: e.matmul(psum[pF][0:64, q * 64:q * 64 + 64], lhsT=QtT[:, h, cs], rhs=Pb[:, h, :], start=True, stop=False), [Rh[q], RP], [R_ps[pF]])
                            sc.op('pe', lambda e, q=q: e.matmul(psum[pF][0:64, q * 64:q * 64 + 64], lhsT=SC[:, 0, q, :], rhs=TM[:, q, 128:192], start=False, stop=True), [R_SC, R_TM], [R_ps[pF]])
                        sc.op('act', lambda e: e.copy(out=Xs[:].rearrange("p q x -> p (q x)"), in_=psum[pF][0:64, 0:256]), [R_ps[pF]], [R_X])
                        for q in range(4):
                            sc.op('pe', lambda e, q=q: e.matmul(psum[pF][0:64, 256 + q * 64:256 + q * 64 + 64], lhsT=MZ[:, q, 64:128], rhs=Xs[:, q, :], start=True, stop=True), [R_MZ, R_X], [R_ps[pF]])
                        sc.op('act', lambda e: e.mul(out=Un[:].rearrange("p q x -> p (q x)"), in_=psum[pF][0:64, 256:512], mul=-1.0), [R_ps[pF]], [R_U])
                        for q, h in enumerate(hh):
                            sc.op('pe', lambda e, q=q: e.matmul(psum[pG][0:64, q * 64:q * 64 + 64], lhsT=TM[:, q, 0:64], rhs=TM[:, q, 128:192], start=True, stop=False), [R_TM], [R_ps[pG]])
                            sc.op('pe', lambda e, q=q: e.matmul(psum[pG][0:64, q * 64:q * 64 + 64], lhsT=TM[:, q, 64:128], rhs=Un[:, q, :], start=False, stop=True), [R_TM, R_U], [R_ps[pG]])
                        for q, h in enumerate(hh):
                            sc.op('pe', lambda e, q=q, h=h: e.matmul(psum[pG][0:64, 256 + q * 64:256 + q * 64 + 64], lhsT=Pb[:, h, :], rhs=RtT[:, h, cs], start=True, stop=False), [Rh[q], RP], [R_ps[pG]])
                            sc.op('pe', lambda e, q=q: e.matmul(psum[pG][0:64, 256 + q * 64:256 + q * 64 + 64], lhsT=TM[:, q, 128:192], rhs=SC[:, 1, q, :], start=False, stop=False), [R_TM, R_SC], [R_ps[pG]])
                            sc.op('pe', lambda e, q=q: e.matmul(psum[pG][0:64, 256 + q * 64:256 + q * 64 + 64], lhsT=Un[:, q, :], rhs=SC[:, 2, q, :], start=False, stop=True), [R_U, R_SC], [R_ps[pG]])
                        sc.op('act', lambda e: e.copy(out=yT[:, hg * 4:hg * 4 + 4, cs], in_=psum[pG][0:64, 256:512].rearrange("p (q x) -> p q x", x=64)), [R_ps[pG]], [R_yT[hg]])
                        for q, h in enumerate(hh):
                            sc.op('dve', lambda e, q=q, h=h: e.scalar_tensor_tensor(out=Pf[:, h, :], in0=Pf[:, h, :], scalar=GC[:, h, c:c + 1], in1=psum[pG][0:64, q * 64:q * 64 + 64], op0=ALU.mult, op1=ALU.add), [R_ps[pG], Rh[q], RP], [RP])
                        sc.op('dve', lambda e: e.tensor_copy(out=Pb[:, hg * 4:hg * 4 + 4, :], in_=Pf[:, hg * 4:hg * 4 + 4, :]), [RP], [RP])

                    def post_head(blk, h):
                        t0 = blk * RB
                        hg = h // 4
                        tx, ty, tz, tw = T[0], T[1], T[2], T[3]
                        Rx, Ry, Rz, Rw = R_T[0], R_T[1], R_T[2], R_T[3]
                        p1 = pre_ps(); p2 = pre_ps()
                        sc.op('pe', lambda e: e.matmul(psum[p1][0:64, 256:512], lhsT=o1, rhs=yT[:, h, :], start=True, stop=True), [R_yT[hg], R_MC], [R_ps[p1]])
                        sc.op('act', lambda e: e.activation(out=tx[:], in_=yT[:, h, :], func=AF.Square), [R_yT[hg]], [Rx])
                        sc.op('pe', lambda e: e.matmul(psum[p2][0:64, 256:512], lhsT=o1, rhs=tx[:], start=True, stop=True), [Rx, R_MC], [R_ps[p2]])
                        sc.op('act', lambda e: e.mul(out=ty[:], in_=psum[p1][0:64, 256:512], mul=1.0 / 64), [R_ps[p1]], [Ry])
                        sc.op('act', lambda e: e.mul(out=tz[:], in_=psum[p2][0:64, 256:512], mul=1.0 / 64), [R_ps[p2]], [Rz])
                        sc.op('dve', lambda e: e.tensor_tensor(out=tw[:], in0=ty[:], in1=ty[:], op=ALU.mult), [Ry], [Rw])
                        sc.op('dve', lambda e: e.tensor_tensor(out=tz[:], in0=tz[:], in1=tw[:], op=ALU.subtract), [Rz, Rw], [Rz])
                        sc.op('dve', lambda e: e.tensor_scalar(out=tz[:], in0=tz[:], scalar1=64e-5, scalar2=None, op0=ALU.add), [Rz], [Rz])
                        sc.op('act', lambda e: e.activation(out=tz[:], in_=tz[:], func=AF.Sqrt), [Rz], [Rz])
                        sc.op('dve', lambda e: e.reciprocal(out=tz[:], in_=tz[:]), [Rz], [Rz])
                        sc.op('dve', lambda e: e.tensor_tensor(out=tw[:], in0=yT[:, h, :], in1=ty[:], op=ALU.subtract), [R_yT[hg], Ry], [Rw])
                        sc.op('dve', lambda e: e.tensor_tensor(out=tw[:], in0=tw[:], in1=tz[:], op=ALU.mult), [Rw, Rz], [Rw])
                        sc.op('dve', lambda e: e.tensor_scalar(out=tw[:], in0=tw[:], scalar1=ln_g[:, h:h + 1], scalar2=ln_b[:, h:h + 1], op0=ALU.mult, op1=ALU.add), [Rw, R_prm], [Rw])
                        sc.op('pool', lambda e: e.tensor_tensor(out=tw[:], in0=tw[:], in1=BON[:, h, :], op=ALU.add), [Rw, R_opsH[h]], [Rw])
                        sc.op('pool', lambda e: e.tensor_tensor(out=OBs[:, h, :], in0=tw[:], in1=Gg[:, h, :], op=ALU.mult), [Rw, R_opsH[h]], [R_OBs])

                    OBs = sb("OBs", [64, 12, RB], BF16, ph); R_OBs = Res()

                    V32 = sb("V32", [64, 12, RB], F32, ph); R_V32 = Res()
                    if l > 0:
                        VFt = sb("VFt", [64, 12, RB], F32, ph); R_VFt = Res()
                        vbf = sb("vbf", [64, 12, RB], BF16, ph); lob = sb("lob", [32, RB], BF16, ph); R_vbf = Res(); R_lob = Res()
                        mvd = sb("mvd", [64, 12, 32], BF16, ph); mvu = sb("mvu", [32, 768], BF16, ph)
                        v0 = prm("v0", rwkv_v0[l - 1])
                        sc.dma('pool', lambda e: e.dma_start(out=mvd[:], in_=rwkv_mv_down[l - 1].rearrange("(h d) m -> d h m", d=64)), [], [R_prm])
                        sc.dma('pool', lambda e: e.dma_start(out=mvu[:], in_=rwkv_mv_up[l - 1]), [], [R_prm])

                    def pre_v(blk):
                        t0 = blk * RB
                        for h in range(12):
                            def one(h):
                                shift('dve', V32[:, h, :], zr[2], (slice(0, 64), h), mu_v[:, h:h + 1], T[2][:], R_z[2], R_T[2], R_V32)
                            one(h)
                        if l == 0:
                            sc.dma('sp', lambda e: e.dma_start(out=VF.rearrange("(h d) s -> d h s", d=64)[:, :, t0:t0 + RB], in_=V32[:]), [R_V32], [R_VF])
                        else:
                            sc.dma('sp', lambda e: e.dma_start(out=VFt[:], in_=VF.rearrange("(h d) s -> d h s", d=64)[:, :, t0:t0 + RB]), [R_VF], [R_VFt])
                            sc.op('act', lambda e: e.copy(out=vbf[:], in_=V32[:]), [R_V32], [R_vbf])
                            pl = pre_ps()
                            for h in range(12):
                                sc.op('pe', lambda e, h=h: e.matmul(psum[pl][0:32, 256:512], lhsT=mvd[:, h, :], rhs=vbf[:, h, :], start=(h == 0), stop=(h == 11)), [R_vbf, R_prm], [R_ps[pl]])
                            sc.op('act', lambda e: e.copy(out=lob[:], in_=psum[pl][0:32, 256:512]), [R_ps[pl]], [R_lob])
                            for h in range(12):
                                def one2(h):
                                    p_ = pre_ps()
                                    sc.op('pe', lambda e: e.matmul(psum[p_][0:64, 256:512], lhsT=mvu[:, h * 64:(h + 1) * 64], rhs=lob[:], start=True, stop=True), [R_lob, R_prm], [R_ps[p_]])
                                    sc.op('act', lambda e: e.activation(out=T[0][:], in_=psum[p_][0:64, 256:512], func=AF.Sigmoid, bias=v0[:, h:h + 1]), [R_ps[p_], R_prm], [R_T[0]])
                                    sc.op('pool', lambda e: e.tensor_tensor(out=T[1][:], in0=VFt[:, h, :], in1=V32[:, h, :], op=ALU.subtract), [R_VFt, R_V32], [R_T[1]])
                                    sc.op('dve', lambda e: e.tensor_tensor(out=T[1][:], in0=T[1][:], in1=T[0][:], op=ALU.mult), [R_T[1], R_T[0]], [R_T[1]])
                                    sc.op('pool', lambda e: e.tensor_tensor(out=V32[:, h, :], in0=V32[:, h, :], in1=T[1][:], op=ALU.add), [R_T[1], R_V32], [R_V32])
                                one2(h)

                    def rw_block(blk):
                        load_block(blk)
                        pre_v(blk)
                        for h in range(12):
                            pre_head(blk, h)
                        if RW_STAGE >= 2:
                            for c in range(NCH):
                                for hg in range(3):
                                    chunk_group(blk, c, hg)
                        if RW_STAGE >= 3:
                            for h in range(12):
                                post_head(blk, h)
                            sc.dma('sp', lambda e: e.dma_start(out=OB.rearrange("(h d) s -> d h s", d=64)[:, :, blk * RB:(blk + 1) * RB], in_=OBs[:]), [R_OBs], [R_OB])

                    for blk in range(NBLK if RW_BLOCKS is None else RW_BLOCKS):
                        rw_block(blk)
                sc.barrier()
            if DO_F:
              with ExitStack() as ph:
                TGF = 256; NTF = S // TGF
                PA = sb("PA", [128, 6, D], BF16, ph); PB = sb("PB", [128, 6, D], BF16, ph); PC = sb("PC", [128, 2, D], BF16, ph); WO = sb("WO", [128, KC, D], BF16, ph); R_W = Res()
                for kc in range(6):
                    sc.dma('pool', lambda e, kc=kc: e.dma_start(out=PA[:, kc, :], in_=p_a[l, kc * 128:(kc + 1) * 128, :]), [], [R_W])
                    sc.dma('pool', lambda e, kc=kc: e.dma_start(out=PB[:, kc, :], in_=p_b[l, kc * 128:(kc + 1) * 128, :]), [], [R_W])
                for kc in range(2):
                    sc.dma('pool', lambda e, kc=kc: e.dma_start(out=PC[:, kc, :], in_=p_c[l, kc * 128:(kc + 1) * 128, :]), [], [R_W])
                for kc in range(KC):
                    sc.dma('pool', lambda e, kc=kc: e.dma_start(out=WO[:, kc, :], in_=w_o[l, kc * 128:(kc + 1) * 128, :]), [], [R_W])
                lng = sb("lng", [128, KC], F32, ph); lnb = sb("lnb", [128, KC], F32, ph); g1p = sb("g1p", [128, KC], F32, ph); R_ln = Res()
                sc.dma('sp', lambda e: e.dma_start(out=lng[:], in_=ln1_g[l].rearrange("(k p) -> p k", p=128), allow_slow_non_contiguous=True), [], [R_ln])
                sc.dma('sp', lambda e: e.dma_start(out=lnb[:], in_=ln1_b[l].rearrange("(k p) -> p k", p=128), allow_slow_non_contiguous=True), [], [R_ln])
                sc.op('dve', lambda e: e.tensor_scalar(out=g1p[:], in0=modT[:, 32:48], scalar1=1.0, scalar2=None, op0=ALU.add), [R_mod], [R_ln])
                oaT = sb("oaT", [128, 6, TGF], BF16, ph); obT = sb("obT", [128, 6, TGF], BF16, ph); ocT = sb("ocT", [128, 2, TGF], BF16, ph); R_o = Res()
                gAB = [sb("gAB%d" % i, [128, 3, 4, TGF], BF16, ph) for i in range(2)]; R_g = [Res(), Res()]
                mrg = sb("mrg", [128, KC, TGF], BF16, ph); R_mrg = Res()
                xy = sb("xy", [128, KC, TGF], F32, ph); R_xy = Res()
                tA = [sb("tA%d" % i, [128, TGF], F32, ph) for i in range(2)]; R_tA = [Res(), Res()]
                tB = [sb("tB%d" % i, [128, TGF], F32, ph) for i in range(2)]; R_tB = [Res(), Res()]
                mean = sb("mean", [128, TGF], F32, ph); rstd = sb("rstd", [128, TGF], F32, ph); R_st = Res()
                XTv = XT.rearrange("(k p) s -> p k s", p=128)
                PRv = PROJ.rearrange("(k p) s -> p k s", p=128)
                ALPHA = 8.0 ** 0.25
                cntF = [0]

                def f_merge_dc(dc, gb):
                    j = dc % 4
                    pa = nextps(); pb_ = nextps(); pc = nextps()
                    for kc in range(6):
                        sc.op('pe', lambda e, kc=kc: e.matmul(psum[pa][:, 0:TGF], lhsT=PA[:, kc, dc * 128:(dc + 1) * 128], rhs=oaT[:, kc, :], start=(kc == 0), stop=(kc == 5)), [R_W, R_o], [R_ps[pa]])
                    for kc in range(6):
                        sc.op('pe', lambda e, kc=kc: e.matmul(psum[pb_][:, 0:TGF], lhsT=PB[:, kc, dc * 128:(dc + 1) * 128], rhs=obT[:, kc, :], start=(kc == 0), stop=(kc == 5)), [R_W, R_o], [R_ps[pb_]])
                    for kc in range(2):
                        sc.op('pe', lambda e, kc=kc: e.matmul(psum[pc][:, 0:TGF], lhsT=PC[:, kc, dc * 128:(dc + 1) * 128], rhs=ocT[:, kc, :], start=(kc == 0), stop=(kc == 1)), [R_W, R_o], [R_ps[pc]])
                    i = cntF[0] % 2; cntF[0] += 1
                    sc.op('dve', lambda e: e.tensor_tensor(out=tA[i][:], in0=psum[pa][:, 0:TGF], in1=gAB[gb][:, 0, j, :], op=ALU.mult), [R_ps[pa], R_g[gb]], [R_tA[i]])
                    sc.op('dve', lambda e: e.tensor_tensor(out=tB[i][:], in0=psum[pb_][:, 0:TGF], in1=gAB[gb][:, 1, j, :], op=ALU.mult), [R_ps[pb_], R_g[gb]], [R_tB[i]])
                    sc.op('pool', lambda e: e.tensor_tensor(out=tA[i][:], in0=tA[i][:], in1=tB[i][:], op=ALU.add), [R_tA[i], R_tB[i]], [R_tA[i]])
                    sc.op('dve', lambda e: e.tensor_tensor(out=tB[i][:], in0=psum[pc][:, 0:TGF], in1=gAB[gb][:, 2, j, :], op=ALU.mult), [R_ps[pc], R_g[gb]], [R_tB[i]])
                    sc.op('pool', lambda e: e.tensor_tensor(out=mrg[:, dc, :], in0=tA[i][:], in1=tB[i][:], op=ALU.add), [R_tA[i], R_tB[i]], [R_mrg])

                def f_wo_dc(dc):
                    pm = nextps()
                    for kc in range(KC):
                        sc.op('pe', lambda e, kc=kc: e.matmul(psum[pm][:, 0:TGF], lhsT=WO[:, kc, dc * 128:(dc + 1) * 128], rhs=mrg[:, kc, :], start=(kc == 0), stop=(kc == KC - 1)), [R_W, R_mrg], [R_ps[pm]])
                    sc.op('act', lambda e: e.mul(out=xy[:, dc, :], in_=xy[:, dc, :], mul=ALPHA), [R_xy], [R_xy])
                    sc.op('dve', lambda e: e.scalar_tensor_tensor(out=xy[:, dc, :], in0=psum[pm][:, 0:TGF], scalar=g1p[:, dc:dc + 1], in1=xy[:, dc, :], op0=ALU.mult, op1=ALU.add), [R_ps[pm], R_ln, R_xy], [R_xy])

                def f_tg(tf):
                    ts = slice(tf * TGF, (tf + 1) * TGF)
                    RX = R_XT[tf // 2]
                    sc.dma('sp', lambda e: e.dma_start(out=oaT[:], in_=OA.rearrange("(k p) s -> p k s", p=128)[:, :, ts]), [R_OA], [R_o])
                    sc.dma('sp', lambda e: e.dma_start(out=obT[:], in_=OB.rearrange("(k p) s -> p k s", p=128)[:, :, ts]), [R_OB], [R_o])
                    sc.dma('sp', lambda e: e.dma_start(out=ocT[:], in_=OC.rearrange("(k p) s -> p k s", p=128)[:, :, ts]), [R_OC], [R_o])
                    sc.dma('sp', lambda e: e.dma_start(out=xy[:], in_=XTv[:, :, ts]), [RX], [R_xy])
                    for g4 in range(4):
                        gb = g4 % 2
                        for br in range(3):
                            k0 = (OFF_G + br * D) // 128 + g4 * 4
                            sc.dma('sp', lambda e, k0=k0, gb=gb, br=br: e.dma_start(out=gAB[gb][:, br, :, :], in_=PRv[:, k0:k0 + 4, ts]), [R_PROJ], [R_g[gb]])
                        for j in range(4):
                            f_merge_dc(g4 * 4 + j, gb)
                    for dc in range(KC):
                        f_wo_dc(dc)
                    ln_feature_major(xy, R_xy, TGF, lng, lnb, R_ln, tA, R_tA, tB, R_tB, mean, rstd, R_st)
                    sc.dma('sp', lambda e: e.dma_start(out=XTv[:, :, ts], in_=xy[:]), [R_xy], [RX])

                for tf in range(NTF):
                    f_tg(tf)
              sc.barrier()
            if DO_G:
              with ExitStack() as ph:
                TGM = 1024; NSUB = 2; NTM = S // TGM
                hTm_ = sb("hTm", [128, KC, TGM], BF16, ph); R_hTm = Res()
                acc = sb("acc", [128, KC, TGM], F32, ph); R_acc = [Res() for _ in range(NSUB)]
                Wgu = [sb("Wgu%d" % i, [128, KC, 512], BF16, ph) for i in range(2)]; Wd = [sb("Wd%d" % i, [128, 2, D], BF16, ph) for i in range(2)]; R_We = [Res(), Res()]
                gwb = [sb("gwb%d" % i, [128, TGM], F32, ph) for i in range(2)]; R_gwb = [Res(), Res()]
                xt4 = [sb("xt4%d" % i, [128, 512], F32, ph) for i in range(2)]; R_xt4 = [Res(), Res()]
                hf = [sb("hf%d" % i, [128, 512], F32, ph) for i in range(2)]; R_hf = [Res(), Res()]
                sg = [sb("sg%d" % i, [128, 512], F32, ph) for i in range(2)]; R_sg = [Res(), Res()]
                actT = [sb("actT%d" % i, [128, 2, 512], BF16, ph) for i in range(2)]; R_actT = [Res(), Res()]
                Wr = sb("Wr", [128, KC, 72], F32, ph); br72 = sb("br72", [128, 4, 72], F32, ph); R_Wr = Res()
                sc.dma('sp', lambda e: e.dma_start(out=Wr[:, :, 0:8], in_=router_grp_w[l].rearrange("(k p) n -> p k n", p=128)), [], [R_Wr])
                sc.dma('sp', lambda e: e.dma_start(out=Wr[:, :, 8:72], in_=router_exp_w[l].rearrange("(k p) n -> p k n", p=128)), [], [R_Wr])
                for t_ in range(4):
                    sc.dma('sp', lambda e, t_=t_: e.dma_start(out=br72[:, t_, 0:8], in_=bass.AP(router_grp_b.tensor, l * 8, [[0, 128], [1, 8]])), [], [R_Wr])
                    sc.dma('sp', lambda e, t_=t_: e.dma_start(out=br72[:, t_, 8:72], in_=bass.AP(router_exp_b.tensor, l * 64, [[0, 128], [1, 64]])), [], [R_Wr])
                lng2 = sb("lng2", [128, KC], F32, ph); lnb2 = sb("lnb2", [128, KC], F32, ph); g2p = sb("g2p", [128, KC], F32, ph); sc2p = sb("sc2p", [128, KC], F32, ph); R_ln2 = Res()
                sc.dma('sp', lambda e: e.dma_start(out=lng2[:], in_=ln2_g[l].rearrange("(k p) -> p k", p=128), allow_slow_non_contiguous=True), [], [R_ln2])
                sc.dma('sp', lambda e: e.dma_start(out=lnb2[:], in_=ln2_b[l].rearrange("(k p) -> p k", p=128), allow_slow_non_contiguous=True), [], [R_ln2])
                sc.op('dve', lambda e: e.tensor_scalar(out=g2p[:], in0=modT[:, 80:96], scalar1=1.0, scalar2=None, op0=ALU.add), [R_mod], [R_ln2])
                sc.op('dve', lambda e: e.tensor_scalar(out=sc2p[:], in0=modT[:, 64:80], scalar1=1.0, scalar2=None, op0=ALU.add), [R_mod], [R_ln2])
                lgt = sb("lgt", [128, 4, 72], F32, ph); R_lgt = Res()
                rt = sb("rt", [128, 8, 64], F32, ph); R_rt = Res()
                gwt = sb("gwt", [128, 64], F32, ph); gwT = sb("gwT", [64, 512], F32, ph); R_gwT = Res()
                tAm = sg; R_tAm = R_sg
                tBm = hf; R_tBm = R_hf
                meanm = sb("meanm", [128, 512], F32, ph); rstdm = sb("rstdm", [128, 512], F32, ph); R_st2 = Res()
                XTv2 = XT.rearrange("(k p) s -> p k s", p=128)
                ALPHA = 8.0 ** 0.25
                cntG = [0]

                def route_tile(tm, sub, t):
                    L = lgt[:, t, :]
                    r = rt
                    c1 = r[:, 0, 0:1]; c2 = r[:, 0, 1:2]; c3 = r[:, 0, 2:3]; c4 = r[:, 0, 3:4]; c5 = r[:, 0, 4:5]; c6 = r[:, 0, 5:6]
                    RL = [R_lgt, R_rt]
                    def O(eng, f):
                        sc.op(eng, f, RL, [R_rt])
                    O('dve', lambda e: e.tensor_reduce(out=c1, in_=L[:, 0:8], axis=AX.X, op=ALU.max))
                    O('dve', lambda e: e.tensor_scalar(out=r[:, 1, 0:8], in0=L[:, 0:8], scalar1=c1, scalar2=None, op0=ALU.subtract))
                    O('act', lambda e: e.activation(out=r[:, 1, 8:16], in_=r[:, 1, 0:8], func=AF.Exp))
                    O('dve', lambda e: e.tensor_reduce(out=c2, in_=r[:, 1, 8:16], axis=AX.X, op=ALU.add))
                    O('dve', lambda e: e.reciprocal(out=c2, in_=c2))
                    O('dve', lambda e: e.tensor_scalar(out=r[:, 1, 16:24], in0=L[:, 0:8], scalar1=c1, scalar2=None, op0=ALU.is_ge))
                    O('dve', lambda e: e.tensor_scalar(out=r[:, 1, 16:24], in0=r[:, 1, 16:24], scalar1=-1.0, scalar2=1e30, op0=ALU.add, op1=ALU.mult))
                    for g in range(8):
                        O('dve', lambda e, g=g: e.tensor_scalar(out=r[:, 2, g * 8:(g + 1) * 8], in0=L[:, 8 + g * 8:16 + g * 8], scalar1=r[:, 1, 16 + g:17 + g], scalar2=None, op0=ALU.add))
                    O('dve', lambda e: e.max(out=r[:, 3, 0:8], in_=r[:, 2, :]))
                    O('dve', lambda e: e.tensor_scalar(out=r[:, 4, :], in0=r[:, 2, :], scalar1=r[:, 3, 0:1], scalar2=None, op0=ALU.is_ge))
                    O('dve', lambda e: e.tensor_scalar(out=r[:, 5, :], in0=r[:, 2, :], scalar1=r[:, 3, 1:2], scalar2=None, op0=ALU.is_ge))
                    O('dve', lambda e: e.tensor_tensor(out=c3, in0=r[:, 3, 1:2], in1=r[:, 3, 0:1], op=ALU.subtract))
                    O('act', lambda e: e.activation(out=c3, in_=c3, func=AF.Exp))
                    O('dve', lambda e: e.tensor_scalar(out=c3, in0=c3, scalar1=1.0, scalar2=None, op0=ALU.add))
                    O('dve', lambda e: e.reciprocal(out=c3, in_=c3))
                    O('dve', lambda e: e.tensor_scalar(out=c4, in0=c3, scalar1=-1.0, scalar2=1.0, op0=ALU.mult, op1=ALU.add))
                    O('dve', lambda e: e.tensor_tensor(out=c5, in0=c3, in1=c4, op=ALU.subtract))
                    O('dve', lambda e: e.tensor_tensor(out=c5, in0=c5, in1=c2, op=ALU.mult))
                    O('dve', lambda e: e.tensor_tensor(out=c6, in0=c4, in1=c2, op=ALU.mult))
                    O('dve', lambda e: e.tensor_scalar(out=r[:, 5, :], in0=r[:, 5, :], scalar1=c6, scalar2=None, op0=ALU.mult))
                    sc.op('dve', lambda e: e.scalar_tensor_tensor(out=gwt[:], in0=r[:, 4, :], scalar=c5, in1=r[:, 5, :], op0=ALU.mult, op1=ALU.add), RL, [R_rt, R_gwT])
                    pt = nextps()
                    sc.op('pe', lambda e: e.transpose(out=psum[pt][0:64, 0:128], in_=gwt[:], identity=ident[:]), [R_gwT, R_ident], [R_ps[pt]])
                    sc.op('act', lambda e: e.copy(out=gwT[:, t * 128:(t + 1) * 128], in_=psum[pt][0:64, 0:128]), [R_ps[pt]], [R_gwT])

                def m_sub_prep(tm, sub):
                    t0 = tm * TGM + sub * 512
                    pl = nextps()
                    for dc in range(KC):
                        def two(dc):
                            b = cntG[0] % 2; cntG[0] += 1
                            i = dc % 2
                            sc.dma('sp', lambda e: e.dma_start(out=xt4[b][:], in_=XTv2[:, dc, t0:t0 + 512]), [R_XT[t0 // 512]], [R_xt4[b]])
                            sc.op('dve', lambda e: e.tensor_scalar(out=hf[i][:], in0=xt4[b][:], scalar1=sc2p[:, dc:dc + 1], scalar2=modT[:, 48 + dc:49 + dc], op0=ALU.mult, op1=ALU.add), [R_xt4[b], R_ln2, R_mod], [R_hf[i]])
                            sc.op('act', lambda e: e.copy(out=hTm_[:, dc, sub * 512:(sub + 1) * 512], in_=hf[i][:]), [R_hf[i]], [R_hTm])
                            for t in range(4):
                                sc.op('pe', lambda e, t=t: e.matmul(psum[pl][:, t * 72:(t + 1) * 72], lhsT=hf[i][:, t * 128:(t + 1) * 128], rhs=Wr[:, dc, :], start=(dc == 0 and t == 0), stop=(dc == KC - 1)), [R_hf[i], R_Wr], [R_ps[pl]])
                        two(dc)
                    sc.op('dve', lambda e: e.tensor_tensor(out=lgt[:], in0=psum[pl][:, 0:288].rearrange("p (t n) -> p t n", n=72), in1=br72[:], op=ALU.add), [R_ps[pl], R_Wr], [R_lgt])
                    for t in range(4):
                        route_tile(tm, sub, t)
                    sc.dma('sp', lambda e: e.dma_start(out=GW[:, t0:t0 + 512], in_=gwT[:]), [R_gwT], [R_GW])

                def m_expert(tm, e_, wb):
                    sc.dma('pool', lambda e: e.dma_start(out=Wgu[wb][:, :, 0:256], in_=exp_w_gate[l, e_].rearrange("(k p) f -> p k f", p=128)), [], [R_We[wb]])
                    sc.dma('pool', lambda e: e.dma_start(out=Wgu[wb][:, :, 256:512], in_=exp_w_up[l, e_].rearrange("(k p) f -> p k f", p=128)), [], [R_We[wb]])
                    sc.dma('pool', lambda e: e.dma_start(out=Wd[wb][:], in_=exp_w_down[l, e_].rearrange("(k p) f -> p k f", p=128)), [], [R_We[wb]])
                    sc.dma('sp', lambda e: e.dma_start(out=gwb[wb][:], in_=bass.AP(GW.tensor, e_ * S + tm * TGM, [[0, 128], [1, TGM]])), [R_GW], [R_gwb[wb]])
                    for sub in range(NSUB):
                        def one(sub):
                            cs = slice(sub * 512, (sub + 1) * 512)
                            ab = cntG[0] % 2; cntG[0] += 1
                            for fc in range(2):
                                def two(fc):
                                    pg = nextps(); pu = nextps()
                                    for k in range(KC):
                                        sc.op('pe', lambda e, k=k: e.matmul(psum[pg][:], lhsT=Wgu[wb][:, k, fc * 128:(fc + 1) * 128], rhs=hTm_[:, k, cs], start=(k == 0), stop=(k == KC - 1)), [R_We[wb], R_hTm], [R_ps[pg]])
                                    for k in range(KC):
                                        sc.op('pe', lambda e, k=k: e.matmul(psum[pu][:], lhsT=Wgu[wb][:, k, 256 + fc * 128:256 + (fc + 1) * 128], rhs=hTm_[:, k, cs], start=(k == 0), stop=(k == KC - 1)), [R_We[wb], R_hTm], [R_ps[pu]])
                                    i = fc
                                    sc.op('act', lambda e: e.activation(out=sg[i][:], in_=psum[pg][:], func=AF.Silu), [R_ps[pg]], [R_sg[i]])
                                    sc.op('dve', lambda e: e.tensor_tensor(out=sg[i][:], in0=psum[pu][:], in1=sg[i][:], op=ALU.mult), [R_ps[pu], R_sg[i]], [R_sg[i]])
                                    sc.op('pool', lambda e: e.tensor_tensor(out=actT[ab][:, fc, :], in0=sg[i][:], in1=gwb[wb][:, cs], op=ALU.mult), [R_sg[i], R_gwb[wb]], [R_actT[ab]])
                                two(fc)
                            for dc in range(KC):
                                def three(dc):
                                    pd = nextps()
                                    for fc in range(2):
                                        sc.op('pe', lambda e, fc=fc: e.matmul(psum[pd][:], lhsT=Wd[wb][:, fc, dc * 128:(dc + 1) * 128], rhs=actT[ab][:, fc, :], start=(fc == 0), stop=(fc == 1)), [R_We[wb], R_actT[ab]], [R_ps[pd]])
                                    if e_ == 0:
                                        sc.op('act', lambda e: e.copy(out=acc[:, dc, cs], in_=psum[pd][:]), [R_ps[pd]], [R_acc[sub]])
                                    else:
                                        sc.op('dve', lambda e: e.tensor_tensor(out=acc[:, dc, cs], in0=psum[pd][:], in1=acc[:, dc, cs], op=ALU.add), [R_ps[pd], R_acc[sub]], [R_acc[sub]])
                                three(dc)
                        one(sub)

                def m_finish(tm, sub):
                    t0 = tm * TGM + sub * 512
                    cs = slice(sub * 512, (sub + 1) * 512)
                    for dc in range(KC):
                        def one(dc):
                            b = cntG[0] % 2; cntG[0] += 1
                            sc.dma('sp', lambda e: e.dma_start(out=xt4[b][:], in_=XTv2[:, dc, t0:t0 + 512]), [R_XT[t0 // 512]], [R_xt4[b]])
                            sc.op('act', lambda e: e.mul(out=xt4[b][:], in_=xt4[b][:], mul=ALPHA), [R_xt4[b]], [R_xt4[b]])
                            sc.op('dve', lambda e: e.scalar_tensor_tensor(out=acc[:, dc, cs], in0=acc[:, dc, cs], scalar=g2p[:, dc:dc + 1], in1=xt4[b][:], op0=ALU.mult, op1=ALU.add), [R_acc[sub], R_ln2, R_xt4[b]], [R_acc[sub]])
                        one(dc)
                    ln_feature_major(acc[:, :, cs], R_acc[sub], 512, lng2, lnb2, R_ln2, tAm, R_tAm, tBm, R_tBm, meanm, rstdm, R_st2)
                    sc.dma('sp', lambda e: e.dma_start(out=XTv2[:, :, t0:t0 + 512], in_=acc[:, :, cs]), [R_acc[sub]], [R_XT[t0 // 512]])

                def m_group(tm):
                    for sub in range(NSUB):
                        m_sub_prep(tm, sub)
                    for e_ in range(NEXP):
                        m_expert(tm, e_, e_ % 2)
                    for sub in range(NSUB):
                        m_finish(tm, sub)

                for tm in range(NTM if MOE_GROUPS is None else MOE_GROUPS):
                    m_group(tm)
              sc.barrier()
        for l_ in range(n_layers):
            layer(l_)
            sc.new_epoch()
        with ExitStack() as ph:
          if DO_Z:
            zin = [sb("zin%d" % i, [128, KC, 128], F32, ph) for i in range(2)]; R_zin = [Res(), Res()]
            zst = [sb("zst%d" % i, [128, D], F32, ph) for i in range(2)]; R_zst = [Res(), Res()]
            def z_tile(t, b):
                sc.dma('sp', lambda e: e.dma_start(out=zin[b][:], in_=XT.rearrange("(k p) s -> p k s", p=128)[:, :, t * 128:(t + 1) * 128]), [R_XT[t // 4]], [R_zin[b]])
                for g in range(4):
                    pi = nextps()
                    for j in range(4):
                        dc = g * 4 + j
                        sc.op('pe', lambda e, pi=pi, j=j, dc=dc: e.transpose(out=psum[pi][:, j * 128:(j + 1) * 128], in_=zin[b][:, dc, :], identity=ident[:]), [R_zin[b], R_ident], [R_ps[pi]])
                    if g % 2 == 0:
                        sc.op('act', lambda e, pi=pi, g=g: e.copy(out=zst[b][:, g * 512:(g + 1) * 512], in_=psum[pi][:]), [R_ps[pi]], [R_zst[b]])
                    else:
                        sc.op('dve', lambda e, pi=pi, g=g: e.tensor_copy(out=zst[b][:, g * 512:(g + 1) * 512], in_=psum[pi][:]), [R_ps[pi]], [R_zst[b]])
                sc.dma('pool', lambda e: e.dma_start(out=Y[t * 128:(t + 1) * 128, :], in_=zst[b][:]), [R_zst[b]], [R_Y])
            for t in range(S // 128):
                z_tile(t, t % 2)
        sc.emit()
    return nc


def kernel(**inputs):
    x = np.ascontiguousarray(inputs["x"], dtype=np.float32)
    nb = x.shape[0]
    nc = build(n_layers=4, debug=False)
    hc = host_consts()
    shared = {k: np.ascontiguousarray(v, dtype=np.float32) for k, v in inputs.items() if k not in ("x", "c")}
    in_maps = []
    for b in range(nb):
        m = {"x": x[b], "c": np.ascontiguousarray(inputs["c"][b], dtype=np.float32)}
        m.update(shared); m.update(hc)
        in_maps.append(m)
    res = run_bass_kernel_spmd(nc, in_maps, core_ids=list(range(nb)))
    return np.stack([np.asarray(r["y"], dtype=np.float32) for r in res.results], axis=0)
```

```python
import numpy as np
from contextlib import ExitStack
import concourse.bass as bass
import concourse.mybir as mybir
from concourse.bass_utils import run_bass_kernel_spmd

F32 = mybir.dt.float32
BF16 = mybir.dt.bfloat16
AF = mybir.ActivationFunctionType
ALU = mybir.AluOpType
AX = mybir.AxisListType

ENGS = ('pe', 'act', 'dve', 'pool', 'sp')
NDMASEM = 24


class Res:
    __slots__ = ('w', 'r', 'name', 'excl')

    def __init__(self, name='', excl=False):
        self.w = None
        self.r = {}
        self.name = name
        self.excl = excl


class Sched:
    def __init__(self, nc):
        self.nc = nc
        self.ops = {e: [] for e in ENGS}
        self.cnt = {e: 0 for e in ENGS}
        self.known = {e: {} for e in ENGS}
        self.dcnt = [0] * NDMASEM
        self.dnext = 0
        self.dlast = [None] * NDMASEM
        self.epoch = 0

    def _need(self, eng, tok, waits, isdma=False):
        if tok is None:
            return
        k, v = tok
        if k[0] != 'd' and k[1] < self.epoch:
            return
        if k[0] != 'd' and k[0] == eng and not isdma:
            return
        if self.known[eng].get(k, 0) >= v:
            return
        self.known[eng][k] = v
        waits.append((k, v))

    def _deps(self, eng, reads, writes, isdma=False):
        waits = []
        for r in reads:
            self._need(eng, r.w, waits, isdma or eng != 'pe')
        for w in writes:
            self._need(eng, w.w, waits, isdma)
            for k, v in w.r.items():
                self._need(eng, (k, v), waits, isdma)
        return waits

    def _commit(self, tok, reads, writes):
        k, v = tok
        for r in reads:
            if r.r.get(k, 0) < v:
                r.r[k] = v
        for w in writes:
            w.w = tok
            w.r = {}

    def op(self, eng, fn, reads=(), writes=()):
        ex = [r for r in reads if r.excl]
        if ex:
            reads = [r for r in reads if not r.excl]
            writes = list(writes) + [r for r in ex if r not in writes]
        waits = self._deps(eng, reads, writes)
        self.cnt[eng] += 1
        tok = ((eng, self.epoch), self.cnt[eng])
        self.ops[eng].append((waits, fn, (eng, self.epoch), 1))
        self._commit(tok, reads, writes)

    def dma(self, q, fn, reads=(), writes=()):
        waits = self._deps(q, reads, writes, True)
        i = self.dnext
        self.dnext = (self.dnext + 1) % NDMASEM
        self._need(q, self.dlast[i], waits)
        self.dcnt[i] += 16
        tok = (('d', i), self.dcnt[i])
        self.dlast[i] = tok
        self.ops[q].append((waits, fn, ('d', i), 16))
        self._commit(tok, reads, writes)

    def barrier(self):
        toks = [((e, self.epoch), self.cnt[e]) for e in ENGS if self.cnt[e] > 0]
        toks += [t for t in self.dlast if t is not None]
        for e in ENGS:
            waits = []
            for t in toks:
                self._need(e, t, waits)
            if waits:
                self.ops[e].append((waits, None, None, 0))

    def new_epoch(self):
        self.barrier()
        self.epoch += 1
        self.cnt = {e: 0 for e in ENGS}

    def emit(self):
        nc = self.nc
        with ExitStack() as st:
            sems = {}
            for ep in range(self.epoch + 1):
                for e in ENGS:
                    sems[(e, ep)] = st.enter_context(nc.semaphore('s_%s_%d' % (e, ep)))
            for i in range(NDMASEM):
                sems[('d', i)] = st.enter_context(nc.semaphore('s_d%d' % i))
            block = st.enter_context(nc.Block())
            self.barrier()

            def runner(e):
                def f(eng):
                    for waits, fn, inc, amt in self.ops[e]:
                        for k, v in waits:
                            eng.wait_ge(sems[k], v)
                        if fn is not None:
                            ins = fn(eng)
                            ins.then_inc(sems[inc], amt)
                return f
            block.tensor(runner('pe'))
            block.scalar(runner('act'))
            block.vector(runner('dve'))
            block.gpsimd(runner('pool'))
            block.sync(runner('sp'))


D = 2048; S = 4096; KC = 16; NTG = 8; TG = 512
IN_COLS = 13312
OFF_A = 0; OFF_C = 2304; OFF_B = 4608; OFF_G = 7168
TVLEN = 3072; HKW = 2944

def t5b(n):
    n = np.maximum(n, 0)
    nf = np.maximum(n, 1).astype(np.float32)
    large = 16 + (np.log(nf / np.float32(16)) / np.float32(np.log(128.0)) * np.float32(16)).astype(np.int32)
    large = np.minimum(large, 31)
    return np.where(n < 16, n, large)

NCONST = int(np.max(np.nonzero(t5b(np.arange(5000)) < 31)[0])) + 1

def host_consts():
    oh = np.zeros((4, 33, TVLEN), np.float32)
    m = np.arange(TVLEN); dist = m - 511
    for tab, (span, dil) in enumerate([(None, 1), (128, 1), (128, 4), (128, 16)]):
        valid = dist >= 0 if span is None else ((dist >= 0) & (dist <= span))
        bk = t5b(np.maximum(dist, 0) * dil)
        oh[tab, bk[valid], m[valid]] = 1.0
        oh[tab, 32, m[~valid]] = 1.0
    mc = np.zeros((3, 16, 16), np.float32)
    for qb in range(16):
        for j in range(16):
            mc[0, qb, j] = 0.0 if j < qb else -1e30
            mc[1, qb, j] = 1.0 if j < qb else 0.0
            mc[2, qb, j] = 1.0 if j == qb else 0.0
    sel = np.zeros((16, 16, 128), np.float32)
    for j in range(16):
        sel[j, j, :] = 1.0
    C = 64
    mus = np.triu(np.ones((C, C), np.float32), 1); mls = np.tril(np.ones((C, C), np.float32), -1); mui = np.triu(np.ones((C, C), np.float32), 0)
    rm = np.stack([np.stack([m] * 4, 0) for m in (-mus, -mls, mui, np.eye(C, dtype=np.float32))], 0)
    rm = rm.transpose(2, 0, 1, 3).reshape(C, 4 * 4 * C).copy()
    return {"rwmask": rm, "ohtab": oh, "negrow": np.full((1, 24), -30000.0, np.float32), "mconst": mc,
            "selc": sel.reshape(16, 2048), "jflip": np.eye(128, dtype=np.float32)[::-1].copy(), "ident": np.eye(128, dtype=np.float32)}


def build(n_layers=1, debug=True, NH_A=12, NH_C=4, DO_F=True, DO_E=True, RW_BLOCKS=None, RW_STAGE=3, DO_X0=True, DO_CD=True, DO_Z=True, DO_G=True, NEXP=64, MOE_GROUPS=None):
    nc = bass.Bass("TRN2", target_bir_lowering=False)
    sc = Sched(nc)
    def din(name, shape, dt=F32):
        return nc.dram_tensor(name, list(shape), dt, kind="ExternalInput").ap()
    def dscr(name, shape, dt):
        return nc.dram_tensor(name, list(shape), dt, kind="ExternalOutput" if debug else "Internal").ap()
    if DO_X0:
        x = din("x", [S, D]); c = din("c", [D])
        w_in = din("w_in", [4, D, IN_COLS]); w_ada = din("w_ada", [4, D, 6 * D]); b_ada = din("b_ada", [4, 6 * D])
    ident_d = din("ident", [128, 128])
    XT = dscr("XT", [D, S], F32); R_XT = [Res() for _ in range(NTG)]
    PROJ = dscr("PROJ", [IN_COLS, S], BF16) if DO_X0 else din("PROJ", [IN_COLS, S], BF16); R_PROJ = Res()
    p_a = din("p_a", [4, 768, D]); p_b = din("p_b", [4, 768, D]); p_c = din("p_c", [4, 256, D]);
    rwkv_v0 = din("rwkv_v0", [3, 768]); rwkv_mv_down = din("rwkv_mv_down", [3, 768, 32]); rwkv_mv_up = din("rwkv_mv_up", [3, 32, 768])
    ln2_g = din("ln2_g", [4, D]); ln2_b = din("ln2_b", [4, D])
    router_grp_w = din("router_grp_w", [4, D, 8]); router_grp_b = din("router_grp_b", [4, 8]); router_exp_w = din("router_exp_w", [4, D, 64]); router_exp_b = din("router_exp_b", [4, 64])
    exp_w_gate = din("exp_w_gate", [4, 64, D, 256]); exp_w_up = din("exp_w_up", [4, 64, D, 256]); exp_w_down = din("exp_w_down", [4, 64, 256, D])
    GW = dscr("GW", [64, S], F32); R_GW = Res(); w_o = din("w_o", [4, D, D])
    ln1_g = din("ln1_g", [4, D]); ln1_b = din("ln1_b", [4, D])
    rwkv_mu = din("rwkv_mu", [4, 2560]); rwkv_w0 = din("rwkv_w0", [4, 768]); rwkv_w_up = din("rwkv_w_up", [4, 64, 768])
    rwkv_a0 = din("rwkv_a0", [4, 768]); rwkv_a_up = din("rwkv_a_up", [4, 64, 768]); rwkv_g_up = din("rwkv_g_up", [4, 128, 768])
    rwkv_k_k = din("rwkv_k_k", [4, 768]); rwkv_k_a = din("rwkv_k_a", [4, 768]); rwkv_r_k = din("rwkv_r_k", [4, 768])
    rwkv_ln_g = din("rwkv_ln_g", [4, 768]); rwkv_ln_b = din("rwkv_ln_b", [4, 768])
    rwmask = din("rwmask", [64, 4 * 4 * 64])
    OB = dscr("OB", [768, S], BF16); R_OB = Res()
    VF = dscr("VF", [768, S], F32); R_VF = Res()
    rel_bias = din("rel_bias", [32, 24]); ohtab = din("ohtab", [4, 33, TVLEN]); negrow = din("negrow", [1, 24])
    mconst = din("mconst", [3, 16, 16]); selc = din("selc", [16, 16 * 128]); jflip = din("jflip", [128, 128])
    TV = dscr("TV", [4 * 24 * TVLEN], BF16); R_TV = Res()
    OA = dscr("OA", [768, S], BF16); R_OA = Res()
    OC = dscr("OC", [256, S], BF16); R_OC = Res()
    Y = nc.dram_tensor("y", [S, D], F32, kind="ExternalOutput").ap(); R_Y = Res()
    uid = [0]
    with ExitStack() as glob:
        def sb(name, shape, dt, st=glob):
            uid[0] += 1
            return st.enter_context(nc.sbuf_tensor("sb%d_%s" % (uid[0], name), list(shape), dt))
        psum = [glob.enter_context(nc.psum_tensor("ps%d" % i, [128, 512], F32)) for i in range(7)]
        R_ps = [Res(excl=True) for _ in range(8)]
        ident = sb("ident", [128, 128], F32); R_ident = Res()
        identb = sb("identb", [128, 128], BF16)
        sc.dma('sp', lambda e: e.dma_start(out=ident[:], in_=ident_d[:, :]), [], [R_ident])
        sc.op('dve', lambda e: e.tensor_copy(out=identb[:], in_=ident[:]), [R_ident], [R_ident])
        condT = sb("condT", [128, KC], F32); R_cond = Res()
        if DO_X0:
            sc.dma('sp', lambda e: e.dma_start(out=condT[:], in_=c.rearrange("(k p) -> p k", p=128), allow_slow_non_contiguous=True), [], [R_cond])
            sc.op('act', lambda e: e.activation(out=condT[:], in_=condT[:], func=AF.Silu), [R_cond], [R_cond])
        modT = sb("modT", [128, 96], F32); R_mod = Res()
        pscnt = [0]
        def nextps():
            i = pscnt[0] % 7; pscnt[0] += 1
            return i
        psT_b = glob.enter_context(nc.psum_tensor("psTb", [128, 1024], BF16));
        MC = sb("MC", [128, 3, 16, 16], F32); SEL = sb("SEL", [16, 16, 128], BF16); JF = sb("JF", [128, 128], BF16)
        ones_b = sb("ones_b", [128, 128], BF16); ones_f = sb("ones_f", [128, 128], F32); CB = sb("CB", [128, 24], F32)
        rb33 = sb("rb33", [33, 24], F32); R_MC = Res()
        sc.dma('sp', lambda e: e.dma_start(out=MC[:].rearrange("p a b c -> p (a b c)"), in_=bass.AP(mconst.tensor, 0, [[0, 128], [1, 768]])), [], [R_MC])
        sc.dma('pool', lambda e: e.dma_start(out=SEL[:].rearrange("p a b -> p (a b)"), in_=selc[:, :]), [], [R_MC])
        sc.dma('pool', lambda e: e.dma_start(out=JF[:], in_=jflip[:, :]), [], [R_MC])
        sc.dma('sp', lambda e: e.dma_start(out=CB[:], in_=bass.AP(rel_bias.tensor, 31 * 24, [[0, 128], [1, 24]])), [], [R_MC])
        sc.dma('sp', lambda e: e.dma_start(out=rb33[0:32, :], in_=rel_bias[:, :]), [], [R_MC])
        sc.dma('sp', lambda e: e.dma_start(out=rb33[32:33, :], in_=negrow[:, :]), [], [R_MC])
        sc.op('dve', lambda e: e.memset(ones_b[:], 1.0), [], [R_MC])
        sc.op('dve', lambda e: e.memset(ones_f[:], 1.0), [], [R_MC])
        with ExitStack() as ph:
            oh = sb("oh", [33, TVLEN], F32, ph); R_oh = Res()
            tvs = sb("tvs", [24, TVLEN], BF16, ph); R_tvs = Res()
            for tab in range(4):
                sc.dma('sp', lambda e, tab=tab: e.dma_start(out=oh[:], in_=ohtab[tab]), [], [R_oh])
                for ch in range(TVLEN // 512):
                    pi = nextps()
                    sc.op('pe', lambda e, pi=pi, ch=ch: e.matmul(psum[pi][0:24, :], lhsT=rb33[:], rhs=oh[:, ch * 512:(ch + 1) * 512], start=True, stop=True), [R_oh, R_MC], [R_ps[pi]])
                    sc.op('act', lambda e, pi=pi, ch=ch: e.mul(out=tvs[:, ch * 512:(ch + 1) * 512], in_=psum[pi][0:24, :], mul=8.0), [R_ps[pi]], [R_tvs])
                sc.dma('sp', lambda e, tab=tab: e.dma_start(out=bass.AP(TV.tensor, tab * 24 * TVLEN, [[TVLEN, 24], [1, TVLEN]]), in_=tvs[:]), [R_tvs], [R_TV])
        sc.barrier()

        with ExitStack() as ph:
          if DO_X0:
            xin = [sb("xin%d" % i, [128, D], F32, ph) for i in range(2)]; R_xin = [Res(), Res()]
            xst = [sb("xst%d" % i, [128, KC, 128], F32, ph) for i in range(2)]; R_xst = [Res(), Res()]
            for t in range(S // 128):
                b = t % 2
                sc.dma('sp', lambda e, t=t, b=b: e.dma_start(out=xin[b][:], in_=x[t * 128:(t + 1) * 128, :]), [], [R_xin[b]])
                for g in range(4):
                    pi = nextps()
                    for j in range(4):
                        dc = g * 4 + j
                        sc.op('pe', lambda e, pi=pi, j=j, dc=dc, b=b: e.transpose(out=psum[pi][:, j * 128:(j + 1) * 128], in_=xin[b][:, dc * 128:(dc + 1) * 128], identity=ident[:]),
                              [R_xin[b], R_ident], [R_ps[pi]])
                    eng = 'act' if g % 2 == 0 else 'dve'
                    if eng == 'act':
                        sc.op('act', lambda e, pi=pi, g=g, b=b: e.copy(out=xst[b][:, g * 4:(g + 1) * 4, :], in_=psum[pi][:].rearrange("p (j t) -> p j t", j=4)), [R_ps[pi]], [R_xst[b]])
                    else:
                        sc.op('dve', lambda e, pi=pi, g=g, b=b: e.tensor_copy(out=xst[b][:, g * 4:(g + 1) * 4, :], in_=psum[pi][:].rearrange("p (j t) -> p j t", j=4)), [R_ps[pi]], [R_xst[b]])
                sc.dma('pool', lambda e, t=t, b=b: e.dma_start(out=XT.rearrange("(k p) s -> p k s", p=128)[:, :, t * 128:(t + 1) * 128], in_=xst[b][:]), [R_xst[b]], [R_XT[t // 4]])
        sc.barrier()

        def ln_feature_major(xy, R_xy, W, lng, lnb, R_ln, tA, R_tA, tB, R_tB, mean, rstd, R_st):
            ps_s = nextps(); ps_q = nextps()
            for dc in range(KC):
                def one(dc):
                    i = dc % 2
                    sc.op('act', lambda e: e.activation(out=tA[i][:, 0:W], in_=xy[:, dc, :], func=AF.Square), [R_xy], [R_tA[i]])
                    sc.op('pe', lambda e: e.matmul(psum[ps_s][:, 0:W], lhsT=ones_f[:], rhs=xy[:, dc, :], start=(dc == 0), stop=(dc == KC - 1)), [R_xy, R_MC], [R_ps[ps_s]])
                    sc.op('pe', lambda e: e.matmul(psum[ps_q][:, 0:W], lhsT=ones_f[:], rhs=tA[i][:, 0:W], start=(dc == 0), stop=(dc == KC - 1)), [R_tA[i], R_MC], [R_ps[ps_q]])
                one(dc)
            sc.op('act', lambda e: e.mul(out=mean[:, 0:W], in_=psum[ps_s][:, 0:W], mul=1.0 / D), [R_ps[ps_s]], [R_st])
            sc.op('act', lambda e: e.mul(out=rstd[:, 0:W], in_=psum[ps_q][:, 0:W], mul=1.0 / D), [R_ps[ps_q]], [R_st])
            sc.op('dve', lambda e: e.tensor_tensor(out=tB[0][:, 0:W], in0=mean[:, 0:W], in1=mean[:, 0:W], op=ALU.mult), [R_st], [R_tB[0]])
            sc.op('dve', lambda e: e.tensor_tensor(out=rstd[:, 0:W], in0=rstd[:, 0:W], in1=tB[0][:, 0:W], op=ALU.subtract), [R_st, R_tB[0]], [R_st])
            sc.op('dve', lambda e: e.tensor_scalar(out=rstd[:, 0:W], in0=rstd[:, 0:W], scalar1=1e-5, scalar2=None, op0=ALU.add), [R_st], [R_st])
            sc.op('act', lambda e: e.activation(out=rstd[:, 0:W], in_=rstd[:, 0:W], func=AF.Sqrt), [R_st], [R_st])
            sc.op('dve', lambda e: e.reciprocal(out=rstd[:, 0:W], in_=rstd[:, 0:W]), [R_st], [R_st])
            for dc in range(KC):
                def two(dc):
                    i = dc % 2
                    sc.op('dve', lambda e: e.tensor_tensor(out=tA[i][:, 0:W], in0=xy[:, dc, :], in1=mean[:, 0:W], op=ALU.subtract), [R_xy, R_st], [R_tA[i]])
                    sc.op('pool', lambda e: e.tensor_tensor(out=tA[i][:, 0:W], in0=tA[i][:, 0:W], in1=rstd[:, 0:W], op=ALU.mult), [R_tA[i], R_st], [R_tA[i]])
                    sc.op('act', lambda e: e.activation(out=xy[:, dc, :], in_=tA[i][:, 0:W], func=AF.Identity, bias=lnb[:, dc:dc + 1], scale=lng[:, dc:dc + 1]), [R_tA[i], R_ln, R_xy], [R_xy])
                two(dc)

        def layer(l):
            with ExitStack() as ph:
              if DO_X0:
                wa = [sb("wa%d" % i, [128, KC, 512], F32, ph) for i in range(2)]; R_wa = [Res(), Res()]
                bT = sb("bT", [128, 96], F32, ph); R_bT = Res()
                with nc.allow_non_contiguous_dma(reason="small"):
                    sc.dma('sp', lambda e: e.dma_start(out=bT[:], in_=b_ada[l].rearrange("(k p) -> p k", p=128), allow_slow_non_contiguous=True), [], [R_bT])
                pi = nextps()
                for cg in range(24):
                    b = cg % 2
                    sc.dma('sp' if cg % 2 == 0 else 'pool', lambda e, cg=cg, b=b: e.dma_start(out=wa[b][:], in_=w_ada[l].rearrange("(k p) n -> p k n", p=128)[:, :, cg * 512:(cg + 1) * 512]), [], [R_wa[b]])
                    for j in range(4):
                        cc = cg * 4 + j
                        for k in range(KC):
                            sc.op('pe', lambda e, cc=cc, k=k, b=b, j=j: e.matmul(psum[pi][:, cc:cc + 1], lhsT=wa[b][:, k, j * 128:(j + 1) * 128], rhs=condT[:, k:k + 1], start=(k == 0), stop=(k == KC - 1)),
                                  [R_wa[b], R_cond], [R_ps[pi]])
                sc.op('dve', lambda e: e.tensor_tensor(out=modT[:], in0=psum[pi][:, 0:96], in1=bT[:], op=ALU.add), [R_ps[pi], R_bT], [R_mod])
            sc.barrier()
            with ExitStack() as ph:
              if DO_X0:
                hT = sb("hT", [128, KC, S], BF16, ph); R_hT = [Res() for _ in range(NTG)]
                sc1p = sb("sc1p", [128, KC], F32, ph); R_sc1p = Res()
                sc.op('dve', lambda e: e.tensor_scalar(out=sc1p[:], in0=modT[:, 16:32], scalar1=1.0, scalar2=None, op0=ALU.add), [R_mod], [R_sc1p])
                xt = [sb("xt%d" % i, [128, 4, TG], F32, ph) for i in range(2)]; R_xt = [Res(), Res()]
                n = 0
                for tg in range(NTG):
                    for q in range(4):
                        b = n % 2; n += 1
                        sc.dma('sp', lambda e, tg=tg, q=q, b=b: e.dma_start(out=xt[b][:], in_=XT.rearrange("(k p) s -> p k s", p=128)[:, q * 4:(q + 1) * 4, tg * TG:(tg + 1) * TG]), [R_XT[tg]], [R_xt[b]])
                        for j in range(4):
                            dc = q * 4 + j
                            sc.op('dve', lambda e, tg=tg, dc=dc, j=j, b=b: e.tensor_scalar(out=hT[:, dc, tg * TG:(tg + 1) * TG], in0=xt[b][:, j, :], scalar1=sc1p[:, dc:dc + 1], scalar2=modT[:, dc:dc + 1], op0=ALU.mult, op1=ALU.add),
                                  [R_xt[b], R_sc1p, R_mod], [R_hT[tg]])
                wt = [sb("wt%d" % i, [128, KC, 256], BF16, ph) for i in range(2)]; R_wt = [Res(), Res()]
                ost = [sb("ost%d" % i, [128, TG], BF16, ph) for i in range(4)]; R_ost = [Res() for _ in range(4)]
                no = 0
                for cp in range(IN_COLS // 256):
                    b = cp % 2
                    sc.dma('pool', lambda e, cp=cp, b=b: e.dma_start(out=wt[b][:], in_=w_in[l].rearrange("(k p) n -> p k n", p=128)[:, :, cp * 256:(cp + 1) * 256]), [], [R_wt[b]])
                    for j in range(2):
                        col0 = cp * 256 + j * 128
                        isgate = col0 >= OFF_G
                        for tg in range(NTG):
                            pi = nextps()
                            for k in range(KC):
                                sc.op('pe', lambda e, pi=pi, k=k, b=b, j=j, tg=tg: e.matmul(psum[pi][:], lhsT=wt[b][:, k, j * 128:(j + 1) * 128], rhs=hT[:, k, tg * TG:(tg + 1) * TG], start=(k == 0), stop=(k == KC - 1)),
                                      [R_wt[b], R_hT[tg]], [R_ps[pi]])
                            ob = no % 4; no += 1
                            if isgate:
                                sc.op('act', lambda e, pi=pi, ob=ob: e.activation(out=ost[ob][:], in_=psum[pi][:], func=AF.Sigmoid), [R_ps[pi]], [R_ost[ob]])
                            elif no % 2 == 0:
                                sc.op('act', lambda e, pi=pi, ob=ob: e.copy(out=ost[ob][:], in_=psum[pi][:]), [R_ps[pi]], [R_ost[ob]])
                            else:
                                sc.op('dve', lambda e, pi=pi, ob=ob: e.tensor_copy(out=ost[ob][:], in_=psum[pi][:]), [R_ps[pi]], [R_ost[ob]])
                            sc.dma('sp', lambda e, col0=col0, tg=tg, ob=ob: e.dma_start(out=PROJ[col0:col0 + 128, tg * TG:(tg + 1) * TG], in_=ost[ob][:]), [R_ost[ob]], [R_PROJ])
            sc.barrier()
            with ExitStack() as ph:
              if DO_CD:
                QT = [sb("QT%d" % i, [64, S], BF16, ph) for i in range(2)]
                KT = [sb("KT%d" % i, [64, S], BF16, ph) for i in range(2)]
                VT = [sb("VT%d" % i, [64, S], BF16, ph) for i in range(2)]
                Vt = [sb("Vt%d" % i, [128, 32, 64], BF16, ph) for i in range(2)]
                HK = [sb("HK%d" % i, [128, HKW], BF16, ph) for i in range(2)]
                R_hd = [Res(), Res()]; R_Vt = [Res(), Res()]; R_HK = [Res(), Res()]
                kmf = sb("kmf", [64, 16], F32, ph); kmhi = sb("kmhi", [64, 16], BF16, ph); kmlo = sb("kmlo", [64, 16], BF16, ph); R_km = Res()
                maskT = [sb("maskT%d" % i, [16, 512], BF16, ph) for i in range(2)]; R_maskT = [Res(), Res()]
                gm = sb("gm", [128, 16], F32, ph); top8 = sb("top8", [128, 8], F32, ph); R_gm = Res()
                PT = [sb("PT%d" % i, [128, 512], BF16, ph) for i in range(3)]; R_PT = [Res() for _ in range(3)]
                rl = sb("rl", [64, 512], F32, ph); R_rl = Res()
                ostg = [sb("ostg%d" % i, [64, 512], BF16, ph) for i in range(2)]; R_ostg = [Res(), Res()]
                npt = [0]; nsb = [0]

                def load_head(b, qrow, krow, vrow, tab, hidx):
                    for (T_, row) in ((QT, qrow), (KT, krow), (VT, vrow)):
                        sc.dma('sp', lambda e, T_=T_, row=row: e.dma_start(out=T_[b][:], in_=PROJ[row:row + 64, :]), [R_PROJ], [R_hd[b]])
                    off = (tab * 24 + hidx) * TVLEN
                    sc.dma('sp', lambda e: e.dma_start(out=HK[b][:], in_=bass.AP(TV.tensor, off, [[1, 128], [1, HKW]])), [R_TV], [R_HK[b]])

                def make_V(b, src, R_src):
                    for g4 in range(8):
                        for j in range(4):
                            t = g4 * 4 + j
                            sc.op('pe', lambda e, t=t, j=j: e.transpose(out=psT_b[:, j * 64:(j + 1) * 64], in_=src[:, t * 128:(t + 1) * 128], identity=identb[0:64, 0:64]), [R_src, R_ident], [R_ps[7]])
                        sc.op('dve', lambda e, g4=g4: e.tensor_copy(out=Vt[b][:, g4 * 4:(g4 + 1) * 4, :], in_=psT_b[:, 0:256].rearrange("p (j d) -> p j d", j=4)), [R_ps[7]], [R_Vt[b]])

                def moba_gate(b, mb, qt, t):
                    qb = qt // 2
                    sc.op('pe', lambda e: e.matmul(psum[6][:, 0:16], lhsT=QT[b][:, qt * 128:(qt + 1) * 128], rhs=kmhi[:], start=True, stop=False), [R_hd[b], R_km], [R_ps[6]])
                    sc.op('pe', lambda e: e.matmul(psum[6][:, 0:16], lhsT=QT[b][:, qt * 128:(qt + 1) * 128], rhs=kmlo[:], start=False, stop=True), [R_hd[b], R_km], [R_ps[6]])
                    sc.op('dve', lambda e: e.tensor_tensor(out=gm[:], in0=psum[6][:, 0:16], in1=MC[:, 0, qb, :], op=ALU.add), [R_ps[6], R_MC], [R_gm])
                    sc.op('dve', lambda e: e.max(out=top8[:], in_=gm[:]), [R_gm], [R_gm])
                    sc.op('dve', lambda e: e.tensor_scalar(out=gm[:], in0=gm[:], scalar1=top8[:, 2:3], scalar2=None, op0=ALU.is_ge), [R_gm], [R_gm])
                    sc.op('dve', lambda e: e.tensor_tensor(out=gm[:], in0=gm[:], in1=MC[:, 1, qb, :], op=ALU.mult), [R_gm, R_MC], [R_gm])
                    sc.op('dve', lambda e: e.tensor_tensor(out=gm[:], in0=gm[:], in1=MC[:, 2, qb, :], op=ALU.add), [R_gm, R_MC], [R_gm])
                    sc.op('dve', lambda e: e.tensor_scalar(out=gm[:], in0=gm[:], scalar1=-1.0, scalar2=240000.0, op0=ALU.add, op1=ALU.mult), [R_gm], [R_gm])
                    sc.op('pe', lambda e: e.transpose(out=psum[6][0:16, 128:256], in_=gm[:], identity=ident[:]), [R_gm, R_ident], [R_ps[6]])
                    sc.op('act', lambda e: e.copy(out=maskT[mb][:, t * 128:(t + 1) * 128], in_=psum[6][0:16, 128:256]), [R_ps[6]], [R_maskT[mb]])

                def moba_tile(h, b, mb, QG, kt, nk, pO, pL):
                    D0 = QG * 512 - kt * 128
                    far = (D0 - 127) >= NCONST
                    pS = npt[0] % 3; pb = npt[0] % 3; npt[0] += 1
                    sc.op('pe', lambda e: e.matmul(psum[pS][:], lhsT=KT[b][:, kt * 128:(kt + 1) * 128], rhs=QT[b][:, QG * 512:(QG + 1) * 512], start=True, stop=False), [R_hd[b]], [R_ps[pS]])
                    sc.op('pe', lambda e: e.matmul(psum[pS][:], lhsT=SEL[:, kt // 2, :], rhs=maskT[mb][:], start=False, stop=far), [R_maskT[mb], R_MC], [R_ps[pS]])
                    if not far:
                        sc.op('pe', lambda e: e.matmul(psum[pS][:], lhsT=JF[:], rhs=HK[b][:, D0 + 384:D0 + 384 + 512], start=False, stop=True), [R_HK[b], R_MC], [R_ps[pS]])
                        sc.op('act', lambda e: e.activation(out=PT[pb][:], in_=psum[pS][:], func=AF.Exp, scale=0.125), [R_ps[pS]], [R_PT[pb]])
                    else:
                        sc.op('act', lambda e: e.activation(out=PT[pb][:], in_=psum[pS][:], func=AF.Exp, scale=0.125, bias=CB[:, h:h + 1]), [R_ps[pS], R_MC], [R_PT[pb]])
                    sc.op('pe', lambda e: e.matmul(psum[pO][0:64, :], lhsT=Vt[b][:, kt, :], rhs=PT[pb][:], start=(kt == 0), stop=(kt == nk - 1)), [R_Vt[b], R_PT[pb]], [R_ps[pO]])
                    sc.op('pe', lambda e: e.matmul(psum[pL][0:64, :], lhsT=ones_b[:, 0:64], rhs=PT[pb][:], start=(kt == 0), stop=(kt == nk - 1)), [R_PT[pb], R_MC], [R_ps[pL]])

                def moba_qg(h, b, QG):
                    mb = QG % 2
                    for t in range(4):
                        moba_gate(b, mb, QG * 4 + t, t)
                    pO = 3 + (QG % 2); pL = 5
                    nk = 4 * QG + 4
                    for kt in range(nk):
                        moba_tile(h, b, mb, QG, kt, nk, pO, pL)
                    ob = nsb[0] % 2; nsb[0] += 1
                    sc.op('dve', lambda e: e.reciprocal(out=rl[:], in_=psum[pL][0:64, :]), [R_ps[pL]], [R_rl])
                    sc.op('dve', lambda e: e.tensor_tensor(out=ostg[ob][:], in0=psum[pO][0:64, :], in1=rl[:], op=ALU.mult), [R_ps[pO], R_rl], [R_ostg[ob]])
                    sc.dma('sp', lambda e: e.dma_start(out=OA[h * 64:(h + 1) * 64, QG * 512:(QG + 1) * 512], in_=ostg[ob][:]), [R_ostg[ob]], [R_OA])

                def moba_head(h, b):
                    load_head(b, OFF_A + h * 64, OFF_A + 768 + h * 64, OFF_A + 1536 + h * 64, 0, h)
                    make_V(b, VT[b], R_hd[b])
                    sc.op('dve', lambda e: e.tensor_reduce(out=kmf[:], in_=KT[b][:].rearrange("p (j k) -> p j k", k=256), axis=AX.X, op=ALU.add), [R_hd[b]], [R_km])
                    sc.op('dve', lambda e: e.tensor_scalar(out=kmf[:], in0=kmf[:], scalar1=1.0 / 256, scalar2=None, op0=ALU.mult), [R_km], [R_km])
                    sc.op('dve', lambda e: e.tensor_copy(out=kmhi[:], in_=kmf[:]), [R_km], [R_km])
                    sc.op('dve', lambda e: e.tensor_tensor(out=kmlo[:], in0=kmf[:], in1=kmhi[:], op=ALU.subtract), [R_km], [R_km])
                    for QG in range(8):
                        moba_qg(h, b, QG)

                for h in range(NH_A):
                    moba_head(h, h % 2)

                QP = sb("QP", [64, S], BF16, ph); KP = sb("KP", [64, S], BF16, ph); VP = sb("VP", [64, S], BF16, ph); R_perm = Res()
                Og = sb("Og", [64, S], F32, ph); Lg = sb("Lg", [64, S], F32, ph); R_OL = Res()
                Os = sb("Os", [64, S], F32, ph); Ls = sb("Ls", [64, S], F32, ph); R_sum = Res()
                obig = sb("obig", [64, S], BF16, ph)

                def dil_qtile(b, qt, tps, g4, jj, srcQ, srcK, R_src, pO, pL):
                    kts = ([qt - 1] if qt % tps != 0 else []) + [qt]
                    for i, kt in enumerate(kts):
                        D0 = (qt - kt) * 128
                        pS = npt[0] % 3; pb = npt[0] % 3; npt[0] += 1
                        sc.op('pe', lambda e, kt=kt, pS=pS: e.matmul(psum[pS][:, 0:128], lhsT=srcK[:, kt * 128:(kt + 1) * 128], rhs=srcQ[:, qt * 128:(qt + 1) * 128], start=True, stop=False), [R_src], [R_ps[pS]])
                        sc.op('pe', lambda e, pS=pS, D0=D0: e.matmul(psum[pS][:, 0:128], lhsT=JF[:], rhs=HK[b][:, D0 + 384:D0 + 384 + 128], start=False, stop=True), [R_HK[b], R_MC], [R_ps[pS]])
                        sc.op('act', lambda e, pS=pS, pb=pb: e.activation(out=PT[pb][:, 0:128], in_=psum[pS][:, 0:128], func=AF.Exp, scale=0.125), [R_ps[pS]], [R_PT[pb]])
                        first = (i == 0) and (jj == 0)
                        sc.op('pe', lambda e, kt=kt, pb=pb, first=first: e.matmul(psum[pO][0:64, jj * 128:(jj + 1) * 128], lhsT=Vt[b][:, kt, :], rhs=PT[pb][:, 0:128], start=first, stop=(kt == qt)), [R_Vt[b], R_PT[pb]], [R_ps[pO]])
                        sc.op('pe', lambda e, kt=kt, pb=pb, first=first: e.matmul(psum[pL][0:64, jj * 128:(jj + 1) * 128], lhsT=ones_b[:, 0:64], rhs=PT[pb][:, 0:128], start=first, stop=(kt == qt)), [R_PT[pb], R_MC], [R_ps[pL]])

                def dil_head(g, j, b):
                    dil = (1, 4, 16)[g]
                    hc = g * 4 + j
                    load_head(b, OFF_C + hc * 64, OFF_C + 768 + hc * 64, OFF_C + 1536 + hc * 64, 1 + g, 12 + hc)
                    if dil > 1:
                        for (src, dst, eng) in ((QT[b], QP, 'pool'), (KT[b], KP, 'pool'), (VT[b], VP, 'act')):
                            if eng == 'pool':
                                sc.op('pool', lambda e, src=src, dst=dst: e.tensor_copy(out=dst[:].rearrange("p (r n) -> p r n", r=dil), in_=src[:].rearrange("p (n r) -> p r n", r=dil)), [R_hd[b]], [R_perm])
                            else:
                                sc.op('act', lambda e, src=src, dst=dst: e.copy(out=dst[:].rearrange("p (r n) -> p r n", r=dil), in_=src[:].rearrange("p (n r) -> p r n", r=dil)), [R_hd[b]], [R_perm])
                        sQ, sK, sV, R_src = QP, KP, VP, R_perm
                    else:
                        sQ, sK, sV, R_src = QT[b], KT[b], VT[b], R_hd[b]
                    make_V(b, sV, R_src)
                    tps = (S // dil) // 128
                    for g4 in range(8):
                        pO = 3 + (g4 % 2); pL = 5 + (g4 % 2)
                        for jj in range(4):
                            dil_qtile(b, g4 * 4 + jj, tps, g4, jj, sQ, sK, R_src, pO, pL)
                        sc.op('act', lambda e, g4=g4, pO=pO: e.copy(out=Og[:, g4 * 512:(g4 + 1) * 512], in_=psum[pO][0:64, :]), [R_ps[pO]], [R_OL])
                        sc.op('dve', lambda e, g4=g4, pL=pL: e.tensor_copy(out=Lg[:, g4 * 512:(g4 + 1) * 512], in_=psum[pL][0:64, :]), [R_ps[pL]], [R_OL])
                    if g == 0:
                        sc.op('dve', lambda e: e.tensor_copy(out=Os[:], in_=Og[:]), [R_OL], [R_sum])
                        sc.op('pool', lambda e: e.tensor_copy(out=Ls[:], in_=Lg[:]), [R_OL], [R_sum])
                    else:
                        sc.op('dve', lambda e: e.tensor_tensor(out=Os[:].rearrange("p (n r) -> p r n", r=dil), in0=Os[:].rearrange("p (n r) -> p r n", r=dil), in1=Og[:].rearrange("p (r n) -> p r n", r=dil), op=ALU.add), [R_OL], [R_sum])
                        sc.op('pool', lambda e: e.tensor_tensor(out=Ls[:].rearrange("p (n r) -> p r n", r=dil), in0=Ls[:].rearrange("p (n r) -> p r n", r=dil), in1=Lg[:].rearrange("p (r n) -> p r n", r=dil), op=ALU.add), [R_OL], [R_sum])

                def dil_slot(j):
                    for g in range(3):
                        dil_head(g, j, (j * 3 + g) % 2)
                    sc.op('dve', lambda e: e.reciprocal(out=Ls[:], in_=Ls[:]), [R_sum], [R_sum])
                    sc.op('dve', lambda e: e.tensor_tensor(out=obig[:], in0=Os[:], in1=Ls[:], op=ALU.mult), [R_sum], [R_sum])
                    sc.dma('sp', lambda e: e.dma_start(out=OC[j * 64:(j + 1) * 64, :], in_=obig[:]), [R_sum], [R_OC])

                for j in range(NH_C):
                    dil_slot(j)
            sc.barrier()
            if DO_E:
                RB = 256; NCH = 4; NBLK = S // RB
                with ExitStack() as ph:
                    def prm(name, src, n=12):
                        t = sb(name, [64, n], F32, ph)
                        sc.dma('sp', lambda e: e.dma_start(out=t[:], in_=src.rearrange("(h d) -> d h", d=64), allow_slow_non_contiguous=True), [], [R_prm])
                        return t
                    R_prm = Res()
                    mu_r = prm("mu_r", rwkv_mu[l, 0:768]); mu_k = prm("mu_k", rwkv_mu[l, 768:1536]); mu_v = prm("mu_v", rwkv_mu[l, 1536:2304])
                    mu_w = prm("mu_w", rwkv_mu[l, 2304:2368], 1); mu_a = prm("mu_a", rwkv_mu[l, 2368:2432], 1)
                    mu_g = sb("mu_g", [128, 1], F32, ph)
                    sc.dma('sp', lambda e: e.dma_start(out=mu_g[:], in_=rwkv_mu[l, 2432:2560].rearrange("(h d) -> d h", d=128), allow_slow_non_contiguous=True), [], [R_prm])
                    w0 = prm("w0", rwkv_w0[l]); a0 = prm("a0", rwkv_a0[l]); k_k = prm("k_k", rwkv_k_k[l]); k_a = prm("k_a", rwkv_k_a[l])
                    r_k = prm("r_k", rwkv_r_k[l]); ln_g = prm("ln_g", rwkv_ln_g[l]); ln_b = prm("ln_b", rwkv_ln_b[l])
                    omka = sb("omka", [64, 12], F32, ph)
                    sc.op('dve', lambda e: e.tensor_scalar(out=omka[:], in0=k_a[:], scalar1=-1.0, scalar2=1.0, op0=ALU.mult, op1=ALU.add), [R_prm], [R_prm])
                    wup = sb("wup", [64, 768], BF16, ph); aup = sb("aup", [64, 768], BF16, ph); gup = sb("gup", [128, 768], BF16, ph)
                    sc.dma('pool', lambda e: e.dma_start(out=wup[:], in_=rwkv_w_up[l]), [], [R_prm])
                    sc.dma('pool', lambda e: e.dma_start(out=aup[:], in_=rwkv_a_up[l]), [], [R_prm])
                    sc.dma('pool', lambda e: e.dma_start(out=gup[:], in_=rwkv_g_up[l]), [], [R_prm])
                    RM = sb("RM", [64, 4, 4, 64], F32, ph); I4 = sb("I4", [64, 4, 64], BF16, ph)
                    sc.dma('sp', lambda e: e.dma_start(out=RM[:].rearrange("p a b c -> p (a b c)"), in_=rwmask[:, :]), [], [R_prm])
                    sc.op('dve', lambda e: e.tensor_copy(out=I4[:], in_=RM[:, 3, :, :]), [R_prm], [R_prm])
                    QtT = sb("QtT", [64, 12, RB], BF16, ph); RtT = sb("RtT", [64, 12, RB], BF16, ph); KhT = sb("KhT", [64, 12, RB], BF16, ph)
                    BhT = sb("BhT", [64, 12, RB], BF16, ph); KbT = sb("KbT", [64, 12, RB], BF16, ph); BbT = sb("BbT", [64, 12, RB], BF16, ph)
                    VTb = sb("VTb", [64, 12, RB], BF16, ph); Gg = sb("Gg", [64, 12, RB], BF16, ph); BON = sb("BON", [64, 12, RB], BF16, ph)
                    GC = sb("GC", [64, 12, NCH], F32, ph); yT = sb("yT", [64, 12, RB], F32, ph)
                    R_opsH = [Res() for _ in range(12)]; R_yT = [Res() for _ in range(3)]
                    Pf = sb("Pf", [64, 12, 64], F32, ph); Pb = sb("Pb", [64, 12, 64], BF16, ph); R_P = [Res() for _ in range(3)]
                    sc.op('dve', lambda e: e.memset(Pf[:], 0.0), [], R_P)
                    sc.op('dve', lambda e: e.memset(Pb[:], 0.0), [], R_P)
                    zr = [sb("zr%d" % i, [64, 12, RB + 1], BF16, ph) for i in range(3)]; R_z = [Res() for _ in range(3)]
                    zlo = sb("zlo", [128, 2, RB + 1], BF16, ph); zgl = sb("zgl", [128, RB + 1], BF16, ph); R_zlo = Res()
                    wl = sb("wl", [64, RB], BF16, ph); al = sb("al", [64, RB], BF16, ph); gl = sb("gl", [128, RB], BF16, ph); R_lo = Res()
                    T = [sb("T%d" % i, [64, RB], F32, ph) for i in range(12)]; R_T = [Res() for _ in range(12)]
                    MZ = sb("MZ", [64, 4, 192], BF16, ph); R_MZ = Res()
                    SC = sb("SC", [64, 3, 4, 64], BF16, ph); R_SC = Res()
                    TM = sb("TM", [64, 4, 192], BF16, ph); R_TM = Res()
                    Xs = sb("Xs", [64, 4, 64], BF16, ph); Un = sb("Un", [64, 4, 64], BF16, ph); R_X = Res(); R_U = Res()
                    o1 = ones_f[0:64, 0:64]
                    pA, pB, pC, pD, pE, pF, pG = range(7)
                    ppre = [0]

                    def pre_ps():
                        i = ppre[0] % 2; ppre[0] += 1
                        return (pC, pE)[i]

                    def shift(eng, out, zt, hsl, mu_ap, tmp, R_src, R_tmp, R_out):
                        sc.op('dve', lambda e: e.tensor_tensor(out=tmp, in0=zt[hsl + (slice(0, RB),)], in1=zt[hsl + (slice(1, RB + 1),)], op=ALU.subtract), [R_src], [R_tmp])
                        sc.op(eng, lambda e: e.scalar_tensor_tensor(out=out, in0=tmp, scalar=mu_ap, in1=zt[hsl + (slice(1, RB + 1),)], op0=ALU.mult, op1=ALU.add), [R_src, R_tmp, R_prm], [R_out])

                    def load_block(blk):
                        t0 = blk * RB
                        for i, off in enumerate((OFF_B, OFF_B + 768, OFF_B + 1536)):
                            src = PROJ[off:off + 768, :].rearrange("(h d) s -> d h s", d=64)
                            if blk == 0:
                                sc.op('pool', lambda e, i=i: e.memset(zr[i][:, :, 0:1], 0.0), [], [R_z[i]])
                                sc.dma('sp', lambda e, i=i, src=src: e.dma_start(out=zr[i][:, :, 1:RB + 1], in_=src[:, :, 0:RB]), [R_PROJ], [R_z[i]])
                            else:
                                sc.dma('sp', lambda e, i=i, src=src: e.dma_start(out=zr[i][:], in_=src[:, :, t0 - 1:t0 + RB]), [R_PROJ], [R_z[i]])
                        lo0 = OFF_B + 2304
                        if blk == 0:
                            sc.op('pool', lambda e: e.memset(zlo[:, :, 0:1], 0.0), [], [R_zlo])
                            sc.op('pool', lambda e: e.memset(zgl[:, 0:1], 0.0), [], [R_zlo])
                            sc.dma('sp', lambda e: e.dma_start(out=zlo[0:64, 0, 1:RB + 1], in_=PROJ[lo0:lo0 + 64, 0:RB]), [R_PROJ], [R_zlo])
                            sc.dma('sp', lambda e: e.dma_start(out=zlo[0:64, 1, 1:RB + 1], in_=PROJ[lo0 + 64:lo0 + 128, 0:RB]), [R_PROJ], [R_zlo])
                            sc.dma('sp', lambda e: e.dma_start(out=zgl[:, 1:RB + 1], in_=PROJ[lo0 + 128:lo0 + 256, 0:RB]), [R_PROJ], [R_zlo])
                        else:
                            sc.dma('sp', lambda e: e.dma_start(out=zlo[0:64, 0, :], in_=PROJ[lo0:lo0 + 64, t0 - 1:t0 + RB]), [R_PROJ], [R_zlo])
                            sc.dma('sp', lambda e: e.dma_start(out=zlo[0:64, 1, :], in_=PROJ[lo0 + 64:lo0 + 128, t0 - 1:t0 + RB]), [R_PROJ], [R_zlo])
                            sc.dma('sp', lambda e: e.dma_start(out=zgl[:], in_=PROJ[lo0 + 128:lo0 + 256, t0 - 1:t0 + RB]), [R_PROJ], [R_zlo])
                        shift('dve', T[0][:], zlo, (slice(0, 64), 0), mu_w[:, 0:1], T[1][:], R_zlo, R_T[1], R_T[0])
                        sc.op('act', lambda e: e.activation(out=wl[:], in_=T[0][:], func=AF.Tanh), [R_T[0]], [R_lo])
                        shift('dve', al[:], zlo, (slice(0, 64), 1), mu_a[:, 0:1], T[1][:], R_zlo, R_T[1], R_lo)
                        sc.op('dve', lambda e: e.tensor_tensor(out=T128[:], in0=zgl[:, 0:RB], in1=zgl[:, 1:RB + 1], op=ALU.subtract), [R_zlo], [R_T128])
                        sc.op('dve', lambda e: e.scalar_tensor_tensor(out=T128[:], in0=T128[:], scalar=mu_g[:, 0:1], in1=zgl[:, 1:RB + 1], op0=ALU.mult, op1=ALU.add), [R_zlo, R_T128, R_prm], [R_T128])
                        sc.op('act', lambda e: e.activation(out=gl[:], in_=T128[:], func=AF.Sigmoid), [R_T128], [R_lo])

                    T128 = sb("T128", [128, RB], F32, ph); R_T128 = Res()

                    def pre_head(blk, h):
                        t0 = blk * RB
                        hs = slice(h * 64, (h + 1) * 64)
                        tr, tk, tv, ta, tld, tlg, tkap, tkt, tb, tx, ty, tz = T
                        Rr, Rk, Rv, Ra, Rld, Rlg, Rkap, Rkt, Rb, Rx, Ry, Rz = R_T
                        hsl = (slice(0, 64), h)
                        shift('dve', tr[:], zr[0], hsl, mu_r[:, h:h + 1], tx[:], R_z[0], Rx, Rr)
                        shift('dve', tk[:], zr[1], hsl, mu_k[:, h:h + 1], ty[:], R_z[1], Ry, Rk)
                        sc.op('pool', lambda e: e.tensor_copy(out=tv[:], in_=V32[:, h, :]), [R_V32], [Rv])
                        p1 = pre_ps()
                        sc.op('pe', lambda e: e.matmul(psum[p1][0:64, 256:512], lhsT=wup[:, hs], rhs=wl[:], start=True, stop=True), [R_lo, R_prm], [R_ps[p1]])
                        sc.op('act', lambda e: e.activation(out=tld[:], in_=psum[p1][0:64, 256:512], func=AF.Sigmoid, bias=w0[:, h:h + 1]), [R_ps[p1], R_prm], [Rld])
                        sc.op('dve', lambda e: e.tensor_scalar(out=tld[:], in0=tld[:], scalar1=-0.6065306597126334, scalar2=None, op0=ALU.mult), [Rld], [Rld])
                        p2 = pre_ps()
                        sc.op('pe', lambda e: e.matmul(psum[p2][0:64, 256:512], lhsT=aup[:, hs], rhs=al[:], start=True, stop=True), [R_lo, R_prm], [R_ps[p2]])
                        sc.op('act', lambda e: e.activation(out=ta[:], in_=psum[p2][0:64, 256:512], func=AF.Sigmoid, bias=a0[:, h:h + 1]), [R_ps[p2], R_prm], [Ra])
                        p3 = pre_ps()
                        sc.op('pe', lambda e: e.matmul(psum[p3][0:64, 256:512], lhsT=gup[:, hs], rhs=gl[:], start=True, stop=True), [R_lo, R_prm], [R_ps[p3]])
                        sc.op('act', lambda e: e.copy(out=Gg[:, h, :], in_=psum[p3][0:64, 256:512]), [R_ps[p3]], [R_opsH[h]])
                        sc.op('dve', lambda e: e.tensor_scalar(out=tkap[:], in0=tk[:], scalar1=k_k[:, h:h + 1], scalar2=None, op0=ALU.mult), [Rk, R_prm], [Rkap])
                        sc.op('act', lambda e: e.activation(out=tx[:], in_=tkap[:], func=AF.Square), [Rkap], [Rx])
                        p4 = pre_ps()
                        sc.op('pe', lambda e: e.matmul(psum[p4][0:64, 256:512], lhsT=o1, rhs=tx[:], start=True, stop=True), [Rx, R_MC], [R_ps[p4]])
                        sc.op('act', lambda e: e.activation(out=tx[:], in_=psum[p4][0:64, 256:512], func=AF.Sqrt), [R_ps[p4]], [Rx])
                        sc.op('dve', lambda e: e.tensor_scalar(out=tx[:], in0=tx[:], scalar1=1e-12, scalar2=None, op0=ALU.max), [Rx], [Rx])
                        sc.op('dve', lambda e: e.reciprocal(out=tx[:], in_=tx[:]), [Rx], [Rx])
                        sc.op('dve', lambda e: e.tensor_tensor(out=tkap[:], in0=tkap[:], in1=tx[:], op=ALU.mult), [Rkap, Rx], [Rkap])
                        sc.op('dve', lambda e: e.tensor_scalar(out=ty[:], in0=ta[:], scalar1=k_a[:, h:h + 1], scalar2=omka[:, h:h + 1], op0=ALU.mult, op1=ALU.add), [Ra, R_prm], [Ry])
                        sc.op('pool', lambda e: e.tensor_tensor(out=tkt[:], in0=tk[:], in1=ty[:], op=ALU.mult), [Rk, Ry], [Rkt])
                        sc.op('pool', lambda e: e.tensor_tensor(out=tb[:], in0=tkap[:], in1=ta[:], op=ALU.mult), [Rkap, Ra], [Rb])
                        sc.op('dve', lambda e: e.tensor_tensor(out=tz[:], in0=tr[:], in1=tkt[:], op=ALU.mult), [Rr, Rkt], [Rz])
                        sc.op('dve', lambda e: e.tensor_scalar(out=tz[:], in0=tz[:], scalar1=r_k[:, h:h + 1], scalar2=None, op0=ALU.mult), [Rz, R_prm], [Rz])
                        p5 = pre_ps()
                        sc.op('pe', lambda e: e.matmul(psum[p5][0:64, 256:512], lhsT=o1, rhs=tz[:], start=True, stop=True), [Rz, R_MC], [R_ps[p5]])
                        sc.op('dve', lambda e: e.tensor_tensor(out=BON[:, h, :], in0=psum[p5][0:64, 256:512], in1=tv[:], op=ALU.mult), [R_ps[p5], Rv], [R_opsH[h]])
                        for c in range(NCH):
                            cs = slice(c * 64, (c + 1) * 64)
                            sc.op('dve', lambda e, cs=cs: e.tensor_tensor_scan(out=tlg[:, cs], data0=ones_f[0:64, 0:64], data1=tld[:, cs], initial=0.0, op0=ALU.mult, op1=ALU.add), [Rld, R_MC], [Rlg])
                        sc.op('act', lambda e: e.activation(out=tx[:], in_=tlg[:], func=AF.Exp), [Rlg], [Rx])
                        sc.op('act', lambda e: e.activation(out=ty[:], in_=tlg[:], func=AF.Exp, scale=-1.0), [Rlg], [Ry])
                        sc.op('dve', lambda e: e.tensor_tensor(out=tz[:], in0=tlg[:], in1=tld[:], op=ALU.subtract), [Rlg, Rld], [Rz])
                        sc.op('act', lambda e: e.activation(out=tz[:], in_=tz[:], func=AF.Exp), [Rz], [Rz])
                        sc.op('dve', lambda e: e.tensor_copy(out=GC[:, h, :], in_=tx[:].rearrange("p (c t) -> p c t", t=64)[:, :, 63]), [Rx], [R_opsH[h]])
                        sc.op('dve', lambda e: e.tensor_tensor(out=QtT[:, h, :], in0=tkap[:], in1=tz[:], op=ALU.mult), [Rkap, Rz], [R_opsH[h]])
                        sc.op('pool', lambda e: e.tensor_tensor(out=RtT[:, h, :], in0=tr[:], in1=tx[:], op=ALU.mult), [Rr, Rx], [R_opsH[h]])
                        sc.op('dve', lambda e: e.tensor_tensor(out=KhT[:, h, :], in0=tkt[:], in1=ty[:], op=ALU.mult), [Rkt, Ry], [R_opsH[h]])
                        sc.op('pool', lambda e: e.tensor_tensor(out=BhT[:, h, :], in0=tb[:], in1=ty[:], op=ALU.mult), [Rb, Ry], [R_opsH[h]])
                        for c in range(NCH):
                            cs = slice(c * 64, (c + 1) * 64)
                            sc.op('dve', lambda e, cs=cs, c=c: e.tensor_scalar(out=ty[:, cs], in0=ty[:, cs], scalar1=tx[:, c * 64 + 63:c * 64 + 64], scalar2=None, op0=ALU.mult), [Ry, Rx], [Ry])
                        sc.op('dve', lambda e: e.tensor_tensor(out=KbT[:, h, :], in0=tkt[:], in1=ty[:], op=ALU.mult), [Rkt, Ry], [R_opsH[h]])
                        sc.op('pool', lambda e: e.tensor_tensor(out=BbT[:, h, :], in0=tb[:], in1=ty[:], op=ALU.mult), [Rb, Ry], [R_opsH[h]])
                        sc.op('act', lambda e: e.copy(out=VTb[:, h, :], in_=tv[:]), [Rv], [R_opsH[h]])

                    def chunk_group(blk, c, hg):
                        cs = slice(c * 64, (c + 1) * 64)
                        hh = [hg * 4 + q for q in range(4)]
                        Rh = [R_opsH[h] for h in hh]
                        for q, h in enumerate(hh):
                            sc.op('pe', lambda e, q=q, h=h: e.matmul(psum[pA][0:64, q * 128:q * 128 + 64], lhsT=BhT[:, h, cs], rhs=QtT[:, h, cs], start=True, stop=True), [Rh[q]], [R_ps[pA]])
                            sc.op('pe', lambda e, q=q, h=h: e.matmul(psum[pA][0:64, q * 128 + 64:q * 128 + 128], lhsT=QtT[:, h, cs], rhs=BhT[:, h, cs], start=True, stop=True), [Rh[q]], [R_ps[pA]])
                            sc.op('pe', lambda e, q=q, h=h: e.matmul(psum[pB][0:64, q * 128:q * 128 + 64], lhsT=KhT[:, h, cs], rhs=QtT[:, h, cs], start=True, stop=True), [Rh[q]], [R_ps[pB]])
                            sc.op('pe', lambda e, q=q, h=h: e.matmul(psum[pB][0:64, q * 128 + 64:q * 128 + 128], lhsT=KhT[:, h, cs], rhs=RtT[:, h, cs], start=True, stop=True), [Rh[q]], [R_ps[pB]])
                            sc.op('pe', lambda e, q=q, h=h: e.matmul(psum[pC][0:64, q * 64:q * 64 + 64], lhsT=BhT[:, h, cs], rhs=RtT[:, h, cs], start=True, stop=True), [Rh[q]], [R_ps[pC]])
                        vA = psum[pA][0:64, :].rearrange("p (q x) -> p q x", x=128)
                        vB = psum[pB][0:64, :].rearrange("p (q x) -> p q x", x=128)
                        vC = psum[pC][0:64, 0:256].rearrange("p (q x) -> p q x", x=64)
                        sc.op('dve', lambda e: e.tensor_tensor(out=MZ[:, :, 0:64], in0=vA[:, :, 0:64], in1=RM[:, 0, :, :], op=ALU.mult), [R_ps[pA], R_prm], [R_MZ])
                        sc.op('dve', lambda e: e.tensor_tensor(out=MZ[:, :, 128:192], in0=vA[:, :, 64:128], in1=RM[:, 1, :, :], op=ALU.mult), [R_ps[pA], R_prm], [R_MZ])
                        sc.op('dve', lambda e: e.tensor_tensor(out=MZ[:, :, 64:128], in0=MZ[:, :, 0:64], in1=I4[:], op=ALU.add), [R_MZ, R_prm], [R_MZ])
                        sc.op('dve', lambda e: e.tensor_tensor(out=SC[:, 0, :, :], in0=vB[:, :, 0:64], in1=RM[:, 0, :, :], op=ALU.mult), [R_ps[pB], R_prm], [R_SC])
                        sc.op('dve', lambda e: e.tensor_scalar(out=SC[:, 0, :, :], in0=SC[:, 0, :, :], scalar1=-1.0, scalar2=None, op0=ALU.mult), [R_SC], [R_SC])
                        sc.op('dve', lambda e: e.tensor_tensor(out=SC[:, 1, :, :], in0=vB[:, :, 64:128], in1=RM[:, 2, :, :], op=ALU.mult), [R_ps[pB], R_prm], [R_SC])
                        sc.op('dve', lambda e: e.tensor_tensor(out=SC[:, 2, :, :], in0=vC, in1=RM[:, 2, :, :], op=ALU.mult), [R_ps[pC], R_prm], [R_SC])
                        for q, h in enumerate(hh):
                            for i, src in enumerate((KbT, BbT, VTb)):
                                sc.op('pe', lambda e, q=q, h=h, i=i, src=src: e.transpose(out=psT_b[0:64, q * 192 + i * 64:q * 192 + i * 64 + 64], in_=src[:, h, cs], identity=identb[0:64, 0:64]), [Rh[q], R_ident], [R_ps[7]])
                        sc.op('act', lambda e: e.copy(out=TM[:].rearrange("p q x -> p (q x)"), in_=psT_b[0:64, 0:768]), [R_ps[7]], [R_TM])
                        vD = psum[pD][0:64, :].rearrange("p (q x) -> p q x", x=128)
                        vE = psum[pE][0:64, 0:256].rearrange("p (q x) -> p q x", x=64)
                        for lev in range(6):
                            for q in range(4):
                                if lev == 0:
                                    sc.op('pe', lambda e, q=q: e.matmul(psum[pD][0:64, q * 128:q * 128 + 64], lhsT=MZ[:, q, 128:192], rhs=MZ[:, q, 0:64], start=True, stop=True), [R_MZ], [R_ps[pD]])
                                elif lev < 5:
                                    sc.op('pe', lambda e, q=q: e.matmul(psum[pD][0:64, q * 128:q * 128 + 128], lhsT=MZ[:, q, 128:192], rhs=MZ[:, q, 0:128], start=True, stop=True), [R_MZ], [R_ps[pD]])
                                else:
                                    sc.op('pe', lambda e, q=q: e.matmul(psum[pD][0:64, q * 128 + 64:q * 128 + 128], lhsT=MZ[:, q, 128:192], rhs=MZ[:, q, 64:128], start=True, stop=True), [R_MZ], [R_ps[pD]])
                                if lev < 5:
                                    sc.op('pe', lambda e, q=q: e.matmul(psum[pE][0:64, q * 64:q * 64 + 64], lhsT=MZ[:, q, 0:64], rhs=MZ[:, q, 128:192], start=True, stop=True), [R_MZ], [R_ps[pE]])
                            if lev >= 1:
                                sc.op('dve', lambda e: e.tensor_tensor(out=MZ[:, :, 64:128], in0=vD[:, :, 64:128], in1=MZ[:, :, 64:128], op=ALU.add), [R_ps[pD], R_MZ], [R_MZ])
                            if lev < 5:
                                sc.op('act', lambda e: e.copy(out=MZ[:, :, 0:64], in_=vD[:, :, 0:64]), [R_ps[pD]], [R_MZ])
                                sc.op('act', lambda e: e.copy(out=MZ[:, :, 128:192], in_=vE), [R_ps[pE]], [R_MZ])
                        RP = R_P[hg]
                        for q, h in enumerate(hh):
                            sc.op('pe', lambda e, q=q, h=h: e.matmul(psum[pF][0:64, q * 64:q * 64 + 64], lhsT=QtT[:, h, cs], rhs=Pb[:, h, :], start=True, stop=False), [Rh[q], RP], [R_ps[pF]])
                            sc.op('pe', lambda e, q=q: e.matmul(psum[pF][0:64, q * 64:q * 64 + 64], lhsT=SC[:, 0, q, :], rhs=TM[:, q, 128:192], start=False, stop=True), [R_SC, R_TM], [R_ps[pF]])
                        sc.op('act', lambda e: e.copy(out=Xs[:].rearrange("p q x -> p (q x)"), in_=psum[pF][0:64, 0:256]), [R_ps[pF]], [R_X])
                        for q in range(4):
                            sc.op('pe', lambda e, q=q: e.matmul(psum[pF][0:64, 256 + q * 64:256 + q * 64 + 64], lhsT=MZ[:, q, 64:128], rhs=Xs[:, q, :], start=True, stop=True), [R_MZ, R_X], [R_ps[pF]])
                        sc.op('act', lambda e: e.mul(out=Un[:].rearrange("p q x -> p (q x)"), in_=psum[pF][0:64, 256:512], mul=-1.0), [R_ps[pF]], [R_U])
                        for q, h in enumerate(hh):
                            sc.op('pe', lambda e, q=q: e.matmul(psum[pG][0:64, q * 64:q * 64 + 64], lhsT=TM[:, q, 0:64], rhs=TM[:, q, 128:192], start=True, stop=False), [R_TM], [R_ps[pG]])
                            sc.op('pe', lambda e, q=q: e.matmul(psum[pG][0:64, q * 64:q * 64 + 64], lhsT=TM[:, q, 64:128], rhs=Un[:, q, :], start=False, stop=True), [R_TM, R_U], [R_ps[pG]])
                        for q, h in enumerate(hh):
                            sc.op('pe', lambda e, q=q, h=h: e.matmul(psum[pG][0:64, 256 + q * 64:256 + q * 64 + 64], lhsT=Pb[:, h, :], rhs=RtT[:, h, cs], start=True, stop=False), [Rh[q], RP], [R_ps[pG]])
                            sc.op('pe', lambda e, q=q: e.matmul(psum[pG][0:64, 256 + q * 64:256 + q * 64 + 64], lhsT=TM[:, q, 128:192], rhs=SC[:, 1, q, :], start=False, stop=False), [R_TM, R_SC], [R_ps[pG]])
                            sc.op('pe', lambda e, q=q: e.matmul(psum[pG][0:64, 256 + q * 64:256 + q * 64 + 64], lhsT=Un[:, q, :], rhs=SC[:, 2, q, :], start=False, stop=True), [R_U, R_SC], [R_ps[pG]])
                        sc.op('act', lambda e: e.copy(out=yT[:, hg * 4:hg * 4 + 4, cs], in_=psum[pG][0:64, 256:512].rearrange("p (q x) -> p q x", x=64)), [R_ps[pG]], [R_yT[hg]])
                        for q, h in enumerate(hh):
                            sc.op('dve', lambda e, q=q, h=h: e.scalar_tensor_tensor(out=Pf[:, h, :], in0=Pf[:, h, :], scalar=GC[:, h, c:c + 1], in1=psum[pG][0:64, q * 64:q * 64 + 64], op0=ALU.mult, op1=ALU.add), [R_ps[pG], Rh[q], RP], [RP])
                        sc.op('dve', lambda e: e.tensor_copy(out=Pb[:, hg * 4:hg * 4 + 4, :], in_=Pf[:, hg * 4:hg * 4 + 4, :]), [RP], [RP])

                    def post_head(blk, h):
                        t0 = blk * RB
                        hg = h // 4
                        tx, ty, tz, tw = T[0], T[1], T[2], T[3]
                        Rx, Ry, Rz, Rw = R_T[0], R_T[1], R_T[2], R_T[3]
                        p1 = pre_ps(); p2 = pre_ps()
                        sc.op('pe', lambda e: e.matmul(psum[p1][0:64, 256:512], lhsT=o1, rhs=yT[:, h, :], start=True, stop=True), [R_yT[hg], R_MC], [R_ps[p1]])
                        sc.op('act', lambda e: e.activation(out=tx[:], in_=yT[:, h, :], func=AF.Square), [R_yT[hg]], [Rx])
                        sc.op('pe', lambda e: e.matmul(psum[p2][0:64, 256:512], lhsT=o1, rhs=tx[:], start=True, stop=True), [Rx, R_MC], [R_ps[p2]])
                        sc.op('act', lambda e: e.mul(out=ty[:], in_=psum[p1][0:64, 256:512], mul=1.0 / 64), [R_ps[p1]], [Ry])
                        sc.op('act', lambda e: e.mul(out=tz[:], in_=psum[p2][0:64, 256:512], mul=1.0 / 64), [R_ps[p2]], [Rz])
                        sc.op('dve', lambda e: e.tensor_tensor(out=tw[:], in0=ty[:], in1=ty[:], op=ALU.mult), [Ry], [Rw])
                        sc.op('dve', lambda e: e.tensor_tensor(out=tz[:], in0=tz[:], in1=tw[:], op=ALU.subtract), [Rz, Rw], [Rz])
                        sc.op('dve', lambda e: e.tensor_scalar(out=tz[:], in0=tz[:], scalar1=64e-5, scalar2=None, op0=ALU.add), [Rz], [Rz])
                        sc.op('act', lambda e: e.activation(out=tz[:], in_=tz[:], func=AF.Sqrt), [Rz], [Rz])
                        sc.op('dve', lambda e: e.reciprocal(out=tz[:], in_=tz[:]), [Rz], [Rz])
                        sc.op('dve', lambda e: e.tensor_tensor(out=tw[:], in0=yT[:, h, :], in1=ty[:], op=ALU.subtract), [R_yT[hg], Ry], [Rw])
                        sc.op('dve', lambda e: e.tensor_tensor(out=tw[:], in0=tw[:], in1=tz[:], op=ALU.mult), [Rw, Rz], [Rw])
                        sc.op('dve', lambda e: e.tensor_scalar(out=tw[:], in0=tw[:], scalar1=ln_g[:, h:h + 1], scalar2=ln_b[:, h:h + 1], op0=ALU.mult, op1=ALU.add), [Rw, R_prm], [Rw])
                        sc.op('pool', lambda e: e.tensor_tensor(out=tw[:], in0=tw[:], in1=BON[:, h, :], op=ALU.add), [Rw, R_opsH[h]], [Rw])
                        sc.op('pool', lambda e: e.tensor_tensor(out=OBs[:, h, :], in0=tw[:], in1=Gg[:, h, :], op=ALU.mult), [Rw, R_opsH[h]], [R_OBs])

                    OBs = sb("OBs", [64, 12, RB], BF16, ph); R_OBs = Res()

                    V32 = sb("V32", [64, 12, RB], F32, ph); R_V32 = Res()
                    if l > 0:
                        VFt = sb("VFt", [64, 12, RB], F32, ph); R_VFt = Res()
                        vbf = sb("vbf", [64, 12, RB], BF16, ph); lob = sb("lob", [32, RB], BF16, ph); R_vbf = Res(); R_lob = Res()
                        mvd = sb("mvd", [64, 12, 32], BF16, ph); mvu = sb("mvu", [32, 768], BF16, ph)
                        v0 = prm("v0", rwkv_v0[l - 1])
                        sc.dma('pool', lambda e: e.dma_start(out=mvd[:], in_=rwkv_mv_down[l - 1].rearrange("(h d) m -> d h m", d=64)), [], [R_prm])
                        sc.dma('pool', lambda e: e.dma_start(out=mvu[:], in_=rwkv_mv_up[l - 1]), [], [R_prm])

                    def pre_v(blk):
                        t0 = blk * RB
                        for h in range(12):
                            def one(h):
                                shift('dve', V32[:, h, :], zr[2], (slice(0, 64), h), mu_v[:, h:h + 1], T[2][:], R_z[2], R_T[2], R_V32)
                            one(h)
                        if l == 0:
                            sc.dma('sp', lambda e: e.dma_start(out=VF.rearrange("(h d) s -> d h s", d=64)[:, :, t0:t0 + RB], in_=V32[:]), [R_V32], [R_VF])
                        else:
                            sc.dma('sp', lambda e: e.dma_start(out=VFt[:], in_=VF.rearrange("(h d) s -> d h s", d=64)[:, :, t0:t0 + RB]), [R_VF], [R_VFt])
                            sc.op('act', lambda e: e.copy(out=vbf[:], in_=V32[:]), [R_V32], [R_vbf])
                            pl = pre_ps()
                            for h in range(12):
                                sc.op('pe', lambda e, h=h: e.matmul(psum[pl][0:32, 256:512], lhsT=mvd[:, h, :], rhs=vbf[:, h, :], start=(h == 0), stop=(h == 11)), [R_vbf, R_prm], [R_ps[pl]])
                            sc.op('act', lambda e: e.copy(out=lob[:], in_=psum[pl][0:32, 256:512]), [R_ps[pl]], [R_lob])
                            for h in range(12):
                                def one2(h):
                                    p_ = pre_ps()
                                    sc.op('pe', lambda e: e.matmul(psum[p_][0:64, 256:512], lhsT=mvu[:, h * 64:(h + 1) * 64], rhs=lob[:], start=True, stop=True), [R_lob, R_prm], [R_ps[p_]])
                                    sc.op('act', lambda e: e.activation(out=T[0][:], in_=psum[p_][0:64, 256:512], func=AF.Sigmoid, bias=v0[:, h:h + 1]), [R_ps[p_], R_prm], [R_T[0]])
                                    sc.op('pool', lambda e: e.tensor_tensor(out=T[1][:], in0=VFt[:, h, :], in1=V32[:, h, :], op=ALU.subtract), [R_VFt, R_V32], [R_T[1]])
                                    sc.op('dve', lambda e: e.tensor_tensor(out=T[1][:], in0=T[1][:], in1=T[0][:], op=ALU.mult), [R_T[1], R_T[0]], [R_T[1]])
                                    sc.op('pool', lambda e: e.tensor_tensor(out=V32[:, h, :], in0=V32[:, h, :], in1=T[1][:], op=ALU.add), [R_T[1], R_V32], [R_V32])
                                one2(h)

                    def rw_block(blk):
                        load_block(blk)
                        pre_v(blk)
                        for h in range(12):
                            pre_head(blk, h)
                        if RW_STAGE >= 2:
                            for c in range(NCH):
                                for hg in range(3):
                                    chunk_group(blk, c, hg)
                        if RW_STAGE >= 3:
                            for h in range(12):
                                post_head(blk, h)
                            sc.dma('sp', lambda e: e.dma_start(out=OB.rearrange("(h d) s -> d h s", d=64)[:, :, blk * RB:(blk + 1) * RB], in_=OBs[:]), [R_OBs], [R_OB])

                    for blk in range(NBLK if RW_BLOCKS is None else RW_BLOCKS):
                        rw_block(blk)
                sc.barrier()
            if DO_F:
              with ExitStack() as ph:
                TGF = 256; NTF = S // TGF
                PA = sb("PA", [128, 6, D], BF16, ph); PB = sb("PB", [128, 6, D], BF16, ph); PC = sb("PC", [128, 2, D], BF16, ph); WO = sb("WO", [128, KC, D], BF16, ph); R_W = Res()
                for kc in range(6):
                    sc.dma('pool', lambda e, kc=kc: e.dma_start(out=PA[:, kc, :], in_=p_a[l, kc * 128:(kc + 1) * 128, :]), [], [R_W])
                    sc.dma('pool', lambda e, kc=kc: e.dma_start(out=PB[:, kc, :], in_=p_b[l, kc * 128:(kc + 1) * 128, :]), [], [R_W])
                for kc in range(2):
                    sc.dma('pool', lambda e, kc=kc: e.dma_start(out=PC[:, kc, :], in_=p_c[l, kc * 128:(kc + 1) * 128, :]), [], [R_W])
                for kc in range(KC):
                    sc.dma('pool', lambda e, kc=kc: e.dma_start(out=WO[:, kc, :], in_=w_o[l, kc * 128:(kc + 1) * 128, :]), [], [R_W])
                lng = sb("lng", [128, KC], F32, ph); lnb = sb("lnb", [128, KC], F32, ph); g1p = sb("g1p", [128, KC], F32, ph); R_ln = Res()
                sc.dma('sp', lambda e: e.dma_start(out=lng[:], in_=ln1_g[l].rearrange("(k p) -> p k", p=128), allow_slow_non_contiguous=True), [], [R_ln])
                sc.dma('sp', lambda e: e.dma_start(out=lnb[:], in_=ln1_b[l].rearrange("(k p) -> p k", p=128), allow_slow_non_contiguous=True), [], [R_ln])
                sc.op('dve', lambda e: e.tensor_scalar(out=g1p[:], in0=modT[:, 32:48], scalar1=1.0, scalar2=None, op0=ALU.add), [R_mod], [R_ln])
                oaT = sb("oaT", [128, 6, TGF], BF16, ph); obT = sb("obT", [128, 6, TGF], BF16, ph); ocT = sb("ocT", [128, 2, TGF], BF16, ph); R_o = Res()
                gAB = [sb("gAB%d" % i, [128, 3, 4, TGF], BF16, ph) for i in range(2)]; R_g = [Res(), Res()]
                mrg = sb("mrg", [128, KC, TGF], BF16, ph); R_mrg = Res()
                xy = sb("xy", [128, KC, TGF], F32, ph); R_xy = Res()
                tA = [sb("tA%d" % i, [128, TGF], F32, ph) for i in range(2)]; R_tA = [Res(), Res()]
                tB = [sb("tB%d" % i, [128, TGF], F32, ph) for i in range(2)]; R_tB = [Res(), Res()]
                mean = sb("mean", [128, TGF], F32, ph); rstd = sb("rstd", [128, TGF], F32, ph); R_st = Res()
                XTv = XT.rearrange("(k p) s -> p k s", p=128)
                PRv = PROJ.rearrange("(k p) s -> p k s", p=128)
                ALPHA = 8.0 ** 0.25
                cntF = [0]

                def f_merge_dc(dc, gb):
                    j = dc % 4
                    pa = nextps(); pb_ = nextps(); pc = nextps()
                    for kc in range(6):
                        sc.op('pe', lambda e, kc=kc: e.matmul(psum[pa][:, 0:TGF], lhsT=PA[:, kc, dc * 128:(dc + 1) * 128], rhs=oaT[:, kc, :], start=(kc == 0), stop=(kc == 5)), [R_W, R_o], [R_ps[pa]])
                    for kc in range(6):
                        sc.op('pe', lambda e, kc=kc: e.matmul(psum[pb_][:, 0:TGF], lhsT=PB[:, kc, dc * 128:(dc + 1) * 128], rhs=obT[:, kc, :], start=(kc == 0), stop=(kc == 5)), [R_W, R_o], [R_ps[pb_]])
                    for kc in range(2):
                        sc.op('pe', lambda e, kc=kc: e.matmul(psum[pc][:, 0:TGF], lhsT=PC[:, kc, dc * 128:(dc + 1) * 128], rhs=ocT[:, kc, :], start=(kc == 0), stop=(kc == 1)), [R_W, R_o], [R_ps[pc]])
                    i = cntF[0] % 2; cntF[0] += 1
                    sc.op('dve', lambda e: e.tensor_tensor(out=tA[i][:], in0=psum[pa][:, 0:TGF], in1=gAB[gb][:, 0, j, :], op=ALU.mult), [R_ps[pa], R_g[gb]], [R_tA[i]])
                    sc.op('dve', lambda e: e.tensor_tensor(out=tB[i][:], in0=psum[pb_][:, 0:TGF], in1=gAB[gb][:, 1, j, :], op=ALU.mult), [R_ps[pb_], R_g[gb]], [R_tB[i]])
                    sc.op('pool', lambda e: e.tensor_tensor(out=tA[i][:], in0=tA[i][:], in1=tB[i][:], op=ALU.add), [R_tA[i], R_tB[i]], [R_tA[i]])
                    sc.op('dve', lambda e: e.tensor_tensor(out=tB[i][:], in0=psum[pc][:, 0:TGF], in1=gAB[gb][:, 2, j, :], op=ALU.mult), [R_ps[pc], R_g[gb]], [R_tB[i]])
                    sc.op('pool', lambda e: e.tensor_tensor(out=mrg[:, dc, :], in0=tA[i][:], in1=tB[i][:], op=ALU.add), [R_tA[i], R_tB[i]], [R_mrg])

                def f_wo_dc(dc):
                    pm = nextps()
                    for kc in range(KC):
                        sc.op('pe', lambda e, kc=kc: e.matmul(psum[pm][:, 0:TGF], lhsT=WO[:, kc, dc * 128:(dc + 1) * 128], rhs=mrg[:, kc, :], start=(kc == 0), stop=(kc == KC - 1)), [R_W, R_mrg], [R_ps[pm]])
                    sc.op('act', lambda e: e.mul(out=xy[:, dc, :], in_=xy[:, dc, :], mul=ALPHA), [R_xy], [R_xy])
                    sc.op('dve', lambda e: e.scalar_tensor_tensor(out=xy[:, dc, :], in0=psum[pm][:, 0:TGF], scalar=g1p[:, dc:dc + 1], in1=xy[:, dc, :], op0=ALU.mult, op1=ALU.add), [R_ps[pm], R_ln, R_xy], [R_xy])

                def f_tg(tf):
                    ts = slice(tf * TGF, (tf + 1) * TGF)
                    RX = R_XT[tf // 2]
                    sc.dma('sp', lambda e: e.dma_start(out=oaT[:], in_=OA.rearrange("(k p) s -> p k s", p=128)[:, :, ts]), [R_OA], [R_o])
                    sc.dma('sp', lambda e: e.dma_start(out=obT[:], in_=OB.rearrange("(k p) s -> p k s", p=128)[:, :, ts]), [R_OB], [R_o])
                    sc.dma('sp', lambda e: e.dma_start(out=ocT[:], in_=OC.rearrange("(k p) s -> p k s", p=128)[:, :, ts]), [R_OC], [R_o])
                    sc.dma('sp', lambda e: e.dma_start(out=xy[:], in_=XTv[:, :, ts]), [RX], [R_xy])
                    for g4 in range(4):
                        gb = g4 % 2
                        for br in range(3):
                            k0 = (OFF_G + br * D) // 128 + g4 * 4
                            sc.dma('sp', lambda e, k0=k0, gb=gb, br=br: e.dma_start(out=gAB[gb][:, br, :, :], in_=PRv[:, k0:k0 + 4, ts]), [R_PROJ], [R_g[gb]])
                        for j in range(4):
                            f_merge_dc(g4 * 4 + j, gb)
                    for dc in range(KC):
                        f_wo_dc(dc)
                    ln_feature_major(xy, R_xy, TGF, lng, lnb, R_ln, tA, R_tA, tB, R_tB, mean, rstd, R_st)
                    sc.dma('sp', lambda e: e.dma_start(out=XTv[:, :, ts], in_=xy[:]), [R_xy], [RX])

                for tf in range(NTF):
                    f_tg(tf)
              sc.barrier()
            if DO_G:
              with ExitStack() as ph:
                TGM = 1024; NSUB = 2; NTM = S // TGM
                hTm_ = sb("hTm", [128, KC, TGM], BF16, ph); R_hTm = Res()
                acc = sb("acc", [128, KC, TGM], F32, ph); R_acc = [Res() for _ in range(NSUB)]
                Wgu = [sb("Wgu%d" % i, [128, KC, 512], BF16, ph) for i in range(2)]; Wd = [sb("Wd%d" % i, [128, 2, D], BF16, ph) for i in range(2)]; R_Wg = [Res(), Res()]; R_Wu = [Res(), Res()]; R_Wdn = [Res(), Res()]
                gwb = [sb("gwb%d" % i, [128, TGM], F32, ph) for i in range(2)]; R_gwb = [Res(), Res()]
                xt4 = [sb("xt4%d" % i, [128, 512], F32, ph) for i in range(2)]; R_xt4 = [Res(), Res()]
                hf = [sb("hf%d" % i, [128, 512], F32, ph) for i in range(2)]; R_hf = [Res(), Res()]
                sg = [sb("sg%d" % i, [128, 512], F32, ph) for i in range(2)]; R_sg = [Res(), Res()]
                actT = [sb("actT%d" % i, [128, 2, 512], BF16, ph) for i in range(2)]; R_actT = [Res(), Res()]
                Wr = sb("Wr", [128, KC, 72], F32, ph); br72 = sb("br72", [128, 4, 72], F32, ph); R_Wr = Res()
                sc.dma('sp', lambda e: e.dma_start(out=Wr[:, :, 0:8], in_=router_grp_w[l].rearrange("(k p) n -> p k n", p=128)), [], [R_Wr])
                sc.dma('sp', lambda e: e.dma_start(out=Wr[:, :, 8:72], in_=router_exp_w[l].rearrange("(k p) n -> p k n", p=128)), [], [R_Wr])
                for t_ in range(4):
                    sc.dma('sp', lambda e, t_=t_: e.dma_start(out=br72[:, t_, 0:8], in_=bass.AP(router_grp_b.tensor, l * 8, [[0, 128], [1, 8]])), [], [R_Wr])
                    sc.dma('sp', lambda e, t_=t_: e.dma_start(out=br72[:, t_, 8:72], in_=bass.AP(router_exp_b.tensor, l * 64, [[0, 128], [1, 64]])), [], [R_Wr])
                lng2 = sb("lng2", [128, KC], F32, ph); lnb2 = sb("lnb2", [128, KC], F32, ph); g2p = sb("g2p", [128, KC], F32, ph); sc2p = sb("sc2p", [128, KC], F32, ph); R_ln2 = Res()
                sc.dma('sp', lambda e: e.dma_start(out=lng2[:], in_=ln2_g[l].rearrange("(k p) -> p k", p=128), allow_slow_non_contiguous=True), [], [R_ln2])
                sc.dma('sp', lambda e: e.dma_start(out=lnb2[:], in_=ln2_b[l].rearrange("(k p) -> p k", p=128), allow_slow_non_contiguous=True), [], [R_ln2])
                sc.op('dve', lambda e: e.tensor_scalar(out=g2p[:], in0=modT[:, 80:96], scalar1=1.0, scalar2=None, op0=ALU.add), [R_mod], [R_ln2])
                sc.op('dve', lambda e: e.tensor_scalar(out=sc2p[:], in0=modT[:, 64:80], scalar1=1.0, scalar2=None, op0=ALU.add), [R_mod], [R_ln2])
                lgt = sb("lgt", [128, 4, 72], F32, ph); R_lgt = Res()
                rt = sb("rt", [128, 8, 64], F32, ph); R_rt = Res()
                gwt = sb("gwt", [128, 64], F32, ph); gwT = sb("gwT", [64, 512], F32, ph); R_gwT = Res()
                tAm = sg; R_tAm = R_sg
                tBm = hf; R_tBm = R_hf
                meanm = sb("meanm", [128, 512], F32, ph); rstdm = sb("rstdm", [128, 512], F32, ph); R_st2 = Res()
                XTv2 = XT.rearrange("(k p) s -> p k s", p=128)
                ALPHA = 8.0 ** 0.25
                cntG = [0]

                def route_tile(tm, sub, t):
                    L = lgt[:, t, :]
                    r = rt
                    c1 = r[:, 0, 0:1]; c2 = r[:, 0, 1:2]; c3 = r[:, 0, 2:3]; c4 = r[:, 0, 3:4]; c5 = r[:, 0, 4:5]; c6 = r[:, 0, 5:6]
                    RL = [R_lgt, R_rt]
                    def O(eng, f):
                        sc.op(eng, f, RL, [R_rt])
                    O('dve', lambda e: e.tensor_reduce(out=c1, in_=L[:, 0:8], axis=AX.X, op=ALU.max))
                    O('dve', lambda e: e.tensor_scalar(out=r[:, 1, 0:8], in0=L[:, 0:8], scalar1=c1, scalar2=None, op0=ALU.subtract))
                    O('act', lambda e: e.activation(out=r[:, 1, 8:16], in_=r[:, 1, 0:8], func=AF.Exp))
                    O('dve', lambda e: e.tensor_reduce(out=c2, in_=r[:, 1, 8:16], axis=AX.X, op=ALU.add))
                    O('dve', lambda e: e.reciprocal(out=c2, in_=c2))
                    O('dve', lambda e: e.tensor_scalar(out=r[:, 1, 16:24], in0=L[:, 0:8], scalar1=c1, scalar2=None, op0=ALU.is_ge))
                    O('dve', lambda e: e.tensor_scalar(out=r[:, 1, 16:24], in0=r[:, 1, 16:24], scalar1=-1.0, scalar2=1e30, op0=ALU.add, op1=ALU.mult))
                    for g in range(8):
                        O('dve', lambda e, g=g: e.tensor_scalar(out=r[:, 2, g * 8:(g + 1) * 8], in0=L[:, 8 + g * 8:16 + g * 8], scalar1=r[:, 1, 16 + g:17 + g], scalar2=None, op0=ALU.add))
                    O('dve', lambda e: e.max(out=r[:, 3, 0:8], in_=r[:, 2, :]))
                    O('dve', lambda e: e.tensor_scalar(out=r[:, 4, :], in0=r[:, 2, :], scalar1=r[:, 3, 0:1], scalar2=None, op0=ALU.is_ge))
                    O('dve', lambda e: e.tensor_scalar(out=r[:, 5, :], in0=r[:, 2, :], scalar1=r[:, 3, 1:2], scalar2=None, op0=ALU.is_ge))
                    O('dve', lambda e: e.tensor_tensor(out=c3, in0=r[:, 3, 1:2], in1=r[:, 3, 0:1], op=ALU.subtract))
                    O('act', lambda e: e.activation(out=c3, in_=c3, func=AF.Exp))
                    O('dve', lambda e: e.tensor_scalar(out=c3, in0=c3, scalar1=1.0, scalar2=None, op0=ALU.add))
                    O('dve', lambda e: e.reciprocal(out=c3, in_=c3))
                    O('dve', lambda e: e.tensor_scalar(out=c4, in0=c3, scalar1=-1.0, scalar2=1.0, op0=ALU.mult, op1=ALU.add))
                    O('dve', lambda e: e.tensor_tensor(out=c5, in0=c3, in1=c4, op=ALU.subtract))
                    O('dve', lambda e: e.tensor_tensor(out=c5, in0=c5, in1=c2, op=ALU.mult))
                    O('dve', lambda e: e.tensor_tensor(out=c6, in0=c4, in1=c2, op=ALU.mult))
                    O('dve', lambda e: e.tensor_scalar(out=r[:, 5, :], in0=r[:, 5, :], scalar1=c6, scalar2=None, op0=ALU.mult))
                    sc.op('dve', lambda e: e.scalar_tensor_tensor(out=gwt[:], in0=r[:, 4, :], scalar=c5, in1=r[:, 5, :], op0=ALU.mult, op1=ALU.add), RL, [R_rt, R_gwT])
                    pt = nextps()
                    sc.op('pe', lambda e: e.transpose(out=psum[pt][0:64, 0:128], in_=gwt[:], identity=ident[:]), [R_gwT, R_ident], [R_ps[pt]])
                    sc.op('act', lambda e: e.copy(out=gwT[:, t * 128:(t + 1) * 128], in_=psum[pt][0:64, 0:128]), [R_ps[pt]], [R_gwT])

                def m_sub_prep(tm, sub):
                    t0 = tm * TGM + sub * 512
                    pl = nextps()
                    for dc in range(KC):
                        def two(dc):
                            b = cntG[0] % 2; cntG[0] += 1
                            i = dc % 2
                            sc.dma('sp', lambda e: e.dma_start(out=xt4[b][:], in_=XTv2[:, dc, t0:t0 + 512]), [R_XT[t0 // 512]], [R_xt4[b]])
                            sc.op('dve', lambda e: e.tensor_scalar(out=hf[i][:], in0=xt4[b][:], scalar1=sc2p[:, dc:dc + 1], scalar2=modT[:, 48 + dc:49 + dc], op0=ALU.mult, op1=ALU.add), [R_xt4[b], R_ln2, R_mod], [R_hf[i]])
                            sc.op('act', lambda e: e.copy(out=hTm_[:, dc, sub * 512:(sub + 1) * 512], in_=hf[i][:]), [R_hf[i]], [R_hTm])
                            for t in range(4):
                                sc.op('pe', lambda e, t=t: e.matmul(psum[pl][:, t * 72:(t + 1) * 72], lhsT=hf[i][:, t * 128:(t + 1) * 128], rhs=Wr[:, dc, :], start=(dc == 0 and t == 0), stop=(dc == KC - 1)), [R_hf[i], R_Wr], [R_ps[pl]])
                        two(dc)
                    sc.op('dve', lambda e: e.tensor_tensor(out=lgt[:], in0=psum[pl][:, 0:288].rearrange("p (t n) -> p t n", n=72), in1=br72[:], op=ALU.add), [R_ps[pl], R_Wr], [R_lgt])
                    for t in range(4):
                        route_tile(tm, sub, t)
                    sc.dma('sp', lambda e: e.dma_start(out=GW[:, t0:t0 + 512], in_=gwT[:]), [R_gwT], [R_GW])

                def m_expert(tm, e_, wb):
                    sc.dma('pool', lambda e: e.dma_start(out=Wgu[wb][:, :, 0:256], in_=exp_w_gate[l, e_].rearrange("(k p) f -> p k f", p=128)), [], [R_Wg[wb]])
                    sc.dma('pool', lambda e: e.dma_start(out=Wgu[wb][:, :, 256:512], in_=exp_w_up[l, e_].rearrange("(k p) f -> p k f", p=128)), [], [R_Wu[wb]])
                    sc.dma('pool', lambda e: e.dma_start(out=Wd[wb][:], in_=exp_w_down[l, e_].rearrange("(k p) f -> p k f", p=128)), [], [R_Wdn[wb]])
                    sc.dma('sp', lambda e: e.dma_start(out=gwb[wb][:], in_=bass.AP(GW.tensor, e_ * S + tm * TGM, [[0, 128], [1, TGM]])), [R_GW], [R_gwb[wb]])
                    for sub in range(NSUB):
                        def one(sub):
                            cs = slice(sub * 512, (sub + 1) * 512)
                            ab = cntG[0] % 2; cntG[0] += 1
                            for fc in range(2):
                                def two(fc):
                                    pg = nextps(); pu = nextps()
                                    for k in range(KC):
                                        sc.op('pe', lambda e, k=k: e.matmul(psum[pg][:], lhsT=Wgu[wb][:, k, fc * 128:(fc + 1) * 128], rhs=hTm_[:, k, cs], start=(k == 0), stop=(k == KC - 1)), [R_Wg[wb], R_hTm], [R_ps[pg]])
                                    for k in range(KC):
                                        sc.op('pe', lambda e, k=k: e.matmul(psum[pu][:], lhsT=Wgu[wb][:, k, 256 + fc * 128:256 + (fc + 1) * 128], rhs=hTm_[:, k, cs], start=(k == 0), stop=(k == KC - 1)), [R_Wu[wb], R_hTm], [R_ps[pu]])
                                    i = fc
                                    sc.op('act', lambda e: e.activation(out=sg[i][:], in_=psum[pg][:], func=AF.Silu), [R_ps[pg]], [R_sg[i]])
                                    sc.op('dve', lambda e: e.tensor_tensor(out=sg[i][:], in0=psum[pu][:], in1=sg[i][:], op=ALU.mult), [R_ps[pu], R_sg[i]], [R_sg[i]])
                                    sc.op('pool', lambda e: e.tensor_tensor(out=actT[ab][:, fc, :], in0=sg[i][:], in1=gwb[wb][:, cs], op=ALU.mult), [R_sg[i], R_gwb[wb]], [R_actT[ab]])
                                two(fc)
                            for dc in range(KC):
                                def three(dc):
                                    pd = nextps()
                                    for fc in range(2):
                                        sc.op('pe', lambda e, fc=fc: e.matmul(psum[pd][:], lhsT=Wd[wb][:, fc, dc * 128:(dc + 1) * 128], rhs=actT[ab][:, fc, :], start=(fc == 0), stop=(fc == 1)), [R_Wdn[wb], R_actT[ab]], [R_ps[pd]])
                                    if e_ == 0:
                                        sc.op('act', lambda e: e.copy(out=acc[:, dc, cs], in_=psum[pd][:]), [R_ps[pd]], [R_acc[sub]])
                                    else:
                                        sc.op('dve', lambda e: e.tensor_tensor(out=acc[:, dc, cs], in0=psum[pd][:], in1=acc[:, dc, cs], op=ALU.add), [R_ps[pd], R_acc[sub]], [R_acc[sub]])
                                three(dc)
                        one(sub)

                def m_finish(tm, sub):
                    t0 = tm * TGM + sub * 512
                    cs = slice(sub * 512, (sub + 1) * 512)
                    for dc in range(KC):
                        def one(dc):
                            b = cntG[0] % 2; cntG[0] += 1
                            sc.dma('sp', lambda e: e.dma_start(out=xt4[b][:], in_=XTv2[:, dc, t0:t0 + 512]), [R_XT[t0 // 512]], [R_xt4[b]])
                            sc.op('act', lambda e: e.mul(out=xt4[b][:], in_=xt4[b][:], mul=ALPHA), [R_xt4[b]], [R_xt4[b]])
                            sc.op('dve', lambda e: e.scalar_tensor_tensor(out=acc[:, dc, cs], in0=acc[:, dc, cs], scalar=g2p[:, dc:dc + 1], in1=xt4[b][:], op0=ALU.mult, op1=ALU.add), [R_acc[sub], R_ln2, R_xt4[b]], [R_acc[sub]])
                        one(dc)
                    ln_feature_major(acc[:, :, cs], R_acc[sub], 512, lng2, lnb2, R_ln2, tAm, R_tAm, tBm, R_tBm, meanm, rstdm, R_st2)
                    sc.dma('sp', lambda e: e.dma_start(out=XTv2[:, :, t0:t0 + 512], in_=acc[:, :, cs]), [R_acc[sub]], [R_XT[t0 // 512]])

                def m_group(tm):
                    for sub in range(NSUB):
                        m_sub_prep(tm, sub)
                    for e_ in range(NEXP):
                        m_expert(tm, e_, e_ % 2)
                    for sub in range(NSUB):
                        m_finish(tm, sub)

                for tm in range(NTM if MOE_GROUPS is None else MOE_GROUPS):
                    m_group(tm)
              sc.barrier()
        for l_ in range(n_layers):
            layer(l_)
            sc.new_epoch()
        with ExitStack() as ph:
          if DO_Z:
            zin = [sb("zin%d" % i, [128, KC, 128], F32, ph) for i in range(2)]; R_zin = [Res(), Res()]
            zst = [sb("zst%d" % i, [128, D], F32, ph) for i in range(2)]; R_zst = [Res(), Res()]
            def z_tile(t, b):
                sc.dma('sp', lambda e: e.dma_start(out=zin[b][:], in_=XT.rearrange("(k p) s -> p k s", p=128)[:, :, t * 128:(t + 1) * 128]), [R_XT[t // 4]], [R_zin[b]])
                for g in range(4):
                    pi = nextps()
                    for j in range(4):
                        dc = g * 4 + j
                        sc.op('pe', lambda e, pi=pi, j=j, dc=dc: e.transpose(out=psum[pi][:, j * 128:(j + 1) * 128], in_=zin[b][:, dc, :], identity=ident[:]), [R_zin[b], R_ident], [R_ps[pi]])
                    if g % 2 == 0:
                        sc.op('act', lambda e, pi=pi, g=g: e.copy(out=zst[b][:, g * 512:(g + 1) * 512], in_=psum[pi][:]), [R_ps[pi]], [R_zst[b]])
                    else:
                        sc.op('dve', lambda e, pi=pi, g=g: e.tensor_copy(out=zst[b][:, g * 512:(g + 1) * 512], in_=psum[pi][:]), [R_ps[pi]], [R_zst[b]])
                sc.dma('pool', lambda e: e.dma_start(out=Y[t * 128:(t + 1) * 128, :], in_=zst[b][:]), [R_zst[b]], [R_Y])
            for t in range(S // 128):
                z_tile(t, t % 2)
        sc.emit()
    return nc


def kernel(**inputs):
    x = np.ascontiguousarray(inputs["x"], dtype=np.float32)
    nb = x.shape[0]
    nc = build(n_layers=4, debug=False)
    hc = host_consts()
    shared = {k: np.ascontiguousarray(v, dtype=np.float32) for k, v in inputs.items() if k not in ("x", "c")}
    in_maps = []
    for b in range(nb):
        m = {"x": x[b], "c": np.ascontiguousarray(inputs["c"][b], dtype=np.float32)}
        m.update(shared); m.update(hc)
        in_maps.append(m)
    res = run_bass_kernel_spmd(nc, in_maps, core_ids=list(range(nb)))
    return np.stack([np.asarray(r["y"], dtype=np.float32) for r in res.results], axis=0)
```

```python
import numpy as np
from contextlib import ExitStack
import concourse.bass as bass
import concourse.mybir as mybir
from concourse.bass_utils import run_bass_kernel_spmd

F32 = mybir.dt.float32
BF16 = mybir.dt.bfloat16
AF = mybir.ActivationFunctionType
ALU = mybir.AluOpType
AX = mybir.AxisListType

ENGS = ('pe', 'act', 'dve', 'pool', 'sp')
NDMASEM = 24


class Res:
    __slots__ = ('w', 'r', 'name', 'excl')

    def __init__(self, name='', excl=False):
        self.w = None
        self.r = {}
        self.name = name
        self.excl = excl


class Sched:
    def __init__(self, nc):
        self.nc = nc
        self.ops = {e: [] for e in ENGS}
        self.cnt = {e: 0 for e in ENGS}
        self.known = {e: {} for e in ENGS}
        self.dcnt = [0] * NDMASEM
        self.dnext = 0
        self.dlast = [None] * NDMASEM
        self.epoch = 0

    def _need(self, eng, tok, waits, isdma=False):
        if tok is None:
            return
        k, v = tok
        if k[0] != 'd' and k[1] < self.epoch:
            return
        if k[0] != 'd' and k[0] == eng and not isdma:
            return
        if self.known[eng].get(k, 0) >= v:
            return
        self.known[eng][k] = v
        waits.append((k, v))

    def _deps(self, eng, reads, writes, isdma=False):
        waits = []
        for r in reads:
            self._need(eng, r.w, waits, isdma or eng != 'pe')
        for w in writes:
            self._need(eng, w.w, waits, isdma)
            for k, v in w.r.items():
                self._need(eng, (k, v), waits, isdma)
        return waits

    def _commit(self, tok, reads, writes):
        k, v = tok
        for r in reads:
            if r.r.get(k, 0) < v:
                r.r[k] = v
        for w in writes:
            w.w = tok
            w.r = {}

    def op(self, eng, fn, reads=(), writes=()):
        ex = [r for r in reads if r.excl]
        if ex:
            reads = [r for r in reads if not r.excl]
            writes = list(writes) + [r for r in ex if r not in writes]
        waits = self._deps(eng, reads, writes)
        self.cnt[eng] += 1
        tok = ((eng, self.epoch), self.cnt[eng])
        self.ops[eng].append((waits, fn, (eng, self.epoch), 1))
        self._commit(tok, reads, writes)

    def dma(self, q, fn, reads=(), writes=()):
        waits = self._deps(q, reads, writes, True)
        i = self.dnext
        self.dnext = (self.dnext + 1) % NDMASEM
        self._need(q, self.dlast[i], waits)
        self.dcnt[i] += 16
        tok = (('d', i), self.dcnt[i])
        self.dlast[i] = tok
        self.ops[q].append((waits, fn, ('d', i), 16))
        self._commit(tok, reads, writes)

    def barrier(self):
        toks = [((e, self.epoch), self.cnt[e]) for e in ENGS if self.cnt[e] > 0]
        toks += [t for t in self.dlast if t is not None]
        for e in ENGS:
            waits = []
            for t in toks:
                self._need(e, t, waits)
            if waits:
                self.ops[e].append((waits, None, None, 0))

    def new_epoch(self):
        self.barrier()
        self.epoch += 1
        self.cnt = {e: 0 for e in ENGS}

    def emit(self):
        nc = self.nc
        with ExitStack() as st:
            sems = {}
            for ep in range(self.epoch + 1):
                for e in ENGS:
                    sems[(e, ep)] = st.enter_context(nc.semaphore('s_%s_%d' % (e, ep)))
            for i in range(NDMASEM):
                sems[('d', i)] = st.enter_context(nc.semaphore('s_d%d' % i))
            block = st.enter_context(nc.Block())
            self.barrier()

            def runner(e):
                def f(eng):
                    for waits, fn, inc, amt in self.ops[e]:
                        for k, v in waits:
                            eng.wait_ge(sems[k], v)
                        if fn is not None:
                            ins = fn(eng)
                            ins.then_inc(sems[inc], amt)
                return f
            block.tensor(runner('pe'))
            block.scalar(runner('act'))
            block.vector(runner('dve'))
            block.gpsimd(runner('pool'))
            block.sync(runner('sp'))


D = 2048; S = 4096; KC = 16; NTG = 8; TG = 512
IN_COLS = 13312
OFF_A = 0; OFF_C = 2304; OFF_B = 4608; OFF_G = 7168
TVLEN = 3072; HKW = 2944

def t5b(n):
    n = np.maximum(n, 0)
    nf = np.maximum(n, 1).astype(np.float32)
    large = 16 + (np.log(nf / np.float32(16)) / np.float32(np.log(128.0)) * np.float32(16)).astype(np.int32)
    large = np.minimum(large, 31)
    return np.where(n < 16, n, large)

NCONST = int(np.max(np.nonzero(t5b(np.arange(5000)) < 31)[0])) + 1

def host_consts():
    oh = np.zeros((4, 33, TVLEN), np.float32)
    m = np.arange(TVLEN); dist = m - 511
    for tab, (span, dil) in enumerate([(None, 1), (128, 1), (128, 4), (128, 16)]):
        valid = dist >= 0 if span is None else ((dist >= 0) & (dist <= span))
        bk = t5b(np.maximum(dist, 0) * dil)
        oh[tab, bk[valid], m[valid]] = 1.0
        oh[tab, 32, m[~valid]] = 1.0
    mc = np.zeros((3, 16, 16), np.float32)
    for qb in range(16):
        for j in range(16):
            mc[0, qb, j] = 0.0 if j < qb else -1e30
            mc[1, qb, j] = 1.0 if j < qb else 0.0
            mc[2, qb, j] = 1.0 if j == qb else 0.0
    sel = np.zeros((16, 16, 128), np.float32)
    for j in range(16):
        sel[j, j, :] = 1.0
    C = 64
    mus = np.triu(np.ones((C, C), np.float32), 1); mls = np.tril(np.ones((C, C), np.float32), -1); mui = np.triu(np.ones((C, C), np.float32), 0)
    rm = np.stack([np.stack([m] * 4, 0) for m in (-mus, -mls, mui, np.eye(C, dtype=np.float32))], 0)
    rm = rm.transpose(2, 0, 1, 3).reshape(C, 4 * 4 * C).copy()
    mc32 = np.repeat(mc, 2, axis=1)
    return {"mconst32": np.ascontiguousarray(mc32), "rwmask": rm, "ohtab": oh, "negrow": np.full((1, 24), -30000.0, np.float32), "mconst": mc,
            "selc": sel.reshape(16, 2048), "jflip": np.eye(128, dtype=np.float32)[::-1].copy(), "ident": np.eye(128, dtype=np.float32)}


def build(n_layers=1, debug=True, NH_A=12, NH_C=4, DO_F=True, DO_E=True, RW_BLOCKS=None, RW_STAGE=3, DO_X0=True, DO_CD=True, DO_Z=True, DO_G=True, NEXP=64, MOE_GROUPS=None):
    nc = bass.Bass("TRN2", target_bir_lowering=False)
    sc = Sched(nc)
    def din(name, shape, dt=F32):
        return nc.dram_tensor(name, list(shape), dt, kind="ExternalInput").ap()
    def dscr(name, shape, dt):
        return nc.dram_tensor(name, list(shape), dt, kind="ExternalOutput" if debug else "Internal").ap()
    if DO_X0:
        x = din("x", [S, D]); c = din("c", [D])
        w_in = din("w_in", [4, D, IN_COLS]); w_ada = din("w_ada", [4, D, 6 * D]); b_ada = din("b_ada", [4, 6 * D])
    ident_d = din("ident", [128, 128])
    XT = dscr("XT", [D, S], F32); R_XT = [Res() for _ in range(NTG)]
    PROJ = dscr("PROJ", [IN_COLS, S], BF16) if DO_X0 else din("PROJ", [IN_COLS, S], BF16); R_PROJ = Res()
    p_a = din("p_a", [4, 768, D]); p_b = din("p_b", [4, 768, D]); p_c = din("p_c", [4, 256, D]);
    rwkv_v0 = din("rwkv_v0", [3, 768]); rwkv_mv_down = din("rwkv_mv_down", [3, 768, 32]); rwkv_mv_up = din("rwkv_mv_up", [3, 32, 768])
    ln2_g = din("ln2_g", [4, D]); ln2_b = din("ln2_b", [4, D])
    router_grp_w = din("router_grp_w", [4, D, 8]); router_grp_b = din("router_grp_b", [4, 8]); router_exp_w = din("router_exp_w", [4, D, 64]); router_exp_b = din("router_exp_b", [4, 64])
    exp_w_gate = din("exp_w_gate", [4, 64, D, 256]); exp_w_up = din("exp_w_up", [4, 64, D, 256]); exp_w_down = din("exp_w_down", [4, 64, 256, D])
    GW = dscr("GW", [64, S], F32); R_GW = Res(); w_o = din("w_o", [4, D, D])
    ln1_g = din("ln1_g", [4, D]); ln1_b = din("ln1_b", [4, D])
    rwkv_mu = din("rwkv_mu", [4, 2560]); rwkv_w0 = din("rwkv_w0", [4, 768]); rwkv_w_up = din("rwkv_w_up", [4, 64, 768])
    rwkv_a0 = din("rwkv_a0", [4, 768]); rwkv_a_up = din("rwkv_a_up", [4, 64, 768]); rwkv_g_up = din("rwkv_g_up", [4, 128, 768])
    rwkv_k_k = din("rwkv_k_k", [4, 768]); rwkv_k_a = din("rwkv_k_a", [4, 768]); rwkv_r_k = din("rwkv_r_k", [4, 768])
    rwkv_ln_g = din("rwkv_ln_g", [4, 768]); rwkv_ln_b = din("rwkv_ln_b", [4, 768])
    rwmask = din("rwmask", [64, 4 * 4 * 64])
    OB = dscr("OB", [768, S], BF16); R_OB = Res()
    VF = dscr("VF", [768, S], F32); R_VF = Res()
    rel_bias = din("rel_bias", [32, 24]); ohtab = din("ohtab", [4, 33, TVLEN]); negrow = din("negrow", [1, 24])
    mconst32 = din("mconst32", [3, 32, 16]); mconst = din("mconst", [3, 16, 16]); selc = din("selc", [16, 16 * 128]); jflip = din("jflip", [128, 128])
    TV = dscr("TV", [4 * 24 * TVLEN], BF16); R_TV = Res()
    OA = dscr("OA", [768, S], BF16); R_OA = Res()
    OC = dscr("OC", [256, S], BF16); R_OC = Res()
    Y = nc.dram_tensor("y", [S, D], F32, kind="ExternalOutput").ap(); R_Y = Res()
    uid = [0]
    with ExitStack() as glob:
        def sb(name, shape, dt, st=glob):
            uid[0] += 1
            return st.enter_context(nc.sbuf_tensor("sb%d_%s" % (uid[0], name), list(shape), dt))
        psum = [glob.enter_context(nc.psum_tensor("ps%d" % i, [128, 512], F32)) for i in range(7)]
        R_ps = [Res(excl=True) for _ in range(8)]
        ident = sb("ident", [128, 128], F32); R_ident = Res()
        identb = sb("identb", [128, 128], BF16)
        sc.dma('sp', lambda e: e.dma_start(out=ident[:], in_=ident_d[:, :]), [], [R_ident])
        sc.op('dve', lambda e: e.tensor_copy(out=identb[:], in_=ident[:]), [R_ident], [R_ident])
        condT = sb("condT", [128, KC], F32); R_cond = Res()
        if DO_X0:
            sc.dma('sp', lambda e: e.dma_start(out=condT[:], in_=c.rearrange("(k p) -> p k", p=128), allow_slow_non_contiguous=True), [], [R_cond])
            sc.op('act', lambda e: e.activation(out=condT[:], in_=condT[:], func=AF.Silu), [R_cond], [R_cond])
        modT = sb("modT", [128, 96], F32); R_mod = Res()
        pscnt = [0]
        def nextps():
            i = pscnt[0] % 7; pscnt[0] += 1
            return i
        psT_b = glob.enter_context(nc.psum_tensor("psTb", [128, 1024], BF16));
        MC = sb("MC", [128, 3, 16, 16], F32); SEL = sb("SEL", [16, 16, 128], BF16); JF = sb("JF", [128, 128], BF16)
        ones_b = sb("ones_b", [128, 128], BF16); ones_f = sb("ones_f", [128, 128], F32); CB = sb("CB", [128, 24], F32)
        rb33 = sb("rb33", [33, 24], F32); R_MC = Res()
        sc.dma('sp', lambda e: e.dma_start(out=MC[:].rearrange("p a b c -> p (a b c)"), in_=bass.AP(mconst.tensor, 0, [[0, 128], [1, 768]])), [], [R_MC])
        sc.dma('pool', lambda e: e.dma_start(out=SEL[:].rearrange("p a b -> p (a b)"), in_=selc[:, :]), [], [R_MC])
        sc.dma('pool', lambda e: e.dma_start(out=JF[:], in_=jflip[:, :]), [], [R_MC])
        sc.dma('sp', lambda e: e.dma_start(out=CB[:], in_=bass.AP(rel_bias.tensor, 31 * 24, [[0, 128], [1, 24]])), [], [R_MC])
        sc.dma('sp', lambda e: e.dma_start(out=rb33[0:32, :], in_=rel_bias[:, :]), [], [R_MC])
        sc.dma('sp', lambda e: e.dma_start(out=rb33[32:33, :], in_=negrow[:, :]), [], [R_MC])
        sc.op('dve', lambda e: e.memset(ones_b[:], 1.0), [], [R_MC])
        sc.op('dve', lambda e: e.memset(ones_f[:], 1.0), [], [R_MC])
        with ExitStack() as ph:
            oh = sb("oh", [33, TVLEN], F32, ph); R_oh = Res()
            tvs = sb("tvs", [24, TVLEN], BF16, ph); R_tvs = Res()
            for tab in range(4):
                sc.dma('sp', lambda e, tab=tab: e.dma_start(out=oh[:], in_=ohtab[tab]), [], [R_oh])
                for ch in range(TVLEN // 512):
                    pi = nextps()
                    sc.op('pe', lambda e, pi=pi, ch=ch: e.matmul(psum[pi][0:24, :], lhsT=rb33[:], rhs=oh[:, ch * 512:(ch + 1) * 512], start=True, stop=True), [R_oh, R_MC], [R_ps[pi]])
                    sc.op('act', lambda e, pi=pi, ch=ch: e.mul(out=tvs[:, ch * 512:(ch + 1) * 512], in_=psum[pi][0:24, :], mul=8.0), [R_ps[pi]], [R_tvs])
                sc.dma('sp', lambda e, tab=tab: e.dma_start(out=bass.AP(TV.tensor, tab * 24 * TVLEN, [[TVLEN, 24], [1, TVLEN]]), in_=tvs[:]), [R_tvs], [R_TV])
        sc.barrier()

        with ExitStack() as ph:
          if DO_X0:
            xin = [sb("xin%d" % i, [128, D], F32, ph) for i in range(2)]; R_xin = [Res(), Res()]
            xst = [sb("xst%d" % i, [128, KC, 128], F32, ph) for i in range(2)]; R_xst = [Res(), Res()]
            for t in range(S // 128):
                b = t % 2
                sc.dma('sp', lambda e, t=t, b=b: e.dma_start(out=xin[b][:], in_=x[t * 128:(t + 1) * 128, :]), [], [R_xin[b]])
                for g in range(4):
                    pi = nextps()
                    for j in range(4):
                        dc = g * 4 + j
                        sc.op('pe', lambda e, pi=pi, j=j, dc=dc, b=b: e.transpose(out=psum[pi][:, j * 128:(j + 1) * 128], in_=xin[b][:, dc * 128:(dc + 1) * 128], identity=ident[:]),
                              [R_xin[b], R_ident], [R_ps[pi]])
                    eng = 'act' if g % 2 == 0 else 'dve'
                    if eng == 'act':
                        sc.op('act', lambda e, pi=pi, g=g, b=b: e.copy(out=xst[b][:, g * 4:(g + 1) * 4, :], in_=psum[pi][:].rearrange("p (j t) -> p j t", j=4)), [R_ps[pi]], [R_xst[b]])
                    else:
                        sc.op('dve', lambda e, pi=pi, g=g, b=b: e.tensor_copy(out=xst[b][:, g * 4:(g + 1) * 4, :], in_=psum[pi][:].rearrange("p (j t) -> p j t", j=4)), [R_ps[pi]], [R_xst[b]])
                sc.dma('pool', lambda e, t=t, b=b: e.dma_start(out=XT.rearrange("(k p) s -> p k s", p=128)[:, :, t * 128:(t + 1) * 128], in_=xst[b][:]), [R_xst[b]], [R_XT[t // 4]])
        sc.barrier()

        def ln_feature_major(xy, R_xy, W, lng, lnb, R_ln, tA, R_tA, tB, R_tB, mean, rstd, R_st):
            ps_s = nextps(); ps_q = nextps()
            for dc in range(KC):
                def one(dc):
                    i = dc % 2
                    sc.op('act', lambda e: e.activation(out=tA[i][:, 0:W], in_=xy[:, dc, :], func=AF.Square), [R_xy], [R_tA[i]])
                    sc.op('pe', lambda e: e.matmul(psum[ps_s][:, 0:W], lhsT=ones_f[:], rhs=xy[:, dc, :], start=(dc == 0), stop=(dc == KC - 1)), [R_xy, R_MC], [R_ps[ps_s]])
                    sc.op('pe', lambda e: e.matmul(psum[ps_q][:, 0:W], lhsT=ones_f[:], rhs=tA[i][:, 0:W], start=(dc == 0), stop=(dc == KC - 1)), [R_tA[i], R_MC], [R_ps[ps_q]])
                one(dc)
            sc.op('act', lambda e: e.mul(out=mean[:, 0:W], in_=psum[ps_s][:, 0:W], mul=1.0 / D), [R_ps[ps_s]], [R_st])
            sc.op('act', lambda e: e.mul(out=rstd[:, 0:W], in_=psum[ps_q][:, 0:W], mul=1.0 / D), [R_ps[ps_q]], [R_st])
            sc.op('dve', lambda e: e.tensor_tensor(out=tB[0][:, 0:W], in0=mean[:, 0:W], in1=mean[:, 0:W], op=ALU.mult), [R_st], [R_tB[0]])
            sc.op('dve', lambda e: e.tensor_tensor(out=rstd[:, 0:W], in0=rstd[:, 0:W], in1=tB[0][:, 0:W], op=ALU.subtract), [R_st, R_tB[0]], [R_st])
            sc.op('dve', lambda e: e.tensor_scalar(out=rstd[:, 0:W], in0=rstd[:, 0:W], scalar1=1e-5, scalar2=None, op0=ALU.add), [R_st], [R_st])
            sc.op('act', lambda e: e.activation(out=rstd[:, 0:W], in_=rstd[:, 0:W], func=AF.Sqrt), [R_st], [R_st])
            sc.op('dve', lambda e: e.reciprocal(out=rstd[:, 0:W], in_=rstd[:, 0:W]), [R_st], [R_st])
            for dc in range(KC):
                def two(dc):
                    i = dc % 2
                    sc.op('dve', lambda e: e.tensor_tensor(out=tA[i][:, 0:W], in0=xy[:, dc, :], in1=mean[:, 0:W], op=ALU.subtract), [R_xy, R_st], [R_tA[i]])
                    sc.op('pool', lambda e: e.tensor_tensor(out=tA[i][:, 0:W], in0=tA[i][:, 0:W], in1=rstd[:, 0:W], op=ALU.mult), [R_tA[i], R_st], [R_tA[i]])
                    sc.op('act', lambda e: e.activation(out=xy[:, dc, :], in_=tA[i][:, 0:W], func=AF.Identity, bias=lnb[:, dc:dc + 1], scale=lng[:, dc:dc + 1]), [R_tA[i], R_ln, R_xy], [R_xy])
                two(dc)

        def layer(l):
            with ExitStack() as ph:
              if DO_X0:
                wa = [sb("wa%d" % i, [128, KC, 512], F32, ph) for i in range(2)]; R_wa = [Res(), Res()]
                bT = sb("bT", [128, 96], F32, ph); R_bT = Res()
                with nc.allow_non_contiguous_dma(reason="small"):
                    sc.dma('sp', lambda e: e.dma_start(out=bT[:], in_=b_ada[l].rearrange("(k p) -> p k", p=128), allow_slow_non_contiguous=True), [], [R_bT])
                pi = nextps()
                for cg in range(24):
                    b = cg % 2
                    sc.dma('sp' if cg % 2 == 0 else 'pool', lambda e, cg=cg, b=b: e.dma_start(out=wa[b][:], in_=w_ada[l].rearrange("(k p) n -> p k n", p=128)[:, :, cg * 512:(cg + 1) * 512]), [], [R_wa[b]])
                    for j in range(4):
                        cc = cg * 4 + j
                        for k in range(KC):
                            sc.op('pe', lambda e, cc=cc, k=k, b=b, j=j: e.matmul(psum[pi][:, cc:cc + 1], lhsT=wa[b][:, k, j * 128:(j + 1) * 128], rhs=condT[:, k:k + 1], start=(k == 0), stop=(k == KC - 1)),
                                  [R_wa[b], R_cond], [R_ps[pi]])
                sc.op('dve', lambda e: e.tensor_tensor(out=modT[:], in0=psum[pi][:, 0:96], in1=bT[:], op=ALU.add), [R_ps[pi], R_bT], [R_mod])
            sc.barrier()
            with ExitStack() as ph:
              if DO_X0:
                hT = sb("hT", [128, KC, S], BF16, ph); R_hT = [Res() for _ in range(NTG)]
                sc1p = sb("sc1p", [128, KC], F32, ph); R_sc1p = Res()
                sc.op('dve', lambda e: e.tensor_scalar(out=sc1p[:], in0=modT[:, 16:32], scalar1=1.0, scalar2=None, op0=ALU.add), [R_mod], [R_sc1p])
                xt = [sb("xt%d" % i, [128, 4, TG], F32, ph) for i in range(2)]; R_xt = [Res(), Res()]
                n = 0
                for tg in range(NTG):
                    for q in range(4):
                        b = n % 2; n += 1
                        sc.dma('sp', lambda e, tg=tg, q=q, b=b: e.dma_start(out=xt[b][:], in_=XT.rearrange("(k p) s -> p k s", p=128)[:, q * 4:(q + 1) * 4, tg * TG:(tg + 1) * TG]), [R_XT[tg]], [R_xt[b]])
                        for j in range(4):
                            dc = q * 4 + j
                            sc.op('dve', lambda e, tg=tg, dc=dc, j=j, b=b: e.tensor_scalar(out=hT[:, dc, tg * TG:(tg + 1) * TG], in0=xt[b][:, j, :], scalar1=sc1p[:, dc:dc + 1], scalar2=modT[:, dc:dc + 1], op0=ALU.mult, op1=ALU.add),
                                  [R_xt[b], R_sc1p, R_mod], [R_hT[tg]])
                wt = [sb("wt%d" % i, [128, KC, 256], BF16, ph) for i in range(2)]; R_wt = [Res(), Res()]
                ost = [sb("ost%d" % i, [128, TG], BF16, ph) for i in range(4)]; R_ost = [Res() for _ in range(4)]
                no = 0
                for cp in range(IN_COLS // 256):
                    b = cp % 2
                    sc.dma('pool', lambda e, cp=cp, b=b: e.dma_start(out=wt[b][:], in_=w_in[l].rearrange("(k p) n -> p k n", p=128)[:, :, cp * 256:(cp + 1) * 256]), [], [R_wt[b]])
                    for j in range(2):
                        col0 = cp * 256 + j * 128
                        isgate = col0 >= OFF_G
                        for tg in range(NTG):
                            pi = nextps()
                            for k in range(KC):
                                sc.op('pe', lambda e, pi=pi, k=k, b=b, j=j, tg=tg: e.matmul(psum[pi][:], lhsT=wt[b][:, k, j * 128:(j + 1) * 128], rhs=hT[:, k, tg * TG:(tg + 1) * TG], start=(k == 0), stop=(k == KC - 1)),
                                      [R_wt[b], R_hT[tg]], [R_ps[pi]])
                            ob = no % 4; no += 1
                            if isgate:
                                sc.op('act', lambda e, pi=pi, ob=ob: e.activation(out=ost[ob][:], in_=psum[pi][:], func=AF.Sigmoid), [R_ps[pi]], [R_ost[ob]])
                            elif no % 2 == 0:
                                sc.op('act', lambda e, pi=pi, ob=ob: e.copy(out=ost[ob][:], in_=psum[pi][:]), [R_ps[pi]], [R_ost[ob]])
                            else:
                                sc.op('dve', lambda e, pi=pi, ob=ob: e.tensor_copy(out=ost[ob][:], in_=psum[pi][:]), [R_ps[pi]], [R_ost[ob]])
                            sc.dma('sp', lambda e, col0=col0, tg=tg, ob=ob: e.dma_start(out=PROJ[col0:col0 + 128, tg * TG:(tg + 1) * TG], in_=ost[ob][:]), [R_ost[ob]], [R_PROJ])
            sc.barrier()
            with ExitStack() as ph:
              if DO_CD:
                QT = [sb("QT%d" % i, [64, S], BF16, ph) for i in range(2)]
                KT = [sb("KT%d" % i, [64, S], BF16, ph) for i in range(2)]
                VT = [sb("VT%d" % i, [64, S], BF16, ph) for i in range(2)]
                Vt = [sb("Vt%d" % i, [128, 32, 64], BF16, ph) for i in range(2)]
                HK = [sb("HK%d" % i, [128, HKW], BF16, ph) for i in range(2)]
                R_hd = [Res(), Res()]; R_Vt = [Res(), Res()]; R_HK = [Res(), Res()]
                kmf = sb("kmf", [64, 16], F32, ph); kmhi = sb("kmhi", [64, 16], BF16, ph); kmlo = sb("kmlo", [64, 16], BF16, ph); R_km = Res()
                maskT = [sb("maskT%d" % i, [16, 512], BF16, ph) for i in range(2)]; R_maskT = [Res(), Res()]
                gm = sb("gm", [128, 16], F32, ph); top8 = sb("top8", [128, 8], F32, ph); R_gm = Res()
                PT = [sb("PT%d" % i, [128, 512], BF16, ph) for i in range(3)]; R_PT = [Res() for _ in range(3)]
                rl = sb("rl", [64, 512], F32, ph); R_rl = Res()
                ostg = [sb("ostg%d" % i, [64, 512], BF16, ph) for i in range(2)]; R_ostg = [Res(), Res()]
                npt = [0]; nsb = [0]

                def load_head(b, qrow, krow, vrow, tab, hidx):
                    for (T_, row) in ((QT, qrow), (KT, krow), (VT, vrow)):
                        sc.dma('sp', lambda e, T_=T_, row=row: e.dma_start(out=T_[b][:], in_=PROJ[row:row + 64, :]), [R_PROJ], [R_hd[b]])
                    off = (tab * 24 + hidx) * TVLEN
                    sc.dma('sp', lambda e: e.dma_start(out=HK[b][:], in_=bass.AP(TV.tensor, off, [[1, 128], [1, HKW]])), [R_TV], [R_HK[b]])

                def make_V(b, src, R_src):
                    for g4 in range(8):
                        for j in range(4):
                            t = g4 * 4 + j
                            sc.op('pe', lambda e, t=t, j=j: e.transpose(out=psT_b[:, j * 64:(j + 1) * 64], in_=src[:, t * 128:(t + 1) * 128], identity=identb[0:64, 0:64]), [R_src, R_ident], [R_ps[7]])
                        sc.op('dve', lambda e, g4=g4: e.tensor_copy(out=Vt[b][:, g4 * 4:(g4 + 1) * 4, :], in_=psT_b[:, 0:256].rearrange("p (j d) -> p j d", j=4)), [R_ps[7]], [R_Vt[b]])

                MC32 = sb("MC32", [128, 3, 32, 16], F32, ph); gmA = sb("gmA", [128, 32, 16], F32, ph); top8a = sb("top8a", [128, 32, 8], F32, ph)
                maskTall = sb("maskTall", [16, S], BF16, ph); R_mT = Res(); R_gmA = Res()
                sc.dma('sp', lambda e: e.dma_start(out=MC32[:].rearrange("p a b c -> p (a b c)"), in_=bass.AP(mconst32.tensor, 0, [[0, 128], [1, 3 * 32 * 16]])), [], [R_MC])

                def moba_gate_all(b):
                    for qt in range(32):
                        sc.op('pe', lambda e, qt=qt: e.matmul(psum[6][:, qt * 16:(qt + 1) * 16], lhsT=QT[b][:, qt * 128:(qt + 1) * 128], rhs=kmhi[:], start=(qt == 0), stop=False), [R_hd[b], R_km], [R_ps[6]])
                        sc.op('pe', lambda e, qt=qt: e.matmul(psum[6][:, qt * 16:(qt + 1) * 16], lhsT=QT[b][:, qt * 128:(qt + 1) * 128], rhs=kmlo[:], start=False, stop=(qt == 31)), [R_hd[b], R_km], [R_ps[6]])
                    sc.op('dve', lambda e: e.tensor_tensor(out=gmA[:], in0=psum[6][:].rearrange("p (t j) -> p t j", j=16), in1=MC32[:, 0, :, :], op=ALU.add), [R_ps[6], R_MC], [R_gmA])
                    for qt in range(32):
                        sc.op('dve', lambda e, qt=qt: e.max(out=top8a[:, qt, :], in_=gmA[:, qt, :]), [R_gmA], [R_gm])
                    for qt in range(32):
                        sc.op('dve', lambda e, qt=qt: e.tensor_scalar(out=gmA[:, qt, :], in0=gmA[:, qt, :], scalar1=top8a[:, qt, 2:3], scalar2=None, op0=ALU.is_ge), [R_gmA, R_gm], [R_gmA])
                    sc.op('dve', lambda e: e.tensor_tensor(out=gmA[:], in0=gmA[:], in1=MC32[:, 1, :, :], op=ALU.mult), [R_gmA, R_MC], [R_gmA])
                    sc.op('dve', lambda e: e.tensor_tensor(out=gmA[:], in0=gmA[:], in1=MC32[:, 2, :, :], op=ALU.add), [R_gmA, R_MC], [R_gmA])
                    sc.op('dve', lambda e: e.tensor_scalar(out=gmA[:], in0=gmA[:], scalar1=-1.0, scalar2=240000.0, op0=ALU.add, op1=ALU.mult), [R_gmA], [R_gmA])
                    for g in range(8):
                        for j in range(4):
                            qt = g * 4 + j
                            sc.op('pe', lambda e, qt=qt, j=j: e.transpose(out=psum[6][0:16, j * 128:(j + 1) * 128], in_=gmA[:, qt, :], identity=ident[:]), [R_gmA, R_ident], [R_ps[6]])
                        sc.op('act', lambda e, g=g: e.copy(out=maskTall[:, g * 512:(g + 1) * 512], in_=psum[6][0:16, :]), [R_ps[6]], [R_mT])

                def moba_tile(h, b, mb, QG, kt, nk, pO, pL):
                    D0 = QG * 512 - kt * 128
                    far = (D0 - 127) >= NCONST
                    pS = npt[0] % 3; pb = npt[0] % 3; npt[0] += 1
                    sc.op('pe', lambda e: e.matmul(psum[pS][:], lhsT=KT[b][:, kt * 128:(kt + 1) * 128], rhs=QT[b][:, QG * 512:(QG + 1) * 512], start=True, stop=False), [R_hd[b]], [R_ps[pS]])
                    sc.op('pe', lambda e: e.matmul(psum[pS][:], lhsT=SEL[:, kt // 2, :], rhs=maskTall[:, QG * 512:(QG + 1) * 512], start=False, stop=far), [R_mT, R_MC], [R_ps[pS]])
                    if not far:
                        sc.op('pe', lambda e: e.matmul(psum[pS][:], lhsT=JF[:], rhs=HK[b][:, D0 + 384:D0 + 384 + 512], start=False, stop=True), [R_HK[b], R_MC], [R_ps[pS]])
                        sc.op('act', lambda e: e.activation(out=PT[pb][:], in_=psum[pS][:], func=AF.Exp, scale=0.125), [R_ps[pS]], [R_PT[pb]])
                    else:
                        sc.op('act', lambda e: e.activation(out=PT[pb][:], in_=psum[pS][:], func=AF.Exp, scale=0.125, bias=CB[:, h:h + 1]), [R_ps[pS], R_MC], [R_PT[pb]])
                    sc.op('pe', lambda e: e.matmul(psum[pO][0:64, :], lhsT=Vt[b][:, kt, :], rhs=PT[pb][:], start=(kt == 0), stop=(kt == nk - 1)), [R_Vt[b], R_PT[pb]], [R_ps[pO]])
                    sc.op('pe', lambda e: e.matmul(psum[pL][0:64, :], lhsT=ones_b[:, 0:64], rhs=PT[pb][:], start=(kt == 0), stop=(kt == nk - 1)), [R_PT[pb], R_MC], [R_ps[pL]])

                def moba_qg(h, b, QG):
                    mb = QG % 2
                    pO = 3 + (QG % 2); pL = 5
                    nk = 4 * QG + 4
                    for kt in range(nk):
                        moba_tile(h, b, mb, QG, kt, nk, pO, pL)
                    ob = nsb[0] % 2; nsb[0] += 1
                    sc.op('dve', lambda e: e.reciprocal(out=rl[:], in_=psum[pL][0:64, :]), [R_ps[pL]], [R_rl])
                    sc.op('dve', lambda e: e.tensor_tensor(out=ostg[ob][:], in0=psum[pO][0:64, :], in1=rl[:], op=ALU.mult), [R_ps[pO], R_rl], [R_ostg[ob]])
                    sc.dma('sp', lambda e: e.dma_start(out=OA[h * 64:(h + 1) * 64, QG * 512:(QG + 1) * 512], in_=ostg[ob][:]), [R_ostg[ob]], [R_OA])

                def moba_head(h, b):
                    load_head(b, OFF_A + h * 64, OFF_A + 768 + h * 64, OFF_A + 1536 + h * 64, 0, h)
                    make_V(b, VT[b], R_hd[b])
                    sc.op('dve', lambda e: e.tensor_reduce(out=kmf[:], in_=KT[b][:].rearrange("p (j k) -> p j k", k=256), axis=AX.X, op=ALU.add), [R_hd[b]], [R_km])
                    sc.op('dve', lambda e: e.tensor_scalar(out=kmf[:], in0=kmf[:], scalar1=1.0 / 256, scalar2=None, op0=ALU.mult), [R_km], [R_km])
                    sc.op('dve', lambda e: e.tensor_copy(out=kmhi[:], in_=kmf[:]), [R_km], [R_km])
                    sc.op('dve', lambda e: e.tensor_tensor(out=kmlo[:], in0=kmf[:], in1=kmhi[:], op=ALU.subtract), [R_km], [R_km])
                    moba_gate_all(b)
                    for QG in range(8):
                        moba_qg(h, b, QG)

                for h in range(NH_A):
                    moba_head(h, h % 2)

                QP = sb("QP", [64, S], BF16, ph); KP = sb("KP", [64, S], BF16, ph); VP = sb("VP", [64, S], BF16, ph); R_perm = Res()
                Og = sb("Og", [64, S], F32, ph); Lg = sb("Lg", [64, S], F32, ph); R_OL = Res()
                Os = sb("Os", [64, S], F32, ph); Ls = sb("Ls", [64, S], F32, ph); R_sum = Res()
                obig = sb("obig", [64, S], BF16, ph)

                def dil_qtile(b, qt, tps, g4, jj, srcQ, srcK, R_src, pO, pL):
                    kts = ([qt - 1] if qt % tps != 0 else []) + [qt]
                    for i, kt in enumerate(kts):
                        D0 = (qt - kt) * 128
                        pS = npt[0] % 3; pb = npt[0] % 3; npt[0] += 1
                        sc.op('pe', lambda e, kt=kt, pS=pS: e.matmul(psum[pS][:, 0:128], lhsT=srcK[:, kt * 128:(kt + 1) * 128], rhs=srcQ[:, qt * 128:(qt + 1) * 128], start=True, stop=False), [R_src], [R_ps[pS]])
                        sc.op('pe', lambda e, pS=pS, D0=D0: e.matmul(psum[pS][:, 0:128], lhsT=JF[:], rhs=HK[b][:, D0 + 384:D0 + 384 + 128], start=False, stop=True), [R_HK[b], R_MC], [R_ps[pS]])
                        sc.op('act', lambda e, pS=pS, pb=pb: e.activation(out=PT[pb][:, 0:128], in_=psum[pS][:, 0:128], func=AF.Exp, scale=0.125), [R_ps[pS]], [R_PT[pb]])
                        first = (i == 0) and (jj == 0)
                        sc.op('pe', lambda e, kt=kt, pb=pb, first=first: e.matmul(psum[pO][0:64, jj * 128:(jj + 1) * 128], lhsT=Vt[b][:, kt, :], rhs=PT[pb][:, 0:128], start=first, stop=(kt == qt)), [R_Vt[b], R_PT[pb]], [R_ps[pO]])
                        sc.op('pe', lambda e, kt=kt, pb=pb, first=first: e.matmul(psum[pL][0:64, jj * 128:(jj + 1) * 128], lhsT=ones_b[:, 0:64], rhs=PT[pb][:, 0:128], start=first, stop=(kt == qt)), [R_PT[pb], R_MC], [R_ps[pL]])

                def dil_head(g, j, b):
                    dil = (1, 4, 16)[g]
                    hc = g * 4 + j
                    load_head(b, OFF_C + hc * 64, OFF_C + 768 + hc * 64, OFF_C + 1536 + hc * 64, 1 + g, 12 + hc)
                    if dil > 1:
                        for (src, dst, eng) in ((QT[b], QP, 'pool'), (KT[b], KP, 'pool'), (VT[b], VP, 'act')):
                            if eng == 'pool':
                                sc.op('pool', lambda e, src=src, dst=dst: e.tensor_copy(out=dst[:].rearrange("p (r n) -> p r n", r=dil), in_=src[:].rearrange("p (n r) -> p r n", r=dil)), [R_hd[b]], [R_perm])
                            else:
                                sc.op('act', lambda e, src=src, dst=dst: e.copy(out=dst[:].rearrange("p (r n) -> p r n", r=dil), in_=src[:].rearrange("p (n r) -> p r n", r=dil)), [R_hd[b]], [R_perm])
                        sQ, sK, sV, R_src = QP, KP, VP, R_perm
                    else:
                        sQ, sK, sV, R_src = QT[b], KT[b], VT[b], R_hd[b]
                    make_V(b, sV, R_src)
                    tps = (S // dil) // 128
                    for g4 in range(8):
                        pO = 3 + (g4 % 2); pL = 5 + (g4 % 2)
                        for jj in range(4):
                            dil_qtile(b, g4 * 4 + jj, tps, g4, jj, sQ, sK, R_src, pO, pL)
                        sc.op('act', lambda e, g4=g4, pO=pO: e.copy(out=Og[:, g4 * 512:(g4 + 1) * 512], in_=psum[pO][0:64, :]), [R_ps[pO]], [R_OL])
                        sc.op('dve', lambda e, g4=g4, pL=pL: e.tensor_copy(out=Lg[:, g4 * 512:(g4 + 1) * 512], in_=psum[pL][0:64, :]), [R_ps[pL]], [R_OL])
                    if g == 0:
                        sc.op('dve', lambda e: e.tensor_copy(out=Os[:], in_=Og[:]), [R_OL], [R_sum])
                        sc.op('pool', lambda e: e.tensor_copy(out=Ls[:], in_=Lg[:]), [R_OL], [R_sum])
                    else:
                        sc.op('dve', lambda e: e.tensor_tensor(out=Os[:].rearrange("p (n r) -> p r n", r=dil), in0=Os[:].rearrange("p (n r) -> p r n", r=dil), in1=Og[:].rearrange("p (r n) -> p r n", r=dil), op=ALU.add), [R_OL], [R_sum])
                        sc.op('pool', lambda e: e.tensor_tensor(out=Ls[:].rearrange("p (n r) -> p r n", r=dil), in0=Ls[:].rearrange("p (n r) -> p r n", r=dil), in1=Lg[:].rearrange("p (r n) -> p r n", r=dil), op=ALU.add), [R_OL], [R_sum])

                def dil_slot(j):
                    for g in range(3):
                        dil_head(g, j, (j * 3 + g) % 2)
                    sc.op('dve', lambda e: e.reciprocal(out=Ls[:], in_=Ls[:]), [R_sum], [R_sum])
                    sc.op('dve', lambda e: e.tensor_tensor(out=obig[:], in0=Os[:], in1=Ls[:], op=ALU.mult), [R_sum], [R_sum])
                    sc.dma('sp', lambda e: e.dma_start(out=OC[j * 64:(j + 1) * 64, :], in_=obig[:]), [R_sum], [R_OC])

                for j in range(NH_C):
                    dil_slot(j)
            sc.barrier()
            if DO_E:
                RB = 256; NCH = 4; NBLK = S // RB
                with ExitStack() as ph:
                    def prm(name, src, n=12):
                        t = sb(name, [64, n], F32, ph)
                        sc.dma('sp', lambda e: e.dma_start(out=t[:], in_=src.rearrange("(h d) -> d h", d=64), allow_slow_non_contiguous=True), [], [R_prm])
                        return t
                    R_prm = Res()
                    mu_r = prm("mu_r", rwkv_mu[l, 0:768]); mu_k = prm("mu_k", rwkv_mu[l, 768:1536]); mu_v = prm("mu_v", rwkv_mu[l, 1536:2304])
                    mu_w = prm("mu_w", rwkv_mu[l, 2304:2368], 1); mu_a = prm("mu_a", rwkv_mu[l, 2368:2432], 1)
                    mu_g = sb("mu_g", [128, 1], F32, ph)
                    sc.dma('sp', lambda e: e.dma_start(out=mu_g[:], in_=rwkv_mu[l, 2432:2560].rearrange("(h d) -> d h", d=128), allow_slow_non_contiguous=True), [], [R_prm])
                    w0 = prm("w0", rwkv_w0[l]); a0 = prm("a0", rwkv_a0[l]); k_k = prm("k_k", rwkv_k_k[l]); k_a = prm("k_a", rwkv_k_a[l])
                    r_k = prm("r_k", rwkv_r_k[l]); ln_g = prm("ln_g", rwkv_ln_g[l]); ln_b = prm("ln_b", rwkv_ln_b[l])
                    omka = sb("omka", [64, 12], F32, ph)
                    sc.op('dve', lambda e: e.tensor_scalar(out=omka[:], in0=k_a[:], scalar1=-1.0, scalar2=1.0, op0=ALU.mult, op1=ALU.add), [R_prm], [R_prm])
                    wup = sb("wup", [64, 768], BF16, ph); aup = sb("aup", [64, 768], BF16, ph); gup = sb("gup", [128, 768], BF16, ph)
                    sc.dma('pool', lambda e: e.dma_start(out=wup[:], in_=rwkv_w_up[l]), [], [R_prm])
                    sc.dma('pool', lambda e: e.dma_start(out=aup[:], in_=rwkv_a_up[l]), [], [R_prm])
                    sc.dma('pool', lambda e: e.dma_start(out=gup[:], in_=rwkv_g_up[l]), [], [R_prm])
                    RM = sb("RM", [64, 4, 4, 64], F32, ph); I4 = sb("I4", [64, 4, 64], BF16, ph)
                    sc.dma('sp', lambda e: e.dma_start(out=RM[:].rearrange("p a b c -> p (a b c)"), in_=rwmask[:, :]), [], [R_prm])
                    sc.op('dve', lambda e: e.tensor_copy(out=I4[:], in_=RM[:, 3, :, :]), [R_prm], [R_prm])
                    QtT = sb("QtT", [64, 12, RB], BF16, ph); RtT = sb("RtT", [64, 12, RB], BF16, ph); KhT = sb("KhT", [64, 12, RB], BF16, ph)
                    BhT = sb("BhT", [64, 12, RB], BF16, ph); KbT = sb("KbT", [64, 12, RB], BF16, ph); BbT = sb("BbT", [64, 12, RB], BF16, ph)
                    VTb = sb("VTb", [64, 12, RB], BF16, ph); Gg = sb("Gg", [64, 12, RB], BF16, ph); BON = sb("BON", [64, 12, RB], BF16, ph)
                    GC = sb("GC", [64, 12, NCH], F32, ph); yT = sb("yT", [64, 12, RB], F32, ph)
                    R_opsH = [Res() for _ in range(12)]; R_yT = [Res() for _ in range(3)]
                    Pf = sb("Pf", [64, 12, 64], F32, ph); Pb = sb("Pb", [64, 12, 64], BF16, ph); R_P = [Res() for _ in range(3)]
                    sc.op('dve', lambda e: e.memset(Pf[:], 0.0), [], R_P)
                    sc.op('dve', lambda e: e.memset(Pb[:], 0.0), [], R_P)
                    zr = [sb("zr%d" % i, [64, 12, RB + 1], BF16, ph) for i in range(3)]; R_z = [Res() for _ in range(3)]
                    zlo = sb("zlo", [128, 2, RB + 1], BF16, ph); zgl = sb("zgl", [128, RB + 1], BF16, ph); R_zlo = Res()
                    wl = sb("wl", [64, RB], BF16, ph); al = sb("al", [64, RB], BF16, ph); gl = sb("gl", [128, RB], BF16, ph); R_lo = Res()
                    T = [sb("T%d" % i, [64, RB], F32, ph) for i in range(12)]; R_T = [Res() for _ in range(12)]
                    MZ = sb("MZ", [64, 4, 192], BF16, ph); R_MZ = Res()
                    SC = sb("SC", [64, 3, 4, 64], BF16, ph); R_SC = Res()
                    TM = sb("TM", [64, 4, 192], BF16, ph); R_TM = Res()
                    Xs = sb("Xs", [64, 4, 64], BF16, ph); Un = sb("Un", [64, 4, 64], BF16, ph); R_X = Res(); R_U = Res()
                    o1 = ones_f[0:64, 0:64]
                    pA, pB, pC, pD, pE, pF, pG = range(7)
                    ppre = [0]

                    def pre_ps():
                        i = ppre[0] % 2; ppre[0] += 1
                        return (pC, pE)[i]

                    def shift(eng, out, zt, hsl, mu_ap, tmp, R_src, R_tmp, R_out):
                        sc.op('dve', lambda e: e.tensor_tensor(out=tmp, in0=zt[hsl + (slice(0, RB),)], in1=zt[hsl + (slice(1, RB + 1),)], op=ALU.subtract), [R_src], [R_tmp])
                        sc.op(eng, lambda e: e.scalar_tensor_tensor(out=out, in0=tmp, scalar=mu_ap, in1=zt[hsl + (slice(1, RB + 1),)], op0=ALU.mult, op1=ALU.add), [R_src, R_tmp, R_prm], [R_out])

                    def load_block(blk):
                        t0 = blk * RB
                        for i, off in enumerate((OFF_B, OFF_B + 768, OFF_B + 1536)):
                            src = PROJ[off:off + 768, :].rearrange("(h d) s -> d h s", d=64)
                            if blk == 0:
                                sc.op('pool', lambda e, i=i: e.memset(zr[i][:, :, 0:1], 0.0), [], [R_z[i]])
                                sc.dma('sp', lambda e, i=i, src=src: e.dma_start(out=zr[i][:, :, 1:RB + 1], in_=src[:, :, 0:RB]), [R_PROJ], [R_z[i]])
                            else:
                                sc.dma('sp', lambda e, i=i, src=src: e.dma_start(out=zr[i][:], in_=src[:, :, t0 - 1:t0 + RB]), [R_PROJ], [R_z[i]])
                        lo0 = OFF_B + 2304
                        if blk == 0:
                            sc.op('pool', lambda e: e.memset(zlo[:, :, 0:1], 0.0), [], [R_zlo])
                            sc.op('pool', lambda e: e.memset(zgl[:, 0:1], 0.0), [], [R_zlo])
                            sc.dma('sp', lambda e: e.dma_start(out=zlo[0:64, 0, 1:RB + 1], in_=PROJ[lo0:lo0 + 64, 0:RB]), [R_PROJ], [R_zlo])
                            sc.dma('sp', lambda e: e.dma_start(out=zlo[0:64, 1, 1:RB + 1], in_=PROJ[lo0 + 64:lo0 + 128, 0:RB]), [R_PROJ], [R_zlo])
                            sc.dma('sp', lambda e: e.dma_start(out=zgl[:, 1:RB + 1], in_=PROJ[lo0 + 128:lo0 + 256, 0:RB]), [R_PROJ], [R_zlo])
                        else:
                            sc.dma('sp', lambda e: e.dma_start(out=zlo[0:64, 0, :], in_=PROJ[lo0:lo0 + 64, t0 - 1:t0 + RB]), [R_PROJ], [R_zlo])
                            sc.dma('sp', lambda e: e.dma_start(out=zlo[0:64, 1, :], in_=PROJ[lo0 + 64:lo0 + 128, t0 - 1:t0 + RB]), [R_PROJ], [R_zlo])
                            sc.dma('sp', lambda e: e.dma_start(out=zgl[:], in_=PROJ[lo0 + 128:lo0 + 256, t0 - 1:t0 + RB]), [R_PROJ], [R_zlo])
                        shift('dve', T[0][:], zlo, (slice(0, 64), 0), mu_w[:, 0:1], T[1][:], R_zlo, R_T[1], R_T[0])
                        sc.op('act', lambda e: e.activation(out=wl[:], in_=T[0][:], func=AF.Tanh), [R_T[0]], [R_lo])
                        shift('dve', al[:], zlo, (slice(0, 64), 1), mu_a[:, 0:1], T[1][:], R_zlo, R_T[1], R_lo)
                        sc.op('dve', lambda e: e.tensor_tensor(out=T128[:], in0=zgl[:, 0:RB], in1=zgl[:, 1:RB + 1], op=ALU.subtract), [R_zlo], [R_T128])
                        sc.op('dve', lambda e: e.scalar_tensor_tensor(out=T128[:], in0=T128[:], scalar=mu_g[:, 0:1], in1=zgl[:, 1:RB + 1], op0=ALU.mult, op1=ALU.add), [R_zlo, R_T128, R_prm], [R_T128])
                        sc.op('act', lambda e: e.activation(out=gl[:], in_=T128[:], func=AF.Sigmoid), [R_T128], [R_lo])

                    T128 = sb("T128", [128, RB], F32, ph); R_T128 = Res()

                    def pre_head(blk, h):
                        t0 = blk * RB
                        hs = slice(h * 64, (h + 1) * 64)
                        tr, tk, tv, ta, tld, tlg, tkap, tkt, tb, tx, ty, tz = T
                        Rr, Rk, Rv, Ra, Rld, Rlg, Rkap, Rkt, Rb, Rx, Ry, Rz = R_T
                        hsl = (slice(0, 64), h)
                        shift('dve', tr[:], zr[0], hsl, mu_r[:, h:h + 1], tx[:], R_z[0], Rx, Rr)
                        shift('dve', tk[:], zr[1], hsl, mu_k[:, h:h + 1], ty[:], R_z[1], Ry, Rk)
                        sc.op('pool', lambda e: e.tensor_copy(out=tv[:], in_=V32[:, h, :]), [R_V32], [Rv])
                        p1 = pre_ps()
                        sc.op('pe', lambda e: e.matmul(psum[p1][0:64, 256:512], lhsT=wup[:, hs], rhs=wl[:], start=True, stop=True), [R_lo, R_prm], [R_ps[p1]])
                        sc.op('act', lambda e: e.activation(out=tld[:], in_=psum[p1][0:64, 256:512], func=AF.Sigmoid, bias=w0[:, h:h + 1]), [R_ps[p1], R_prm], [Rld])
                        sc.op('dve', lambda e: e.tensor_scalar(out=tld[:], in0=tld[:], scalar1=-0.6065306597126334, scalar2=None, op0=ALU.mult), [Rld], [Rld])
                        p2 = pre_ps()
                        sc.op('pe', lambda e: e.matmul(psum[p2][0:64, 256:512], lhsT=aup[:, hs], rhs=al[:], start=True, stop=True), [R_lo, R_prm], [R_ps[p2]])
                        sc.op('act', lambda e: e.activation(out=ta[:], in_=psum[p2][0:64, 256:512], func=AF.Sigmoid, bias=a0[:, h:h + 1]), [R_ps[p2], R_prm], [Ra])
                        p3 = pre_ps()
                        sc.op('pe', lambda e: e.matmul(psum[p3][0:64, 256:512], lhsT=gup[:, hs], rhs=gl[:], start=True, stop=True), [R_lo, R_prm], [R_ps[p3]])
                        sc.op('act', lambda e: e.copy(out=Gg[:, h, :], in_=psum[p3][0:64, 256:512]), [R_ps[p3]], [R_opsH[h]])
                        sc.op('dve', lambda e: e.tensor_scalar(out=tkap[:], in0=tk[:], scalar1=k_k[:, h:h + 1], scalar2=None, op0=ALU.mult), [Rk, R_prm], [Rkap])
                        sc.op('act', lambda e: e.activation(out=tx[:], in_=tkap[:], func=AF.Square), [Rkap], [Rx])
                        p4 = pre_ps()
                        sc.op('pe', lambda e: e.matmul(psum[p4][0:64, 256:512], lhsT=o1, rhs=tx[:], start=True, stop=True), [Rx, R_MC], [R_ps[p4]])
                        sc.op('act', lambda e: e.activation(out=tx[:], in_=psum[p4][0:64, 256:512], func=AF.Sqrt), [R_ps[p4]], [Rx])
                        sc.op('dve', lambda e: e.tensor_scalar(out=tx[:], in0=tx[:], scalar1=1e-12, scalar2=None, op0=ALU.max), [Rx], [Rx])
                        sc.op('dve', lambda e: e.reciprocal(out=tx[:], in_=tx[:]), [Rx], [Rx])
                        sc.op('dve', lambda e: e.tensor_tensor(out=tkap[:], in0=tkap[:], in1=tx[:], op=ALU.mult), [Rkap, Rx], [Rkap])
                        sc.op('dve', lambda e: e.tensor_scalar(out=ty[:], in0=ta[:], scalar1=k_a[:, h:h + 1], scalar2=omka[:, h:h + 1], op0=ALU.mult, op1=ALU.add), [Ra, R_prm], [Ry])
                        sc.op('pool', lambda e: e.tensor_tensor(out=tkt[:], in0=tk[:], in1=ty[:], op=ALU.mult), [Rk, Ry], [Rkt])
                        sc.op('pool', lambda e: e.tensor_tensor(out=tb[:], in0=tkap[:], in1=ta[:], op=ALU.mult), [Rkap, Ra], [Rb])
                        sc.op('dve', lambda e: e.tensor_tensor(out=tz[:], in0=tr[:], in1=tkt[:], op=ALU.mult), [Rr, Rkt], [Rz])
                        sc.op('dve', lambda e: e.tensor_scalar(out=tz[:], in0=tz[:], scalar1=r_k[:, h:h + 1], scalar2=None, op0=ALU.mult), [Rz, R_prm], [Rz])
                        p5 = pre_ps()
                        sc.op('pe', lambda e: e.matmul(psum[p5][0:64, 256:512], lhsT=o1, rhs=tz[:], start=True, stop=True), [Rz, R_MC], [R_ps[p5]])
                        sc.op('dve', lambda e: e.tensor_tensor(out=BON[:, h, :], in0=psum[p5][0:64, 256:512], in1=tv[:], op=ALU.mult), [R_ps[p5], Rv], [R_opsH[h]])
                        for c in range(NCH):
                            cs = slice(c * 64, (c + 1) * 64)
                            sc.op('dve', lambda e, cs=cs: e.tensor_tensor_scan(out=tlg[:, cs], data0=ones_f[0:64, 0:64], data1=tld[:, cs], initial=0.0, op0=ALU.mult, op1=ALU.add), [Rld, R_MC], [Rlg])
                        sc.op('act', lambda e: e.activation(out=tx[:], in_=tlg[:], func=AF.Exp), [Rlg], [Rx])
                        sc.op('act', lambda e: e.activation(out=ty[:], in_=tlg[:], func=AF.Exp, scale=-1.0), [Rlg], [Ry])
                        sc.op('dve', lambda e: e.tensor_tensor(out=tz[:], in0=tlg[:], in1=tld[:], op=ALU.subtract), [Rlg, Rld], [Rz])
                        sc.op('act', lambda e: e.activation(out=tz[:], in_=tz[:], func=AF.Exp), [Rz], [Rz])
                        sc.op('dve', lambda e: e.tensor_copy(out=GC[:, h, :], in_=tx[:].rearrange("p (c t) -> p c t", t=64)[:, :, 63]), [Rx], [R_opsH[h]])
                        sc.op('dve', lambda e: e.tensor_tensor(out=QtT[:, h, :], in0=tkap[:], in1=tz[:], op=ALU.mult), [Rkap, Rz], [R_opsH[h]])
                        sc.op('pool', lambda e: e.tensor_tensor(out=RtT[:, h, :], in0=tr[:], in1=tx[:], op=ALU.mult), [Rr, Rx], [R_opsH[h]])
                        sc.op('dve', lambda e: e.tensor_tensor(out=KhT[:, h, :], in0=tkt[:], in1=ty[:], op=ALU.mult), [Rkt, Ry], [R_opsH[h]])
                        sc.op('pool', lambda e: e.tensor_tensor(out=BhT[:, h, :], in0=tb[:], in1=ty[:], op=ALU.mult), [Rb, Ry], [R_opsH[h]])
                        for c in range(NCH):
                            cs = slice(c * 64, (c + 1) * 64)
                            sc.op('dve', lambda e, cs=cs, c=c: e.tensor_scalar(out=ty[:, cs], in0=ty[:, cs], scalar1=tx[:, c * 64 + 63:c * 64 + 64], scalar2=None, op0=ALU.mult), [Ry, Rx], [Ry])
                        sc.op('dve', lambda e: e.tensor_tensor(out=KbT[:, h, :], in0=tkt[:], in1=ty[:], op=ALU.mult), [Rkt, Ry], [R_opsH[h]])
                        sc.op('pool', lambda e: e.tensor_tensor(out=BbT[:, h, :], in0=tb[:], in1=ty[:], op=ALU.mult), [Rb, Ry], [R_opsH[h]])
                        sc.op('act', lambda e: e.copy(out=VTb[:, h, :], in_=tv[:]), [Rv], [R_opsH[h]])

                    def chunk_group(blk, c, hg):
                        cs = slice(c * 64, (c + 1) * 64)
                        hh = [hg * 4 + q for q in range(4)]
                        Rh = [R_opsH[h] for h in hh]
                        for q, h in enumerate(hh):
                            sc.op('pe', lambda e, q=q, h=h: e.matmul(psum[pA][0:64, q * 128:q * 128 + 64], lhsT=BhT[:, h, cs], rhs=QtT[:, h, cs], start=True, stop=True), [Rh[q]], [R_ps[pA]])
                            sc.op('pe', lambda e, q=q, h=h: e.matmul(psum[pA][0:64, q * 128 + 64:q * 128 + 128], lhsT=QtT[:, h, cs], rhs=BhT[:, h, cs], start=True, stop=True), [Rh[q]], [R_ps[pA]])
                            sc.op('pe', lambda e, q=q, h=h: e.matmul(psum[pB][0:64, q * 128:q * 128 + 64], lhsT=KhT[:, h, cs], rhs=QtT[:, h, cs], start=True, stop=True), [Rh[q]], [R_ps[pB]])
                            sc.op('pe', lambda e, q=q, h=h: e.matmul(psum[pB][0:64, q * 128 + 64:q * 128 + 128], lhsT=KhT[:, h, cs], rhs=RtT[:, h, cs], start=True, stop=True), [Rh[q]], [R_ps[pB]])
                            sc.op('pe', lambda e, q=q, h=h: e.matmul(psum[pC][0:64, q * 64:q * 64 + 64], lhsT=BhT[:, h, cs], rhs=RtT[:, h, cs], start=True, stop=True), [Rh[q]], [R_ps[pC]])
                        vA = psum[pA][0:64, :].rearrange("p (q x) -> p q x", x=128)
                        vB = psum[pB][0:64, :].rearrange("p (q x) -> p q x", x=128)
                        vC = psum[pC][0:64, 0:256].rearrange("p (q x) -> p q x", x=64)
                        sc.op('dve', lambda e: e.tensor_tensor(out=MZ[:, :, 0:64], in0=vA[:, :, 0:64], in1=RM[:, 0, :, :], op=ALU.mult), [R_ps[pA], R_prm], [R_MZ])
                        sc.op('dve', lambda e: e.tensor_tensor(out=MZ[:, :, 128:192], in0=vA[:, :, 64:128], in1=RM[:, 1, :, :], op=ALU.mult), [R_ps[pA], R_prm], [R_MZ])
                        sc.op('dve', lambda e: e.tensor_tensor(out=MZ[:, :, 64:128], in0=MZ[:, :, 0:64], in1=I4[:], op=ALU.add), [R_MZ, R_prm], [R_MZ])
                        sc.op('dve', lambda e: e.tensor_tensor(out=SC[:, 0, :, :], in0=vB[:, :, 0:64], in1=RM[:, 0, :, :], op=ALU.mult), [R_ps[pB], R_prm], [R_SC])
                        sc.op('dve', lambda e: e.tensor_scalar(out=SC[:, 0, :, :], in0=SC[:, 0, :, :], scalar1=-1.0, scalar2=None, op0=ALU.mult), [R_SC], [R_SC])
                        sc.op('dve', lambda e: e.tensor_tensor(out=SC[:, 1, :, :], in0=vB[:, :, 64:128], in1=RM[:, 2, :, :], op=ALU.mult), [R_ps[pB], R_prm], [R_SC])
                        sc.op('dve', lambda e: e.tensor_tensor(out=SC[:, 2, :, :], in0=vC, in1=RM[:, 2, :, :], op=ALU.mult), [R_ps[pC], R_prm], [R_SC])
                        for q, h in enumerate(hh):
                            for i, src in enumerate((KbT, BbT, VTb)):
                                sc.op('pe', lambda e, q=q, h=h, i=i, src=src: e.transpose(out=psT_b[0:64, q * 192 + i * 64:q * 192 + i * 64 + 64], in_=src[:, h, cs], identity=identb[0:64, 0:64]), [Rh[q], R_ident], [R_ps[7]])
                        sc.op('act', lambda e: e.copy(out=TM[:].rearrange("p q x -> p (q x)"), in_=psT_b[0:64, 0:768]), [R_ps[7]], [R_TM])
                        vD = psum[pD][0:64, :].rearrange("p (q x) -> p q x", x=128)
                        vE = psum[pE][0:64, 0:256].rearrange("p (q x) -> p q x", x=64)
                        for lev in range(6):
                            for q in range(4):
                                if lev == 0:
                                    sc.op('pe', lambda e, q=q: e.matmul(psum[pD][0:64, q * 128:q * 128 + 64], lhsT=MZ[:, q, 128:192], rhs=MZ[:, q, 0:64], start=True, stop=True), [R_MZ], [R_ps[pD]])
                                elif lev < 5:
                                    sc.op('pe', lambda e, q=q: e.matmul(psum[pD][0:64, q * 128:q * 128 + 128], lhsT=MZ[:, q, 128:192], rhs=MZ[:, q, 0:128], start=True, stop=True), [R_MZ], [R_ps[pD]])
                                else:
                                    sc.op('pe', lambda e, q=q: e.matmul(psum[pD][0:64, q * 128 + 64:q * 128 + 128], lhsT=MZ[:, q, 128:192], rhs=MZ[:, q, 64:128], start=True, stop=True), [R_MZ], [R_ps[pD]])
                                if lev < 5:
                                    sc.op('pe', lambda e, q=q: e.matmul(psum[pE][0:64, q * 64:q * 64 + 64], lhsT=MZ[:, q, 0:64], rhs=MZ[:, q, 128:192], start=True, stop=True), [R_MZ], [R_ps[pE]])
                            if lev >= 1:
                                sc.op('dve', lambda e: e.tensor_tensor(out=MZ[:, :, 64:128], in0=vD[:, :, 64:128], in1=MZ[:, :, 64:128], op=ALU.add), [R_ps[pD], R_MZ], [R_MZ])
                            if lev < 5:
                                sc.op('act', lambda e: e.copy(out=MZ[:, :, 0:64], in_=vD[:, :, 0:64]), [R_ps[pD]], [R_MZ])
                                sc.op('act', lambda e: e.copy(out=MZ[:, :, 128:192], in_=vE), [R_ps[pE]], [R_MZ])
                        RP = R_P[hg]
                        for q, h in enumerate(hh):
                            sc.op('pe', lambda e, q=q, h=h: e.matmul(psum[pF][0:64, q * 64:q * 64 + 64], lhsT=QtT[:, h, cs], rhs=Pb[:, h, :], start=True, stop=False), [Rh[q], RP], [R_ps[pF]])
                            sc.op('pe', lambda e, q=q: e.matmul(psum[pF][0:64, q * 64:q * 64 + 64], lhsT=SC[:, 0, q, :], rhs=TM[:, q, 128:192], start=False, stop=True), [R_SC, R_TM], [R_ps[pF]])
                        sc.op('act', lambda e: e.copy(out=Xs[:].rearrange("p q x -> p (q x)"), in_=psum[pF][0:64, 0:256]), [R_ps[pF]], [R_X])
                        for q in range(4):
                            sc.op('pe', lambda e, q=q: e.matmul(psum[pF][0:64, 256 + q * 64:256 + q * 64 + 64], lhsT=MZ[:, q, 64:128], rhs=Xs[:, q, :], start=True, stop=True), [R_MZ, R_X], [R_ps[pF]])
                        sc.op('act', lambda e: e.mul(out=Un[:].rearrange("p q x -> p (q x)"), in_=psum[pF][0:64, 256:512], mul=-1.0), [R_ps[pF]], [R_U])
                        for q, h in enumerate(hh):
                            sc.op('pe', lambda e, q=q: e.matmul(psum[pG][0:64, q * 64:q * 64 + 64], lhsT=TM[:, q, 0:64], rhs=TM[:, q, 128:192], start=True, stop=False), [R_TM], [R_ps[pG]])
                            sc.op('pe', lambda e, q=q: e.matmul(psum[pG][0:64, q * 64:q * 64 + 64], lhsT=TM[:, q, 64:128], rhs=Un[:, q, :], start=False, stop=True), [R_TM, R_U], [R_ps[pG]])
                        for q, h in enumerate(hh):
                            sc.op('pe', lambda e, q=q, h=h: e.matmul(psum[pG][0:64, 256 + q * 64:256 + q * 64 + 64], lhsT=Pb[:, h, :], rhs=RtT[:, h, cs], start=True, stop=False), [Rh[q], RP], [R_ps[pG]])
                            sc.op('pe', lambda e, q=q: e.matmul(psum[pG][0:64, 256 + q * 64:256 + q * 64 + 64], lhsT=TM[:, q, 128:192], rhs=SC[:, 1, q, :], start=False, stop=False), [R_TM, R_SC], [R_ps[pG]])
                            sc.op('pe', lambda e, q=q: e.matmul(psum[pG][0:64, 256 + q * 64:256 + q * 64 + 64], lhsT=Un[:, q, :], rhs=SC[:, 2, q, :], start=False, stop=True), [R_U, R_SC], [R_ps[pG]])
                        sc.op('act', lambda e: e.copy(out=yT[:, hg * 4:hg * 4 + 4, cs], in_=psum[pG][0:64, 256:512].rearrange("p (q x) -> p q x", x=64)), [R_ps[pG]], [R_yT[hg]])
                        for q, h in enumerate(hh):
                            sc.op('dve', lambda e, q=q, h=h: e.scalar_tensor_tensor(out=Pf[:, h, :], in0=Pf[:, h, :], scalar=GC[:, h, c:c + 1], in1=psum[pG][0:64, q * 64:q * 64 + 64], op0=ALU.mult, op1=ALU.add), [R_ps[pG], Rh[q], RP], [RP])
                        sc.op('dve', lambda e: e.tensor_copy(out=Pb[:, hg * 4:hg * 4 + 4, :], in_=Pf[:, hg * 4:hg * 4 + 4, :]), [RP], [RP])

                    def post_head(blk, h):
                        t0 = blk * RB
                        hg = h // 4
                        tx, ty, tz, tw = T[0], T[1], T[2], T[3]
                        Rx, Ry, Rz, Rw = R_T[0], R_T[1], R_T[2], R_T[3]
                        p1 = pre_ps(); p2 = pre_ps()
                        sc.op('pe', lambda e: e.matmul(psum[p1][0:64, 256:512], lhsT=o1, rhs=yT[:, h, :], start=True, stop=True), [R_yT[hg], R_MC], [R_ps[p1]])
                        sc.op('act', lambda e: e.activation(out=tx[:], in_=yT[:, h, :], func=AF.Square), [R_yT[hg]], [Rx])
                        sc.op('pe', lambda e: e.matmul(psum[p2][0:64, 256:512], lhsT=o1, rhs=tx[:], start=True, stop=True), [Rx, R_MC], [R_ps[p2]])
                        sc.op('act', lambda e: e.mul(out=ty[:], in_=psum[p1][0:64, 256:512], mul=1.0 / 64), [R_ps[p1]], [Ry])
                        sc.op('act', lambda e: e.mul(out=tz[:], in_=psum[p2][0:64, 256:512], mul=1.0 / 64), [R_ps[p2]], [Rz])
                        sc.op('dve', lambda e: e.tensor_tensor(out=tw[:], in0=ty[:], in1=ty[:], op=ALU.mult), [Ry], [Rw])
                        sc.op('dve', lambda e: e.tensor_tensor(out=tz[:], in0=tz[:], in1=tw[:], op=ALU.subtract), [Rz, Rw], [Rz])
                        sc.op('dve', lambda e: e.tensor_scalar(out=tz[:], in0=tz[:], scalar1=64e-5, scalar2=None, op0=ALU.add), [Rz], [Rz])
                        sc.op('act', lambda e: e.activation(out=tz[:], in_=tz[:], func=AF.Sqrt), [Rz], [Rz])
                        sc.op('dve', lambda e: e.reciprocal(out=tz[:], in_=tz[:]), [Rz], [Rz])
                        sc.op('dve', lambda e: e.tensor_tensor(out=tw[:], in0=yT[:, h, :], in1=ty[:], op=ALU.subtract), [R_yT[hg], Ry], [Rw])
                        sc.op('dve', lambda e: e.tensor_tensor(out=tw[:], in0=tw[:], in1=tz[:], op=ALU.mult), [Rw, Rz], [Rw])
                        sc.op('dve', lambda e: e.tensor_scalar(out=tw[:], in0=tw[:], scalar1=ln_g[:, h:h + 1], scalar2=ln_b[:, h:h + 1], op0=ALU.mult, op1=ALU.add), [Rw, R_prm], [Rw])
                        sc.op('pool', lambda e: e.tensor_tensor(out=tw[:], in0=tw[:], in1=BON[:, h, :], op=ALU.add), [Rw, R_opsH[h]], [Rw])
                        sc.op('pool', lambda e: e.tensor_tensor(out=OBs[:, h, :], in0=tw[:], in1=Gg[:, h, :], op=ALU.mult), [Rw, R_opsH[h]], [R_OBs])

                    OBs = sb("OBs", [64, 12, RB], BF16, ph); R_OBs = Res()

                    V32 = sb("V32", [64, 12, RB], F32, ph); R_V32 = Res()
                    if l > 0:
                        VFt = sb("VFt", [64, 12, RB], F32, ph); R_VFt = Res()
                        vbf = sb("vbf", [64, 12, RB], BF16, ph); lob = sb("lob", [32, RB], BF16, ph); R_vbf = Res(); R_lob = Res()
                        mvd = sb("mvd", [64, 12, 32], BF16, ph); mvu = sb("mvu", [32, 768], BF16, ph)
                        v0 = prm("v0", rwkv_v0[l - 1])
                        sc.dma('pool', lambda e: e.dma_start(out=mvd[:], in_=rwkv_mv_down[l - 1].rearrange("(h d) m -> d h m", d=64)), [], [R_prm])
                        sc.dma('pool', lambda e: e.dma_start(out=mvu[:], in_=rwkv_mv_up[l - 1]), [], [R_prm])

                    def pre_v(blk):
                        t0 = blk * RB
                        for h in range(12):
                            def one(h):
                                shift('dve', V32[:, h, :], zr[2], (slice(0, 64), h), mu_v[:, h:h + 1], T[2][:], R_z[2], R_T[2], R_V32)
                            one(h)
                        if l == 0:
                            sc.dma('sp', lambda e: e.dma_start(out=VF.rearrange("(h d) s -> d h s", d=64)[:, :, t0:t0 + RB], in_=V32[:]), [R_V32], [R_VF])
                        else:
                            sc.dma('sp', lambda e: e.dma_start(out=VFt[:], in_=VF.rearrange("(h d) s -> d h s", d=64)[:, :, t0:t0 + RB]), [R_VF], [R_VFt])
                            sc.op('act', lambda e: e.copy(out=vbf[:], in_=V32[:]), [R_V32], [R_vbf])
                            pl = pre_ps()
                            for h in range(12):
                                sc.op('pe', lambda e, h=h: e.matmul(psum[pl][0:32, 256:512], lhsT=mvd[:, h, :], rhs=vbf[:, h, :], start=(h == 0), stop=(h == 11)), [R_vbf, R_prm], [R_ps[pl]])
                            sc.op('act', lambda e: e.copy(out=lob[:], in_=psum[pl][0:32, 256:512]), [R_ps[pl]], [R_lob])
                            for h in range(12):
                                def one2(h):
                                    p_ = pre_ps()
                                    sc.op('pe', lambda e: e.matmul(psum[p_][0:64, 256:512], lhsT=mvu[:, h * 64:(h + 1) * 64], rhs=lob[:], start=True, stop=True), [R_lob, R_prm], [R_ps[p_]])
                                    sc.op('act', lambda e: e.activation(out=T[0][:], in_=psum[p_][0:64, 256:512], func=AF.Sigmoid, bias=v0[:, h:h + 1]), [R_ps[p_], R_prm], [R_T[0]])
                                    sc.op('pool', lambda e: e.tensor_tensor(out=T[1][:], in0=VFt[:, h, :], in1=V32[:, h, :], op=ALU.subtract), [R_VFt, R_V32], [R_T[1]])
                                    sc.op('dve', lambda e: e.tensor_tensor(out=T[1][:], in0=T[1][:], in1=T[0][:], op=ALU.mult), [R_T[1], R_T[0]], [R_T[1]])
                                    sc.op('pool', lambda e: e.tensor_tensor(out=V32[:, h, :], in0=V32[:, h, :], in1=T[1][:], op=ALU.add), [R_T[1], R_V32], [R_V32])
                                one2(h)

                    def rw_block(blk):
                        load_block(blk)
                        pre_v(blk)
                        for h in range(12):
                            pre_head(blk, h)
                        if RW_STAGE >= 2:
                            for c in range(NCH):
                                for hg in range(3):
                                    chunk_group(blk, c, hg)
                        if RW_STAGE >= 3:
                            for h in range(12):
                                post_head(blk, h)
                            sc.dma('sp', lambda e: e.dma_start(out=OB.rearrange("(h d) s -> d h s", d=64)[:, :, blk * RB:(blk + 1) * RB], in_=OBs[:]), [R_OBs], [R_OB])

                    for blk in range(NBLK if RW_BLOCKS is None else RW_BLOCKS):
                        rw_block(blk)
                sc.barrier()
            if DO_F:
              with ExitStack() as ph:
                TGF = 256; NTF = S // TGF
                PA = sb("PA", [128, 6, D], BF16, ph); PB = sb("PB", [128, 6, D], BF16, ph); PC = sb("PC", [128, 2, D], BF16, ph); WO = sb("WO", [128, KC, D], BF16, ph); R_W = Res()
                for kc in range(6):
                    sc.dma('pool', lambda e, kc=kc: e.dma_start(out=PA[:, kc, :], in_=p_a[l, kc * 128:(kc + 1) * 128, :]), [], [R_W])
                    sc.dma('pool', lambda e, kc=kc: e.dma_start(out=PB[:, kc, :], in_=p_b[l, kc * 128:(kc + 1) * 128, :]), [], [R_W])
                for kc in range(2):
                    sc.dma('pool', lambda e, kc=kc: e.dma_start(out=PC[:, kc, :], in_=p_c[l, kc * 128:(kc + 1) * 128, :]), [], [R_W])
                for kc in range(KC):
                    sc.dma('pool', lambda e, kc=kc: e.dma_start(out=WO[:, kc, :], in_=w_o[l, kc * 128:(kc + 1) * 128, :]), [], [R_W])
                lng = sb("lng", [128, KC], F32, ph); lnb = sb("lnb", [128, KC], F32, ph); g1p = sb("g1p", [128, KC], F32, ph); R_ln = Res()
                sc.dma('sp', lambda e: e.dma_start(out=lng[:], in_=ln1_g[l].rearrange("(k p) -> p k", p=128), allow_slow_non_contiguous=True), [], [R_ln])
                sc.dma('sp', lambda e: e.dma_start(out=lnb[:], in_=ln1_b[l].rearrange("(k p) -> p k", p=128), allow_slow_non_contiguous=True), [], [R_ln])
                sc.op('dve', lambda e: e.tensor_scalar(out=g1p[:], in0=modT[:, 32:48], scalar1=1.0, scalar2=None, op0=ALU.add), [R_mod], [R_ln])
                oaT = sb("oaT", [128, 6, TGF], BF16, ph); obT = sb("obT", [128, 6, TGF], BF16, ph); ocT = sb("ocT", [128, 2, TGF], BF16, ph); R_o = Res()
                gAB = [sb("gAB%d" % i, [128, 3, 4, TGF], BF16, ph) for i in range(2)]; R_g = [Res(), Res()]
                mrg = sb("mrg", [128, KC, TGF], BF16, ph); R_mrg = Res()
                xy = sb("xy", [128, KC, TGF], F32, ph); R_xy = Res()
                tA = [sb("tA%d" % i, [128, TGF], F32, ph) for i in range(2)]; R_tA = [Res(), Res()]
                tB = [sb("tB%d" % i, [128, TGF], F32, ph) for i in range(2)]; R_tB = [Res(), Res()]
                mean = sb("mean", [128, TGF], F32, ph); rstd = sb("rstd", [128, TGF], F32, ph); R_st = Res()
                XTv = XT.rearrange("(k p) s -> p k s", p=128)
                PRv = PROJ.rearrange("(k p) s -> p k s", p=128)
                ALPHA = 8.0 ** 0.25
                cntF = [0]

                def f_merge_dc(dc, gb):
                    j = dc % 4
                    pa = nextps(); pb_ = nextps(); pc = nextps()
                    for kc in range(6):
                        sc.op('pe', lambda e, kc=kc: e.matmul(psum[pa][:, 0:TGF], lhsT=PA[:, kc, dc * 128:(dc + 1) * 128], rhs=oaT[:, kc, :], start=(kc == 0), stop=(kc == 5)), [R_W, R_o], [R_ps[pa]])
                    for kc in range(6):
                        sc.op('pe', lambda e, kc=kc: e.matmul(psum[pb_][:, 0:TGF], lhsT=PB[:, kc, dc * 128:(dc + 1) * 128], rhs=obT[:, kc, :], start=(kc == 0), stop=(kc == 5)), [R_W, R_o], [R_ps[pb_]])
                    for kc in range(2):
                        sc.op('pe', lambda e, kc=kc: e.matmul(psum[pc][:, 0:TGF], lhsT=PC[:, kc, dc * 128:(dc + 1) * 128], rhs=ocT[:, kc, :], start=(kc == 0), stop=(kc == 1)), [R_W, R_o], [R_ps[pc]])
                    i = cntF[0] % 2; cntF[0] += 1
                    sc.op('dve', lambda e: e.tensor_tensor(out=tA[i][:], in0=psum[pa][:, 0:TGF], in1=gAB[gb][:, 0, j, :], op=ALU.mult), [R_ps[pa], R_g[gb]], [R_tA[i]])
                    sc.op('dve', lambda e: e.tensor_tensor(out=tB[i][:], in0=psum[pb_][:, 0:TGF], in1=gAB[gb][:, 1, j, :], op=ALU.mult), [R_ps[pb_], R_g[gb]], [R_tB[i]])
                    sc.op('pool', lambda e: e.tensor_tensor(out=tA[i][:], in0=tA[i][:], in1=tB[i][:], op=ALU.add), [R_tA[i], R_tB[i]], [R_tA[i]])
                    sc.op('dve', lambda e: e.tensor_tensor(out=tB[i][:], in0=psum[pc][:, 0:TGF], in1=gAB[gb][:, 2, j, :], op=ALU.mult), [R_ps[pc], R_g[gb]], [R_tB[i]])
                    sc.op('pool', lambda e: e.tensor_tensor(out=mrg[:, dc, :], in0=tA[i][:], in1=tB[i][:], op=ALU.add), [R_tA[i], R_tB[i]], [R_mrg])

                def f_wo_dc(dc):
                    pm = nextps()
                    for kc in range(KC):
                        sc.op('pe', lambda e, kc=kc: e.matmul(psum[pm][:, 0:TGF], lhsT=WO[:, kc, dc * 128:(dc + 1) * 128], rhs=mrg[:, kc, :], start=(kc == 0), stop=(kc == KC - 1)), [R_W, R_mrg], [R_ps[pm]])
                    sc.op('act', lambda e: e.mul(out=xy[:, dc, :], in_=xy[:, dc, :], mul=ALPHA), [R_xy], [R_xy])
                    sc.op('dve', lambda e: e.scalar_tensor_tensor(out=xy[:, dc, :], in0=psum[pm][:, 0:TGF], scalar=g1p[:, dc:dc + 1], in1=xy[:, dc, :], op0=ALU.mult, op1=ALU.add), [R_ps[pm], R_ln, R_xy], [R_xy])

                def f_tg(tf):
                    ts = slice(tf * TGF, (tf + 1) * TGF)
                    RX = R_XT[tf // 2]
                    sc.dma('sp', lambda e: e.dma_start(out=oaT[:], in_=OA.rearrange("(k p) s -> p k s", p=128)[:, :, ts]), [R_OA], [R_o])
                    sc.dma('sp', lambda e: e.dma_start(out=obT[:], in_=OB.rearrange("(k p) s -> p k s", p=128)[:, :, ts]), [R_OB], [R_o])
                    sc.dma('sp', lambda e: e.dma_start(out=ocT[:], in_=OC.rearrange("(k p) s -> p k s", p=128)[:, :, ts]), [R_OC], [R_o])
                    sc.dma('sp', lambda e: e.dma_start(out=xy[:], in_=XTv[:, :, ts]), [RX], [R_xy])
                    for g4 in range(4):
                        gb = g4 % 2
                        for br in range(3):
                            k0 = (OFF_G + br * D) // 128 + g4 * 4
                            sc.dma('sp', lambda e, k0=k0, gb=gb, br=br: e.dma_start(out=gAB[gb][:, br, :, :], in_=PRv[:, k0:k0 + 4, ts]), [R_PROJ], [R_g[gb]])
                        for j in range(4):
                            f_merge_dc(g4 * 4 + j, gb)
                    for dc in range(KC):
                        f_wo_dc(dc)
                    ln_feature_major(xy, R_xy, TGF, lng, lnb, R_ln, tA, R_tA, tB, R_tB, mean, rstd, R_st)
                    sc.dma('sp', lambda e: e.dma_start(out=XTv[:, :, ts], in_=xy[:]), [R_xy], [RX])

                for tf in range(NTF):
                    f_tg(tf)
              sc.barrier()
            if DO_G:
              with ExitStack() as ph:
                TGM = 1024; NSUB = 2; NTM = S // TGM
                hTm_ = sb("hTm", [128, KC, TGM], BF16, ph); R_hTm = Res()
                acc = sb("acc", [128, KC, TGM], F32, ph); R_acc = [Res() for _ in range(NSUB)]
                Wgu = [sb("Wgu%d" % i, [128, KC, 512], BF16, ph) for i in range(2)]; Wd = [sb("Wd%d" % i, [128, 2, D], BF16, ph) for i in range(2)]; R_Wg = [Res(), Res()]; R_Wu = [Res(), Res()]; R_Wdn = [Res(), Res()]
                gwb = [sb("gwb%d" % i, [128, TGM], F32, ph) for i in range(2)]; R_gwb = [Res(), Res()]
                xt4 = [sb("xt4%d" % i, [128, 512], F32, ph) for i in range(2)]; R_xt4 = [Res(), Res()]
                hf = [sb("hf%d" % i, [128, 512], F32, ph) for i in range(2)]; R_hf = [Res(), Res()]
                sg = [sb("sg%d" % i, [128, 512], F32, ph) for i in range(2)]; R_sg = [Res(), Res()]
                actT = [sb("actT%d" % i, [128, 2, 512], BF16, ph) for i in range(2)]; R_actT = [Res(), Res()]
                Wr = sb("Wr", [128, KC, 72], F32, ph); br72 = sb("br72", [128, 4, 72], F32, ph); R_Wr = Res()
                sc.dma('sp', lambda e: e.dma_start(out=Wr[:, :, 0:8], in_=router_grp_w[l].rearrange("(k p) n -> p k n", p=128)), [], [R_Wr])
                sc.dma('sp', lambda e: e.dma_start(out=Wr[:, :, 8:72], in_=router_exp_w[l].rearrange("(k p) n -> p k n", p=128)), [], [R_Wr])
                for t_ in range(4):
                    sc.dma('sp', lambda e, t_=t_: e.dma_start(out=br72[:, t_, 0:8], in_=bass.AP(router_grp_b.tensor, l * 8, [[0, 128], [1, 8]])), [], [R_Wr])
                    sc.dma('sp', lambda e, t_=t_: e.dma_start(out=br72[:, t_, 8:72], in_=bass.AP(router_exp_b.tensor, l * 64, [[0, 128], [1, 64]])), [], [R_Wr])
                lng2 = sb("lng2", [128, KC], F32, ph); lnb2 = sb("lnb2", [128, KC], F32, ph); g2p = sb("g2p", [128, KC], F32, ph); sc2p = sb("sc2p", [128, KC], F32, ph); R_ln2 = Res()
                sc.dma('sp', lambda e: e.dma_start(out=lng2[:], in_=ln2_g[l].rearrange("(k p) -> p k", p=128), allow_slow_non_contiguous=True), [], [R_ln2])
                sc.dma('sp', lambda e: e.dma_start(out=lnb2[:], in_=ln2_b[l].rearrange("(k p) -> p k", p=128), allow_slow_non_contiguous=True), [], [R_ln2])
                sc.op('dve', lambda e: e.tensor_scalar(out=g2p[:], in0=modT[:, 80:96], scalar1=1.0, scalar2=None, op0=ALU.add), [R_mod], [R_ln2])
                sc.op('dve', lambda e: e.tensor_scalar(out=sc2p[:], in0=modT[:, 64:80], scalar1=1.0, scalar2=None, op0=ALU.add), [R_mod], [R_ln2])
                lgt = sb("lgt", [128, 4, 72], F32, ph); R_lgt = Res()
                rt = sb("rt", [128, 8, 64], F32, ph); R_rt = Res()
                gwt = sb("gwt", [128, 64], F32, ph); gwT = sb("gwT", [64, 512], F32, ph); R_gwT = Res()
                tAm = sg; R_tAm = R_sg
                tBm = hf; R_tBm = R_hf
                meanm = sb("meanm", [128, 512], F32, ph); rstdm = sb("rstdm", [128, 512], F32, ph); R_st2 = Res()
                XTv2 = XT.rearrange("(k p) s -> p k s", p=128)
                ALPHA = 8.0 ** 0.25
                cntG = [0]

                def route_tile(tm, sub, t):
                    L = lgt[:, t, :]
                    r = rt
                    c1 = r[:, 0, 0:1]; c2 = r[:, 0, 1:2]; c3 = r[:, 0, 2:3]; c4 = r[:, 0, 3:4]; c5 = r[:, 0, 4:5]; c6 = r[:, 0, 5:6]
                    RL = [R_lgt, R_rt]
                    def O(eng, f):
                        sc.op(eng, f, RL, [R_rt])
                    O('dve', lambda e: e.tensor_reduce(out=c1, in_=L[:, 0:8], axis=AX.X, op=ALU.max))
                    O('dve', lambda e: e.tensor_scalar(out=r[:, 1, 0:8], in0=L[:, 0:8], scalar1=c1, scalar2=None, op0=ALU.subtract))
                    O('act', lambda e: e.activation(out=r[:, 1, 8:16], in_=r[:, 1, 0:8], func=AF.Exp))
                    O('dve', lambda e: e.tensor_reduce(out=c2, in_=r[:, 1, 8:16], axis=AX.X, op=ALU.add))
                    O('dve', lambda e: e.reciprocal(out=c2, in_=c2))
                    O('dve', lambda e: e.tensor_scalar(out=r[:, 1, 16:24], in0=L[:, 0:8], scalar1=c1, scalar2=None, op0=ALU.is_ge))
                    O('dve', lambda e: e.tensor_scalar(out=r[:, 1, 16:24], in0=r[:, 1, 16:24], scalar1=-1.0, scalar2=1e30, op0=ALU.add, op1=ALU.mult))
                    for g in range(8):
                        O('dve', lambda e, g=g: e.tensor_scalar(out=r[:, 2, g * 8:(g + 1) * 8], in0=L[:, 8 + g * 8:16 + g * 8], scalar1=r[:, 1, 16 + g:17 + g], scalar2=None, op0=ALU.add))
                    O('dve', lambda e: e.max(out=r[:, 3, 0:8], in_=r[:, 2, :]))
                    O('dve', lambda e: e.tensor_scalar(out=r[:, 4, :], in0=r[:, 2, :], scalar1=r[:, 3, 0:1], scalar2=None, op0=ALU.is_ge))
                    O('dve', lambda e: e.tensor_scalar(out=r[:, 5, :], in0=r[:, 2, :], scalar1=r[:, 3, 1:2], scalar2=None, op0=ALU.is_ge))
                    O('dve', lambda e: e.tensor_tensor(out=c3, in0=r[:, 3, 1:2], in1=r[:, 3, 0:1], op=ALU.subtract))
                    O('act', lambda e: e.activation(out=c3, in_=c3, func=AF.Exp))
                    O('dve', lambda e: e.tensor_scalar(out=c3, in0=c3, scalar1=1.0, scalar2=None, op0=ALU.add))
                    O('dve', lambda e: e.reciprocal(out=c3, in_=c3))
                    O('dve', lambda e: e.tensor_scalar(out=c4, in0=c3, scalar1=-1.0, scalar2=1.0, op0=ALU.mult, op1=ALU.add))
                    O('dve', lambda e: e.tensor_tensor(out=c5, in0=c3, in1=c4, op=ALU.subtract))
                    O('dve', lambda e: e.tensor_tensor(out=c5, in0=c5, in1=c2, op=ALU.mult))
                    O('dve', lambda e: e.tensor_tensor(out=c6, in0=c4, in1=c2, op=ALU.mult))
                    O('dve', lambda e: e.tensor_scalar(out=r[:, 5, :], in0=r[:, 5, :], scalar1=c6, scalar2=None, op0=ALU.mult))
                    sc.op('dve', lambda e: e.scalar_tensor_tensor(out=gwt[:], in0=r[:, 4, :], scalar=c5, in1=r[:, 5, :], op0=ALU.mult, op1=ALU.add), RL, [R_rt, R_gwT])
                    pt = nextps()
                    sc.op('pe', lambda e: e.transpose(out=psum[pt][0:64, 0:128], in_=gwt[:], identity=ident[:]), [R_gwT, R_ident], [R_ps[pt]])
                    sc.op('act', lambda e: e.copy(out=gwT[:, t * 128:(t + 1) * 128], in_=psum[pt][0:64, 0:128]), [R_ps[pt]], [R_gwT])

                def m_sub_prep(tm, sub):
                    t0 = tm * TGM + sub * 512
                    pl = nextps()
                    for dc in range(KC):
                        def two(dc):
                            b = cntG[0] % 2; cntG[0] += 1
                            i = dc % 2
                            sc.dma('sp', lambda e: e.dma_start(out=xt4[b][:], in_=XTv2[:, dc, t0:t0 + 512]), [R_XT[t0 // 512]], [R_xt4[b]])
                            sc.op('dve', lambda e: e.tensor_scalar(out=hf[i][:], in0=xt4[b][:], scalar1=sc2p[:, dc:dc + 1], scalar2=modT[:, 48 + dc:49 + dc], op0=ALU.mult, op1=ALU.add), [R_xt4[b], R_ln2, R_mod], [R_hf[i]])
                            sc.op('act', lambda e: e.copy(out=hTm_[:, dc, sub * 512:(sub + 1) * 512], in_=hf[i][:]), [R_hf[i]], [R_hTm])
                            for t in range(4):
                                sc.op('pe', lambda e, t=t: e.matmul(psum[pl][:, t * 72:(t + 1) * 72], lhsT=hf[i][:, t * 128:(t + 1) * 128], rhs=Wr[:, dc, :], start=(dc == 0 and t == 0), stop=(dc == KC - 1)), [R_hf[i], R_Wr], [R_ps[pl]])
                        two(dc)
                    sc.op('dve', lambda e: e.tensor_tensor(out=lgt[:], in0=psum[pl][:, 0:288].rearrange("p (t n) -> p t n", n=72), in1=br72[:], op=ALU.add), [R_ps[pl], R_Wr], [R_lgt])
                    for t in range(4):
                        route_tile(tm, sub, t)
                    sc.dma('sp', lambda e: e.dma_start(out=GW[:, t0:t0 + 512], in_=gwT[:]), [R_gwT], [R_GW])

                def m_load(tm, e_, wb):
                    sc.dma('pool', lambda e: e.dma_start(out=Wgu[wb][:, :, 0:256], in_=exp_w_gate[l, e_].rearrange("(k p) f -> p k f", p=128)), [], [R_Wg[wb]])
                    sc.dma('pool', lambda e: e.dma_start(out=Wgu[wb][:, :, 256:512], in_=exp_w_up[l, e_].rearrange("(k p) f -> p k f", p=128)), [], [R_Wu[wb]])
                    sc.dma('pool', lambda e: e.dma_start(out=Wd[wb][:], in_=exp_w_down[l, e_].rearrange("(k p) f -> p k f", p=128)), [], [R_Wdn[wb]])
                    sc.dma('sp', lambda e: e.dma_start(out=gwb[wb][:], in_=bass.AP(GW.tensor, e_ * S + tm * TGM, [[0, 128], [1, TGM]])), [R_GW], [R_gwb[wb]])

                def m_gu(tm, e_, wb, sub):
                    cs = slice(sub * 512, (sub + 1) * 512)
                    ab = cntG[0] % 2; cntG[0] += 1
                    for fc in range(2):
                        def two(fc):
                            pg = nextps(); pu = nextps()
                            for k in range(KC):
                                sc.op('pe', lambda e, k=k: e.matmul(psum[pg][:], lhsT=Wgu[wb][:, k, fc * 128:(fc + 1) * 128], rhs=hTm_[:, k, cs], start=(k == 0), stop=(k == KC - 1)), [R_Wg[wb], R_hTm], [R_ps[pg]])
                            for k in range(KC):
                                sc.op('pe', lambda e, k=k: e.matmul(psum[pu][:], lhsT=Wgu[wb][:, k, 256 + fc * 128:256 + (fc + 1) * 128], rhs=hTm_[:, k, cs], start=(k == 0), stop=(k == KC - 1)), [R_Wu[wb], R_hTm], [R_ps[pu]])
                            i = fc
                            sc.op('act', lambda e: e.activation(out=sg[i][:], in_=psum[pg][:], func=AF.Silu), [R_ps[pg]], [R_sg[i]])
                            sc.op('dve', lambda e: e.tensor_tensor(out=sg[i][:], in0=psum[pu][:], in1=sg[i][:], op=ALU.mult), [R_ps[pu], R_sg[i]], [R_sg[i]])
                            sc.op('pool', lambda e: e.tensor_tensor(out=actT[ab][:, fc, :], in0=sg[i][:], in1=gwb[wb][:, cs], op=ALU.mult), [R_sg[i], R_gwb[wb]], [R_actT[ab]])
                        two(fc)
                    return ab

                def m_dn(tm, e_, wb, sub, ab):
                    cs = slice(sub * 512, (sub + 1) * 512)
                    for dc in range(KC):
                        def three(dc):
                            pd = nextps()
                            for fc in range(2):
                                sc.op('pe', lambda e, fc=fc: e.matmul(psum[pd][:], lhsT=Wd[wb][:, fc, dc * 128:(dc + 1) * 128], rhs=actT[ab][:, fc, :], start=(fc == 0), stop=(fc == 1)), [R_Wdn[wb], R_actT[ab]], [R_ps[pd]])
                            if e_ == 0:
                                sc.op('act', lambda e: e.copy(out=acc[:, dc, cs], in_=psum[pd][:]), [R_ps[pd]], [R_acc[sub]])
                            else:
                                sc.op('dve', lambda e: e.tensor_tensor(out=acc[:, dc, cs], in0=psum[pd][:], in1=acc[:, dc, cs], op=ALU.add), [R_ps[pd], R_acc[sub]], [R_acc[sub]])
                        three(dc)

                def m_finish(tm, sub):
                    t0 = tm * TGM + sub * 512
                    cs = slice(sub * 512, (sub + 1) * 512)
                    for dc in range(KC):
                        def one(dc):
                            b = cntG[0] % 2; cntG[0] += 1
                            sc.dma('sp', lambda e: e.dma_start(out=xt4[b][:], in_=XTv2[:, dc, t0:t0 + 512]), [R_XT[t0 // 512]], [R_xt4[b]])
                            sc.op('act', lambda e: e.mul(out=xt4[b][:], in_=xt4[b][:], mul=ALPHA), [R_xt4[b]], [R_xt4[b]])
                            sc.op('dve', lambda e: e.scalar_tensor_tensor(out=acc[:, dc, cs], in0=acc[:, dc, cs], scalar=g2p[:, dc:dc + 1], in1=xt4[b][:], op0=ALU.mult, op1=ALU.add), [R_acc[sub], R_ln2, R_xt4[b]], [R_acc[sub]])
                        one(dc)
                    ln_feature_major(acc[:, :, cs], R_acc[sub], 512, lng2, lnb2, R_ln2, tAm, R_tAm, tBm, R_tBm, meanm, rstdm, R_st2)
                    sc.dma('sp', lambda e: e.dma_start(out=XTv2[:, :, t0:t0 + 512], in_=acc[:, :, cs]), [R_acc[sub]], [R_XT[t0 // 512]])

                def m_group(tm):
                    for sub in range(NSUB):
                        m_sub_prep(tm, sub)
                    prev = None
                    for e_ in range(NEXP):
                        m_load(tm, e_, e_ % 2)
                        for sub in range(NSUB):
                            ab = m_gu(tm, e_, e_ % 2, sub)
                            if prev is not None:
                                m_dn(*prev)
                            prev = (tm, e_, e_ % 2, sub, ab)
                    m_dn(*prev)
                    for sub in range(NSUB):
                        m_finish(tm, sub)

                for tm in range(NTM if MOE_GROUPS is None else MOE_GROUPS):
                    m_group(tm)
              sc.barrier()
        for l_ in range(n_layers):
            layer(l_)
            sc.new_epoch()
        with ExitStack() as ph:
          if DO_Z:
            zin = [sb("zin%d" % i, [128, KC, 128], F32, ph) for i in range(2)]; R_zin = [Res(), Res()]
            zst = [sb("zst%d" % i, [128, D], F32, ph) for i in range(2)]; R_zst = [Res(), Res()]
            def z_tile(t, b):
                sc.dma('sp', lambda e: e.dma_start(out=zin[b][:], in_=XT.rearrange("(k p) s -> p k s", p=128)[:, :, t * 128:(t + 1) * 128]), [R_XT[t // 4]], [R_zin[b]])
                for g in range(4):
                    pi = nextps()
                    for j in range(4):
                        dc = g * 4 + j
                        sc.op('pe', lambda e, pi=pi, j=j, dc=dc: e.transpose(out=psum[pi][:, j * 128:(j + 1) * 128], in_=zin[b][:, dc, :], identity=ident[:]), [R_zin[b], R_ident], [R_ps[pi]])
                    if g % 2 == 0:
                        sc.op('act', lambda e, pi=pi, g=g: e.copy(out=zst[b][:, g * 512:(g + 1) * 512], in_=psum[pi][:]), [R_ps[pi]], [R_zst[b]])
                    else:
                        sc.op('dve', lambda e, pi=pi, g=g: e.tensor_copy(out=zst[b][:, g * 512:(g + 1) * 512], in_=psum[pi][:]), [R_ps[pi]], [R_zst[b]])
                sc.dma('pool', lambda e: e.dma_start(out=Y[t * 128:(t + 1) * 128, :], in_=zst[b][:]), [R_zst[b]], [R_Y])
            for t in range(S // 128):
                z_tile(t, t % 2)
        sc.emit()
    return nc


def kernel(**inputs):
    x = np.ascontiguousarray(inputs["x"], dtype=np.float32)
    nb = x.shape[0]
    nc = build(n_layers=4, debug=False)
    hc = host_consts()
    shared = {k: np.ascontiguousarray(v, dtype=np.float32) for k, v in inputs.items() if k not in ("x", "c")}
    in_maps = []
    for b in range(nb):
        m = {"x": x[b], "c": np.ascontiguousarray(inputs["c"][b], dtype=np.float32)}
        m.update(shared); m.update(hc)
        in_maps.append(m)
    res = run_bass_kernel_spmd(nc, in_maps, core_ids=list(range(nb)))
    return np.stack([np.asarray(r["y"], dtype=np.float32) for r in res.results], axis=0)
```
